# Optimizing a Trainium2 kernel written in Bass

```python
import jax, jax.numpy as jnp
from jax import lax
import numpy as np

D_MODEL = 1024
BATCH = 4
SEQ = 8192
DEPTH = 2

CHUNK = 64
N_EVEN = (DEPTH + 1) // 2
N_ODD = DEPTH // 2
A_WIDTH = D_MODEL // 2
A_GROUPS = 8
A_WIDTH_CONV = 31
B_WIDTH = D_MODEL // 2
B_GROUPS = 8
B_WIDTH_CONV = 3
IN_EVEN = 2 * A_WIDTH + 3 * B_WIDTH
MIX_EVEN = A_WIDTH + B_WIDTH
C_WIDTH = D_MODEL
C_GROUPS = 8
C_GROUP_DIM = C_WIDTH // C_GROUPS
C_BLOCK = 128
MEM_LEN = 256
XA_HEADS = 4
XA_HEAD_DIM = D_MODEL // XA_HEADS
D_FF = 2816
N_EXPERTS = 8
TOP_K = 2
RMS_EPS = 1e-6
LN_EPS = 1e-5

kernel_name = "hybrid_conv_gmlp_moe_encoder"


def rms_norm(x, g):
    xf = x.astype(jnp.float32)
    y = xf * lax.rsqrt(jnp.mean(xf * xf, axis=-1, keepdims=True) + RMS_EPS)
    return (y * g.astype(jnp.float32)).astype(x.dtype)


def layer_norm(x, g, b):
    xf = x.astype(jnp.float32)
    mu = jnp.mean(xf, axis=-1, keepdims=True)
    var = jnp.mean(jnp.square(xf - mu), axis=-1, keepdims=True)
    y = (xf - mu) * lax.rsqrt(var + LN_EPS)
    return (y * g.astype(jnp.float32) + b.astype(jnp.float32)).astype(x.dtype)


def causal_depthwise_conv(x, w):
    k_len, ch = w.shape
    return lax.conv_general_dilated(
        x, w[:, None, :].astype(x.dtype), window_strides=(1,), padding=[(k_len - 1, 0)],
        dimension_numbers=("NWC", "WIO", "NWC"), feature_group_count=ch)


def swiglu(h, w_gate, w_up, w_down):
    return (jax.nn.silu(h @ w_gate) * (h @ w_up)) @ w_down


def conv_mixers(h, w_in, a_conv_w, a_conv_b, a_ln_g, a_ln_b, b_conv_w, w_out):
    z = h @ w_in
    a_in, b_in = z[..., :2 * A_WIDTH], z[..., 2 * A_WIDTH:]
    a_val, a_gate = jnp.split(a_in, 2, axis=-1)
    a = a_val * jax.nn.sigmoid(a_gate)
    a = causal_depthwise_conv(a, a_conv_w) + a_conv_b.astype(a.dtype)
    a = jax.nn.silu(layer_norm(a, a_ln_g, a_ln_b))
    g_b, g_c, hb = jnp.split(b_in, 3, axis=-1)
    bo = g_b * causal_depthwise_conv(g_c * hb, b_conv_w)
    return jnp.concatenate([a, bo], axis=-1) @ w_out


def spatial_gating_mixer(h, w_in, ln_g, ln_b, w_s, b_s, w_out):
    bsz, seq, _ = h.shape
    z = jax.nn.gelu(h @ w_in)
    u, v = jnp.split(z, 2, axis=-1)
    v = layer_norm(v, ln_g, ln_b)
    v = v.reshape(bsz, seq // C_BLOCK, C_BLOCK, C_GROUPS, C_GROUP_DIM)
    pos = jnp.arange(C_BLOCK)
    mask = (pos[None, :] // CHUNK) <= (pos[:, None] // CHUNK)
    ws = jnp.where(mask[None], w_s, jnp.zeros_like(w_s)).astype(v.dtype)
    s = jnp.einsum("gij,bnjgc->bnigc", ws, v) + b_s.T[:, :, None].astype(v.dtype)
    s = s.reshape(bsz, seq, C_WIDTH)
    return (u * s) @ w_out


def memory_cross_attention(h, mem_n, w_q, w_k, w_v, w_o):
    bsz, seq, _ = h.shape
    m = mem_n.shape[1]
    q = (h @ w_q).reshape(bsz, seq, XA_HEADS, XA_HEAD_DIM)
    k = (mem_n @ w_k).reshape(bsz, m, XA_HEADS, XA_HEAD_DIM)
    v = (mem_n @ w_v).reshape(bsz, m, XA_HEADS, XA_HEAD_DIM)
    s = jnp.einsum("bshd,bmhd->bhsm", q, k).astype(jnp.float32) * (XA_HEAD_DIM ** -0.5)
    p = jax.nn.softmax(s, axis=-1).astype(v.dtype)
    o = jnp.einsum("bhsm,bmhd->bshd", p, v).reshape(bsz, seq, XA_HEADS * XA_HEAD_DIM)
    return o @ w_o


def moe_swiglu(h, w_router, w_gate, w_up, w_down):
    bsz, seq, d = h.shape
    t = h.reshape(bsz * seq, d)
    logits = (t @ w_router).astype(jnp.float32)
    top_v, top_i = lax.top_k(logits, TOP_K)
    top_w = jax.nn.softmax(top_v, axis=-1)
    gates = jnp.sum(jax.nn.one_hot(top_i, N_EXPERTS, dtype=jnp.float32) * top_w[..., None], axis=1)
    gates = gates.astype(t.dtype)
    out = jnp.zeros_like(t)
    for e in range(N_EXPERTS):
        out = out + gates[:, e:e + 1] * swiglu(t, w_gate[e], w_up[e], w_down[e])
    return out.reshape(bsz, seq, d)


def setup_inputs(seed: int = 0) -> dict:
    key = jax.random.key(seed)
    ks = iter(jax.random.split(key, 40))
    f32 = jnp.float32

    def nrm(shape, scale):
        return jax.random.normal(next(ks), shape, f32) * scale

    def gain(shape):
        return 1.0 + 0.05 * jax.random.normal(next(ks), shape, f32)

    d = D_MODEL
    return {
        "x": nrm((BATCH, SEQ, d), 1.0),
        "mem": nrm((BATCH, MEM_LEN, d), 1.0),
        "norm_mix_g": gain((DEPTH, d)),
        "norm_xattn_g": gain((DEPTH, d)),
        "norm_mem_g": gain((DEPTH, d)),
        "norm_ffn_g": gain((DEPTH, d)),
        "final_norm_g": gain((d,)),
        "xa_w_q": nrm((DEPTH, d, d), d ** -0.5),
        "xa_w_k": nrm((DEPTH, d, d), d ** -0.5),
        "xa_w_v": nrm((DEPTH, d, d), d ** -0.5),
        "xa_w_o": nrm((DEPTH, d, d), d ** -0.5),
        "cv_w_in": nrm((N_EVEN, d, IN_EVEN), d ** -0.5),
        "cv_a_conv_w": nrm((N_EVEN, A_WIDTH_CONV, A_WIDTH), A_WIDTH_CONV ** -0.5),
        "cv_a_conv_b": nrm((N_EVEN, A_WIDTH), 0.02),
        "cv_a_ln_g": gain((N_EVEN, A_WIDTH)),
        "cv_a_ln_b": nrm((N_EVEN, A_WIDTH), 0.02),
        "cv_b_conv_w": nrm((N_EVEN, B_WIDTH_CONV, B_WIDTH), B_WIDTH_CONV ** -0.5),
        "cv_w_out": nrm((N_EVEN, MIX_EVEN, d), MIX_EVEN ** -0.5),
        "ffn_w_gate": nrm((N_EVEN, d, D_FF), d ** -0.5),
        "ffn_w_up": nrm((N_EVEN, d, D_FF), d ** -0.5),
        "ffn_w_down": nrm((N_EVEN, D_FF, d), D_FF ** -0.5),
        "sg_w_in": nrm((N_ODD, d, 2 * C_WIDTH), d ** -0.5),
        "sg_ln_g": gain((N_ODD, C_WIDTH)),
        "sg_ln_b": nrm((N_ODD, C_WIDTH), 0.02),
        "sg_w_s": nrm((N_ODD, C_GROUPS, C_BLOCK, C_BLOCK), C_BLOCK ** -0.5),
        "sg_b_s": gain((N_ODD, C_GROUPS, C_BLOCK)),
        "sg_w_out": nrm((N_ODD, C_WIDTH, d), C_WIDTH ** -0.5),
        "moe_w_router": nrm((N_ODD, d, N_EXPERTS), d ** -0.5),
        "moe_w_gate": nrm((N_ODD, N_EXPERTS, d, D_FF), d ** -0.5),
        "moe_w_up": nrm((N_ODD, N_EXPERTS, d, D_FF), d ** -0.5),
        "moe_w_down": nrm((N_ODD, N_EXPERTS, D_FF, d), D_FF ** -0.5),
    }


def reference(x, mem, norm_mix_g, norm_xattn_g, norm_mem_g, norm_ffn_g, final_norm_g,
              xa_w_q, xa_w_k, xa_w_v, xa_w_o,
              cv_w_in, cv_a_conv_w, cv_a_conv_b, cv_a_ln_g, cv_a_ln_b, cv_b_conv_w, cv_w_out,
              ffn_w_gate, ffn_w_up, ffn_w_down,
              sg_w_in, sg_ln_g, sg_ln_b, sg_w_s, sg_b_s, sg_w_out,
              moe_w_router, moe_w_gate, moe_w_up, moe_w_down):
    h = x
    for i in range(DEPTH):
        j = i // 2
        hn = rms_norm(h, norm_mix_g[i])
        if i % 2 == 0:
            h = h + conv_mixers(hn, cv_w_in[j], cv_a_conv_w[j], cv_a_conv_b[j], cv_a_ln_g[j],
                                cv_a_ln_b[j], cv_b_conv_w[j], cv_w_out[j])
        else:
            h = h + spatial_gating_mixer(hn, sg_w_in[j], sg_ln_g[j], sg_ln_b[j], sg_w_s[j],
                                         sg_b_s[j], sg_w_out[j])
        h = h + memory_cross_attention(rms_norm(h, norm_xattn_g[i]), rms_norm(mem, norm_mem_g[i]),
                                       xa_w_q[i], xa_w_k[i], xa_w_v[i], xa_w_o[i])
        hn = rms_norm(h, norm_ffn_g[i])
        if i % 2 == 0:
            h = h + swiglu(hn, ffn_w_gate[j], ffn_w_up[j], ffn_w_down[j])
        else:
            h = h + moe_swiglu(hn, moe_w_router[j], moe_w_gate[j], moe_w_up[j], moe_w_down[j])
    return rms_norm(h, final_norm_g)
```

```python
import contextlib
import types
import numpy as np
import concourse.bass as bass
import concourse.mybir as mybir
from concourse.bass_utils import run_bass_kernel_spmd

F32 = mybir.dt.float32
BF16 = mybir.dt.bfloat16
I32 = mybir.dt.int32
AX = mybir.AxisListType
AF = mybir.ActivationFunctionType
ALU = mybir.AluOpType

NCORES = 8
D = 1024
SEQ = 8192
TOK_CORE = 4096
RANGE = 2048
NT = RANGE // 128
NG = RANGE // 512
DFF = 2816
NF = DFF // 128
NE = 8
FGROUPS = [list(range(0, 6)), list(range(6, 12)), list(range(12, 17)), list(range(17, 22))]
HALO = 128
SPARSE = True
COMBINED = True
ARENA = 65536

ENGS = ("pe", "act", "dve", "pool", "sp")


def _snap(fn):
    if fn is None or fn.__closure__ is None:
        return fn
    cells = []
    for c in fn.__closure__:
        try:
            cells.append(types.CellType(c.cell_contents))
        except ValueError:
            cells.append(c)
    return types.FunctionType(fn.__code__, fn.__globals__, fn.__name__, fn.__defaults__, tuple(cells))


class Chan:
    def __init__(self, h):
        self.h = h
        self.count = 0


class Prog:
    def __init__(self, nc, stack):
        self.nc = nc
        self.stack = stack
        self.streams = {e: [] for e in ENGS}
        self.chan = {e: Chan(stack.enter_context(nc.semaphore("c_" + e))) for e in ENGS}
        self.allchans = list(self.chan.values())
        self.engchans = set(self.chan.values())
        self.seen = {e: {} for e in ENGS}
        self.lastw = {}
        self.readers = {}
        self.nsem = 0
        self.named = {}

    def new_chan(self, name):
        if name in self.named:
            return self.named[name]
        c = Chan(self.stack.enter_context(self.nc.semaphore("d_" + name)))
        self.allchans.append(c)
        self.named[name] = c
        return c

    def op(self, eng, fn, r=(), w=(), chan=None):
        need = {}

        def add(ch, val):
            if eng == "pe" and ch is self.chan["pe"]:
                return
            if ch not in self.engchans:
                val = ch.count
            if self.seen[eng].get(ch, 0) >= val:
                return
            if need.get(ch, 0) < val:
                need[ch] = val

        for k in r:
            t = self.lastw.get(k)
            if t:
                add(*t)
        for k in w:
            t = self.lastw.get(k)
            if t:
                add(*t)
            for ch, val in self.readers.get(k, {}).items():
                add(ch, val)
        for ch, val in need.items():
            self.seen[eng][ch] = val
        if chan is None:
            ch = self.chan[eng]
            ch.count += 1
            inc = 1
        else:
            ch = chan
            ch.count += 16
            inc = 16
        tok = (ch, ch.count)
        self.streams[eng].append((list(need.items()), _snap(fn), ch.h, inc))
        for k in r:
            d = self.readers.setdefault(k, {})
            d[ch] = ch.count
        for k in w:
            self.lastw[k] = tok
            self.readers[k] = {}
        return tok

    def barrier(self):
        for e in ENGS:
            waits = []
            for ch in self.allchans:
                if ch.count > 0 and self.seen[e].get(ch, 0) < ch.count:
                    if e == "pe" and ch is self.chan["pe"]:
                        continue
                    waits.append((ch, ch.count))
                    self.seen[e][ch] = ch.count
            if waits:
                self.streams[e].append((waits, None, None, 0))
        self.lastw = {}
        self.readers = {}

    def replay(self, eng, e):
        for waits, fn, semh, inc in self.streams[eng]:
            for ch, val in waits:
                e.wait_ge(ch.h, val)
            if fn is not None:
                fn(e).then_inc(semh, inc)


class Ring:
    def __init__(self, P, name, views):
        self.views = views
        self.n = len(views)
        self.chans = [P.new_chan(f"{name}{i}") for i in range(self.n)]
        self.chans2 = [P.new_chan(f"{name}b{i}") for i in range(self.n)]
        self.i = 0
        self.name = name

    def next(self):
        s = self.i % self.n
        self.i += 1
        self.last2 = self.chans2[s]
        return s, self.views[s], self.chans[s], (self.name, s)


def build_program(stop=None, nranges=2, only=None):
    nc = bass.Bass("TRN2", target_bir_lowering=False)

    def din(name, shape):
        return nc.dram_tensor(name, list(shape), F32, kind="ExternalInput").ap()

    x_d = din("x", [TOK_CORE, D])
    xh_d = din("xh", [2, 128, D])
    mem_d = din("mem", [256, D])
    gcols_d = din("gcols", [128, 64])
    gfin_d = din("gfin", [128, D])
    ident_d = din("ident", [128, 128])
    tri_d = din("tri", [128, 128])
    cvin_d = din("cvin", [20, 128, 1024])
    cvsm_d = din("cvsm", [128, 148])
    cvout_d = din("cvout", [D, D])
    xq_d = [din(f"xq{i}", [8, 128, 1024]) for i in range(2)]
    xk_d = [din(f"xk{i}", [8, 128, 1024]) for i in range(2)]
    xv_d = [din(f"xv{i}", [D, D]) for i in range(2)]
    xo_d = [din(f"xo{i}", [D, D]) for i in range(2)]
    fg_d = din("fg", [NF, 128, 1024])
    fu_d = din("fu", [NF, 128, 1024])
    fd_d = din("fd", [DFF, D])
    sgu_d = din("sgu", [8, 128, 1024])
    sgv_d = din("sgv", [D, D])
    sglng_d = din("sglng", [128, D])
    sglnb_d = din("sglnb", [128, D])
    sgws_d = din("sgws", [128, 1024])
    sgbs_d = din("sgbs", [128, 1024])
    sgout_d = din("sgout", [D, D])
    mr_d = din("mr", [128, 64])
    mg_d = din("mg", [NE * NF, 128, 1024])
    mu_d = din("mu", [NE * NF, 128, 1024])
    md_d = din("md", [NE * DFF, D])
    y_d = nc.dram_tensor("y", [TOK_CORE, D], F32, kind="ExternalOutput").ap()
    NSLOT = 12288
    kt_d = [nc.dram_tensor(f"kt_scr{i}", [128, 2048], BF16, kind="Internal").ap() for i in range(2)]
    v_d = [nc.dram_tensor(f"v_scr{i}", [128, 2048], BF16, kind="Internal").ap() for i in range(2)]
    hn_d = nc.dram_tensor("hn_scr", [TOK_CORE, D], BF16, kind="Internal").ap()
    hpark_d = nc.dram_tensor("hpark", [TOK_CORE, D], F32, kind="Internal").ap()
    slot_tab_d = nc.dram_tensor("slot_tab", [NSLOT, 16], I32, kind="Internal").ap()
    yslot_d = nc.dram_tensor("yslot", [NSLOT, D], F32, kind="Internal").ap()
    mg_flat = mg_d.rearrange("c p n -> (c p) n")
    mu_flat = mu_d.rearrange("c p n -> (c p) n")

    stack = contextlib.ExitStack()
    with stack:
        def sb(name, shape, dt):
            return stack.enter_context(nc.sbuf_tensor(name, list(shape), dt))

        h_t = sb("h", [128, NT, D], F32)
        arena = sb("arena", [128, ARENA], BF16)
        identb = sb("identb", [128, 128], BF16)
        identf = sb("identf", [128, 128], F32)
        onesb = sb("onesb", [128, 128], BF16)
        onesm = sb("onesm", [128, 128], BF16)
        trib = sb("trib", [128, 128], BF16)
        gcols = sb("gcols_s", [128, 64], F32)
        gfin = sb("gfin_s", [128, D], F32)
        eps6 = sb("eps6", [128, 1], F32)
        eps5 = sb("eps5", [128, 1], F32)
        stat = sb("stat", [128, 64], F32)
        gates_g = sb("gates_g", [128, 2 * NT, 8], F32)[:, :, :]
        sel_g = sb("sel_g", [128, 2 * NT, 8], F32)[:, :, :]
        slotA_ig = sb("slotA_ig", [128, 2 * NT], I32)[:, :]
        slotB_ig = sb("slotB_ig", [128, 2 * NT], I32)[:, :]
        widx_g = sb("widx_g", [128, 24, NF], I32)[:, :, :]
        psT_t = stack.enter_context(nc.psum_tensor("psT", [128, 1024], F32))
        psM_t = [stack.enter_context(nc.psum_tensor(f"psM{i}", [128, 512], F32)) for i in range(4)]
        psO_t = [stack.enter_context(nc.psum_tensor(f"psO{i}", [128, 512], F32)) for i in range(2)]

        P = Prog(nc, stack)
        c_const = P.new_chan("const")
        c_constsw = P.new_chan("constsw")
        c_xs = [P.new_chan(f"x{q}") for q in range(4)]
        c_outs = [P.new_chan(f"out{q}") for q in range(2)]
        c_out = c_outs[0]
        c_ph = P.new_chan("ph")
        c_phsw = P.new_chan("phsw")

        psTb = psT_t[:, :].bitcast(BF16).rearrange("p (a b) -> p a b", a=8)
        psTf = psT_t[:, :].rearrange("p (a b) -> p a b", a=8)

        state = {"off": 0, "pm": 0, "po": 0, "pp": 0, "nrm": 0, "po4": 0}

        def reset_arena(off=0):
            state["off"] = off

        def alloc(shape, dt):
            n = int(np.prod(shape))
            wide = dt in (F32, I32)
            ne = n * (2 if wide else 1)
            ne = (ne + 31) // 32 * 32
            off = state["off"]
            assert off + ne <= ARENA, f"arena overflow {off}+{ne}"
            state["off"] = off + ne
            v = arena[:, off:off + ne]
            if wide:
                v = v.bitcast(dt)
            v = v[:, 0:n]
            if len(shape) == 1:
                return v
            if len(shape) == 2:
                return v.rearrange("p (a b) -> p a b", a=shape[0])
            if len(shape) == 3:
                return v.rearrange("p (a b c) -> p a b c", a=shape[0], b=shape[1])
            raise ValueError

        def pm():
            i = state["pm"] % 4
            state["pm"] += 1
            return psM_t[i], ("psM", i)

        def pm_pair():
            i = 2 * (state["pp"] % 2)
            state["pp"] += 1
            return (psM_t[i], ("psM", i)), (psM_t[i + 1], ("psM", i + 1))

        def po():
            i = state["po"] % 2
            state["po"] += 1
            return psO_t[i], ("psO", i)

        def po4():
            i = state["po4"] % 4
            state["po4"] += 1
            if i < 2:
                return psO_t[i], ("psO", i)
            return psM_t[i], ("psM", i)

        P.op("pool", lambda e: e.dma_start(out=identb[:], in_=ident_d), w=["identb"], chan=c_constsw)
        P.op("sp", lambda e: e.dma_start(out=identf[:], in_=ident_d), w=["identf"], chan=c_const)
        P.op("pool", lambda e: e.dma_start(out=trib[:], in_=tri_d), w=["trib"], chan=c_constsw)
        P.op("sp", lambda e: e.dma_start(out=gcols[:], in_=gcols_d), w=["gcols"], chan=c_const)
        P.op("sp", lambda e: e.dma_start(out=gfin[:], in_=gfin_d), w=["gfin"], chan=c_const)
        P.op("dve", lambda e: e.memset(onesb[:], 1.0), w=["onesb"])
        P.op("dve", lambda e: e.memset(onesm[:], 1.0 / 512.0), w=["onesm"])
        P.op("dve", lambda e: e.memset(eps6[:], 1e-6), w=["eps6"])
        P.op("dve", lambda e: e.memset(eps5[:], 1e-5), w=["eps5"])

        def hkey(s):
            return ("h", s)

        def rstd_of(src_ap, src_key, junk, col, eps_t, eps_key):
            P.op("act", lambda e: e.activation(out=junk, in_=src_ap, func=AF.Square,
                                               accum_out=stat[:, col:col + 1]),
                 r=[src_key], w=["junk", ("stat", col)])
            P.op("act", lambda e: e.activation(out=stat[:, col + 1:col + 2], in_=stat[:, col:col + 1],
                                               func=AF.Sqrt, scale=1.0 / D, bias=eps_t[:, 0:1]),
                 r=[("stat", col), eps_key], w=[("stat", col + 1)])
            P.op("dve", lambda e: e.reciprocal(out=stat[:, col + 2:col + 3], in_=stat[:, col + 1:col + 2]),
                 r=[("stat", col + 1)], w=[("stat", col + 2)])

        def norm_tiles(srcs, gidx, hnT, hnT_keyf, hn_bufs, junk, col0):
            n = len(srcs)
            base = 32 * (state["nrm"] % 2)
            state["nrm"] += 1
            sk = ("statg", base)
            for j in range(n):
                src, skey = srcs[j]
                P.op("act", lambda e: e.activation(out=junk, in_=src, func=AF.Square, accum_out=stat[:, base + j:base + j + 1]),
                     r=[skey], w=["junk", (sk, "ss", j)])
            P.op("act", lambda e: e.activation(out=stat[:, base + 8:base + 8 + n], in_=stat[:, base:base + n],
                                               func=AF.Sqrt, scale=1.0 / D, bias=eps6[:, 0:1]),
                 r=[(sk, "ss", j) for j in range(n)] + ["eps6"], w=[(sk, "sd")])
            P.op("dve", lambda e: e.reciprocal(out=stat[:, base + 16:base + 16 + n], in_=stat[:, base + 8:base + 8 + n]),
                 r=[(sk, "sd")], w=[(sk, "rs")])
            for j0 in range(0, n, 2):
                pair = list(range(j0, min(j0 + 2, n)))
                for j in pair:
                    src, skey = srcs[j]
                    hb, hbk = hn_bufs[j % 2]
                    P.op("act", lambda e: e.activation(out=hb, in_=src, func=AF.Identity, scale=stat[:, base + 16 + j:base + 17 + j]),
                         r=[skey, (sk, "rs")], w=[hbk])
                    for kc in range(8):
                        jj = j - j0
                        P.op("pe", lambda e: e.transpose(out=psTb[:, kc, jj * 128:(jj + 1) * 128], in_=hb[:, kc * 128:(kc + 1) * 128],
                                                         identity=identb[:]), r=[hbk, "identb"], w=[("psT", jj)])
                w = len(pair) * 128
                c0 = j0 * 128
                P.op("dve", lambda e: e.tensor_tensor(
                    out=hnT[:, :, c0:c0 + w], in0=psTb[:, :, 0:w],
                    in1=gcols[:, gidx * 8:(gidx + 1) * 8].unsqueeze(2).to_broadcast([128, 8, w]), op=ALU.mult),
                    r=[("psT", jj) for jj in range(len(pair))] + ["gcols"],
                    w=[hnT_keyf(j) for j in pair])

        def load_resident(dst, src_ap, key):
            P.op("pool", lambda e: e.dma_start(out=dst, in_=src_ap), w=[key], chan=c_phsw)

        def nat_view(w_d, kc=8):
            return w_d.rearrange("(kc p) n -> p kc n", p=128)

        def load_x(r):
            src = x_d[r * RANGE:(r + 1) * RANGE, :].rearrange("(s p) d -> p s d", p=128)
            for q in range(4):
                P.op("sp", lambda e, q=q: e.dma_start(out=h_t[:, q * 4:(q + 1) * 4, :], in_=src[:, q * 4:(q + 1) * 4, :]),
                     w=[hkey(s) for s in range(q * 4, q * 4 + 4)], chan=c_xs[q])

        def conv_mixer(r):
            reset_arena()
            wout = alloc([8, 1024], BF16)
            diagA = alloc([4, 31, 128], BF16)
            diagB = alloc([4, 3, 128], BF16)
            wc = Ring(P, "wc", [alloc([128 * 8], BF16) for _ in range(4)])
            hn_bufs = [(alloc([1024], BF16), ("hn", i)) for i in range(2)]
            junk = alloc([1024], BF16)
            hnT = alloc([8, 512], BF16)
            aT = alloc([4, HALO + 512], BF16)
            pT = alloc([4, HALO + 512], BF16)
            gbT = alloc([4, 512], BF16)
            sig = [alloc([512], F32) for _ in range(2)]
            gct = [alloc([512], F32) for _ in range(2)]
            cA = alloc([4, 512], BF16)
            sq = alloc([4, 512], BF16)
            mean_sb = alloc([512], F32)
            var_sb = alloc([512], F32)
            rstd_sb = alloc([512], F32)
            t1 = [alloc([512], F32) for _ in range(2)]
            mixT = alloc([8, 512], BF16)
            xht = alloc([1024], F32)
            cvsm = alloc([148], F32)
            OFF_AW, OFF_AB, OFF_LG, OFF_LB, OFF_BW = 0, 124, 128, 132, 136

            P.op("sp", lambda e: e.dma_start(out=cvsm, in_=cvsm_d), w=["cvsm"], chan=c_ph)
            P.op("sp", lambda e: e.dma_start(out=xht, in_=xh_d[r]), w=["xht"], chan=c_ph)
            load_resident(wout, nat_view(cvout_d), "wout")
            def build_diags():
                for c in range(4):
                    for k in range(31):
                        P.op("dve", lambda e, c=c, k=k: e.tensor_scalar(
                            out=diagA[:, c, k, :], in0=identf[:], scalar1=cvsm[:, OFF_AW + c * 31 + k:OFF_AW + c * 31 + k + 1],
                            scalar2=None, op0=ALU.mult), r=["identf", "cvsm"], w=[("diagA", c)])
                    for k in range(3):
                        P.op("dve", lambda e, c=c, k=k: e.tensor_scalar(
                            out=diagB[:, c, k, :], in0=identf[:], scalar1=cvsm[:, OFF_BW + c * 3 + k:OFF_BW + c * 3 + k + 1],
                            scalar2=None, op0=ALU.mult), r=["identf", "cvsm"], w=[("diagB", c)])

            def in_chunk(ci, ntok):
                s, view, ch, key = wc.next()
                P.op("pool", lambda e: e.dma_start(out=view, in_=cvin_d[ci]), w=[key], chan=ch)
                ps, pk = pm()
                rk = [("hnTt", j) for j in range(max(1, ntok // 128))]
                for kc in range(8):
                    P.op("pe", lambda e, kc=kc: e.matmul(ps[:, 0:ntok], lhsT=view[:, kc * 128:(kc + 1) * 128],
                                                         rhs=hnT[:, kc, 0:ntok], start=(kc == 0), stop=(kc == 7)),
                         r=[key] + rk, w=[pk])
                return ps, pk

            def in_proj(ntok, col0, with_gb):
                for c in range(4):
                    pv, pvk = in_chunk(c, ntok)
                    pg, pgk = in_chunk(4 + c, ntok)
                    sg_, sgk = sig[c % 2], ("sig", c % 2)
                    P.op("act", lambda e, pg=pg, sg_=sg_: e.activation(out=sg_[:, 0:ntok], in_=pg[:, 0:ntok], func=AF.Sigmoid),
                         r=[pgk], w=[sgk])
                    P.op("dve", lambda e, pv=pv, sg_=sg_, c=c: e.tensor_tensor(
                        out=aT[:, c, col0:col0 + ntok], in0=pv[:, 0:ntok], in1=sg_[:, 0:ntok], op=ALU.mult),
                        r=[pvk, sgk], w=[("aT", c)])
                for c in range(4):
                    pgc, pgck = in_chunk(12 + c, ntok)
                    phb, phbk = in_chunk(16 + c, ntok)
                    g_, gk = gct[c % 2], ("gct", c % 2)
                    P.op("act", lambda e, pgc=pgc, g_=g_: e.activation(out=g_[:, 0:ntok], in_=pgc[:, 0:ntok], func=AF.Identity),
                         r=[pgck], w=[gk])
                    P.op("dve", lambda e, phb=phb, g_=g_, c=c: e.tensor_tensor(
                        out=pT[:, c, col0:col0 + ntok], in0=phb[:, 0:ntok], in1=g_[:, 0:ntok], op=ALU.mult),
                        r=[phbk, gk], w=[("pT", c)])
                if with_gb:
                    for c in range(4):
                        pgb, pgbk = in_chunk(8 + c, ntok)
                        P.op("act", lambda e, pgb=pgb, c=c: e.activation(out=gbT[:, c, 0:ntok], in_=pgb[:, 0:ntok], func=AF.Identity),
                             r=[pgbk], w=[("gbT", c)])

            def norm_into(srcs):
                norm_tiles(srcs, 0, hnT, lambda j: ("hnTt", j), hn_bufs, junk, 0)

            norm_into([(xht, "xht")])
            in_proj(128, 0, False)

            def normin(g):
                srcs = [(h_t[:, g * 4 + j, :], hkey(g * 4 + j)) for j in range(4)]
                norm_into(srcs)
                in_proj(512, HALO, True)

            normin(0)
            build_diags()
            for g in range(NG):
                for c in range(4):
                    ps, pk = pm()
                    for k in range(31):
                        o = HALO - 30 + k
                        P.op("pe", lambda e, c=c, k=k, o=o, ps=ps: e.matmul(
                            ps[:, :], lhsT=diagA[:, c, k, :], rhs=aT[:, c, o:o + 512], start=(k == 0), stop=(k == 30)),
                            r=[("diagA", c), ("aT", c)], w=[pk])
                    P.op("act", lambda e, c=c, ps=ps: e.activation(out=cA[:, c, :], in_=ps[:, :], func=AF.Identity,
                                                                   bias=cvsm[:, OFF_AB + c:OFF_AB + c + 1]),
                         r=[pk, "cvsm"], w=[("cA", c)])
                    P.op("act", lambda e, c=c, ps=ps: e.activation(out=sq[:, c, :], in_=ps[:, :], func=AF.Square,
                                                                   bias=cvsm[:, OFF_AB + c:OFF_AB + c + 1]),
                         r=[pk, "cvsm"], w=[("sq", c)])
                pmean, pmk = pm()
                for c in range(4):
                    P.op("pe", lambda e, c=c: e.matmul(pmean[:, :], lhsT=onesm[:], rhs=cA[:, c, :], start=(c == 0), stop=(c == 3)),
                         r=["onesm", ("cA", c)], w=[pmk])
                pex, pexk = pm()
                for c in range(4):
                    P.op("pe", lambda e, c=c: e.matmul(pex[:, :], lhsT=onesm[:], rhs=sq[:, c, :], start=(c == 0), stop=(c == 3)),
                         r=["onesm", ("sq", c)], w=[pexk])
                P.op("act", lambda e: e.activation(out=mean_sb, in_=pmean[:, :], func=AF.Identity), r=[pmk], w=["mean_sb"])
                P.op("dve", lambda e: e.tensor_tensor(out=var_sb, in0=mean_sb, in1=mean_sb, op=ALU.mult), r=["mean_sb"], w=["var_sb"])
                P.op("dve", lambda e: e.tensor_tensor(out=var_sb, in0=pex[:, :], in1=var_sb, op=ALU.subtract), r=[pexk, "var_sb"], w=["var_sb"])
                P.op("act", lambda e: e.activation(out=var_sb, in_=var_sb, func=AF.Sqrt, bias=eps5[:, 0:1]), r=["var_sb", "eps5"], w=["var_sb"])
                P.op("dve", lambda e: e.reciprocal(out=rstd_sb, in_=var_sb), r=["var_sb"], w=["rstd_sb"])
                for c in range(4):
                    tt, tk = t1[c % 2], ("t1", c % 2)
                    P.op("dve", lambda e, c=c, tt=tt: e.tensor_tensor(out=tt, in0=cA[:, c, :], in1=mean_sb, op=ALU.subtract),
                         r=[("cA", c), "mean_sb"], w=[tk])
                    P.op("dve", lambda e, tt=tt: e.tensor_tensor(out=tt, in0=tt, in1=rstd_sb, op=ALU.mult), r=[tk, "rstd_sb"], w=[tk])
                    P.op("act", lambda e, c=c, tt=tt: e.activation(out=mixT[:, c, :], in_=tt, func=AF.Silu,
                                                                   scale=cvsm[:, OFF_LG + c:OFF_LG + c + 1],
                                                                   bias=cvsm[:, OFF_LB + c:OFF_LB + c + 1]),
                         r=[tk, "cvsm"], w=[("mixT", c)])
                for c in range(4):
                    ps, pk = pm()
                    for k in range(3):
                        o = HALO - 2 + k
                        P.op("pe", lambda e, c=c, k=k, o=o, ps=ps: e.matmul(
                            ps[:, :], lhsT=diagB[:, c, k, :], rhs=pT[:, c, o:o + 512], start=(k == 0), stop=(k == 2)),
                            r=[("diagB", c), ("pT", c)], w=[pk])
                    P.op("dve", lambda e, c=c, ps=ps: e.tensor_tensor(out=mixT[:, 4 + c, :], in0=ps[:, :], in1=gbT[:, c, :], op=ALU.mult),
                         r=[pk, ("gbT", c)], w=[("mixT", 4 + c)])
                for c in range(4):
                    P.op("dve", lambda e, c=c: e.tensor_copy(out=aT[:, c, 0:HALO], in_=aT[:, c, 512:512 + HALO]), r=[("aT", c)], w=[("aT", c)])
                    P.op("dve", lambda e, c=c: e.tensor_copy(out=pT[:, c, 0:HALO], in_=pT[:, c, 512:512 + HALO]), r=[("pT", c)], w=[("pT", c)])
                if g + 1 < NG:
                    normin(g + 1)
                for s in range(4):
                    for half in range(2):
                        pso, pok = po4()
                        for kc in range(8):
                            P.op("pe", lambda e, kc=kc, s=s, half=half, pso=pso: e.matmul(
                                pso[:, :], lhsT=mixT[:, kc, s * 128:(s + 1) * 128], rhs=wout[:, kc, half * 512:(half + 1) * 512],
                                start=(kc == 0), stop=(kc == 7)), r=[("mixT", kc), "wout"], w=[pok])
                        ti = g * 4 + s
                        P.op("dve", lambda e, ti=ti, half=half, pso=pso: e.tensor_tensor(
                            out=h_t[:, ti, half * 512:(half + 1) * 512], in0=pso[:, :], in1=h_t[:, ti, half * 512:(half + 1) * 512], op=ALU.add),
                            r=[pok, hkey(ti)], w=[hkey(ti)])
            P.barrier()

        def xattn(r, li):
            reset_arena()
            KT = alloc([8, 256], BF16)
            V = alloc([2, 1024], BF16)
            base = state["off"]
            gi_x, gi_m = (1, 2) if li == 0 else (5, 6)
            if r == 0:
                wk = alloc([8, 1024], BF16)
                wv = alloc([8, 1024], BF16)
                memx = alloc([2, 1024], F32)
                hn_bufs = [(alloc([1024], BF16), ("hn", i)) for i in range(2)]
                junk = alloc([1024], BF16)
                memT = alloc([8, 256], BF16)
                P.op("sp", lambda e: e.dma_start(out=memx, in_=mem_d.rearrange("(s p) d -> p s d", p=128)), w=["memx"], chan=c_ph)
                load_resident(wk, xk_d[li].rearrange("c p n -> p c n"), "wk")
                load_resident(wv, nat_view(xv_d[li]), "wv")
                norm_tiles([(memx[:, j, :], "memx") for j in range(2)], gi_m, memT, lambda j: "memT", hn_bufs, junk, 0)
                for oc in range(8):
                    ps, pk = pm()
                    for kc in range(8):
                        P.op("pe", lambda e, oc=oc, kc=kc, ps=ps: e.matmul(ps[:, 0:256], lhsT=wk[:, oc, kc * 128:(kc + 1) * 128],
                                                                          rhs=memT[:, kc, :], start=(kc == 0), stop=(kc == 7)),
                             r=["wk", "memT"], w=[pk])
                    P.op("act", lambda e, oc=oc, ps=ps: e.activation(out=KT[:, oc, :], in_=ps[:, 0:256], func=AF.Identity), r=[pk], w=["KT"])
                for mt in range(2):
                    for half in range(2):
                        ps, pk = pm()
                        for kc in range(8):
                            P.op("pe", lambda e, mt=mt, half=half, kc=kc, ps=ps: e.matmul(
                                ps[:, :], lhsT=memT[:, kc, mt * 128:(mt + 1) * 128], rhs=wv[:, kc, half * 512:(half + 1) * 512],
                                start=(kc == 0), stop=(kc == 7)), r=["wv", "memT"], w=[pk])
                        P.op("dve", lambda e, mt=mt, half=half, ps=ps: e.tensor_copy(out=V[:, mt, half * 512:(half + 1) * 512], in_=ps[:, :]),
                             r=[pk], w=["V"])
                P.op("sp", lambda e: e.dma_start(out=kt_d[li].rearrange("p (a b) -> p a b", a=8), in_=KT), r=["KT"], w=[("kt_d", li)], chan=c_ph)
                P.op("sp", lambda e: e.dma_start(out=v_d[li].rearrange("p (a b) -> p a b", a=2), in_=V), r=["V"], w=[("v_d", li)], chan=c_ph)
                P.barrier()
            else:
                P.op("sp", lambda e: e.dma_start(out=KT, in_=kt_d[li].rearrange("p (a b) -> p a b", a=8)), w=["KT"], chan=c_ph)
                P.op("sp", lambda e: e.dma_start(out=V, in_=v_d[li].rearrange("p (a b) -> p a b", a=2)), w=["V"], chan=c_ph)
            reset_arena(base)
            wq = alloc([8, 1024], BF16)
            wo = alloc([8, 1024], BF16)
            hn_bufs = [(alloc([1024], BF16), ("hn", i)) for i in range(2)]
            junk = alloc([1024], BF16)
            hnT2 = [alloc([8, 512], BF16) for _ in range(2)]
            qT2 = [alloc([8, 512], BF16) for _ in range(2)]
            ET = alloc([4, 2, 512], BF16)
            rden = [alloc([512], F32) for _ in range(4)]
            oT = alloc([8, 512], BF16)
            load_resident(wq, xq_d[li].rearrange("c p n -> p c n"), "wq")
            load_resident(wo, nat_view(xo_d[li]), "wo")

            def stage_a(g):
                hnT, qT, pb = hnT2[g % 2], qT2[g % 2], g % 2
                srcs = [(h_t[:, g * 4 + j, :], hkey(g * 4 + j)) for j in range(4)]
                norm_tiles(srcs, gi_x, hnT, lambda j: ("hnT", pb), hn_bufs, junk, 0)
                for oc in range(8):
                    ps, pk = pm()
                    for kc in range(8):
                        P.op("pe", lambda e: e.matmul(ps[:, :], lhsT=wq[:, oc, kc * 128:(kc + 1) * 128],
                                                      rhs=hnT[:, kc, :], start=(kc == 0), stop=(kc == 7)),
                             r=["wq", ("hnT", pb)], w=[pk])
                    P.op("act", lambda e: e.activation(out=qT[:, oc, :], in_=ps[:, :], func=AF.Identity, scale=0.0625),
                         r=[pk], w=[("qT", pb, oc)])

            def stage_b(g):
                qT, pb = qT2[g % 2], g % 2
                for hh in range(4):
                    for mc in range(2):
                        ps, pk = pm()
                        for dc in range(2):
                            P.op("pe", lambda e: e.matmul(
                                ps[:, :], lhsT=KT[:, 2 * hh + dc, mc * 128:(mc + 1) * 128], rhs=qT[:, 2 * hh + dc, :],
                                start=(dc == 0), stop=(dc == 1)), r=["KT", ("qT", pb, 2 * hh + dc)], w=[pk])
                        P.op("act", lambda e: e.activation(out=ET[:, hh, mc, :], in_=ps[:, :], func=AF.Exp),
                             r=[pk], w=[("ET", hh, mc)])
                for hh in range(4):
                    ps, pk = pm()
                    for mc in range(2):
                        P.op("pe", lambda e: e.matmul(ps[:, :], lhsT=onesb[:], rhs=ET[:, hh, mc, :],
                                                      start=(mc == 0), stop=(mc == 1)),
                             r=["onesb", ("ET", hh, mc)], w=[pk])
                    rd, rdk = rden[hh], ("rden", hh)
                    P.op("dve", lambda e: e.reciprocal(out=rd, in_=ps[:, :]), r=[pk], w=[rdk])
                for hh in range(4):
                    rd, rdk = rden[hh], ("rden", hh)
                    for dc in range(2):
                        ps2, pk2 = pm()
                        oc = 2 * hh + dc
                        for mc in range(2):
                            P.op("pe", lambda e: e.matmul(
                                ps2[:, :], lhsT=V[:, mc, oc * 128:(oc + 1) * 128], rhs=ET[:, hh, mc, :],
                                start=(mc == 0), stop=(mc == 1)), r=["V", ("ET", hh, mc)], w=[pk2])
                        P.op("dve", lambda e: e.tensor_tensor(out=oT[:, oc, :], in0=ps2[:, :], in1=rd, op=ALU.mult),
                             r=[pk2, rdk], w=[("oT", oc)])
                for s in range(4):
                    for half in range(2):
                        pso, pok = po4()
                        for kc in range(8):
                            P.op("pe", lambda e: e.matmul(
                                pso[:, :], lhsT=oT[:, kc, s * 128:(s + 1) * 128], rhs=wo[:, kc, half * 512:(half + 1) * 512],
                                start=(kc == 0), stop=(kc == 7)), r=[("oT", kc), "wo"], w=[pok])
                        ti = g * 4 + s
                        hv = h_t[:, ti, half * 512:(half + 1) * 512]
                        P.op("dve", lambda e: e.tensor_tensor(out=hv, in0=pso[:, :], in1=hv, op=ALU.add),
                             r=[pok, hkey(ti)], w=[hkey(ti)])

            stage_a(0)
            for g in range(NG):
                if g + 1 < NG:
                    stage_a(g + 1)
                stage_b(g)
            P.barrier()

        def ffn_core(hnT, h1T, gu, dr, silu_t, wg_of, wu_of, wd_of, gate_of):
            for grp in FGROUPS:
                dslots = {}
                for fi, f in enumerate(grp):
                    s, gv, gch, gkey = gu.next()
                    P.op("pool", lambda e, gv=gv, f=f: e.dma_start(out=gv[:, 0, :], in_=wg_of(f)), w=[(gkey, 0)], chan=gch)
                    P.op("pool", lambda e, gv=gv, f=f: e.dma_start(out=gv[:, 1, :], in_=wu_of(f)), w=[(gkey, 1)], chan=gu.last2)
                    s2, dv, dch, dkey = dr.next()
                    P.op("pool", lambda e, dv=dv, f=f: e.dma_start(out=dv, in_=wd_of(f)), w=[dkey], chan=dch)
                    dslots[fi] = (dv, dkey)
                    for sg in range(NG):
                        (pg, pgk), (pu, puk) = pm_pair()
                        for kc in range(8):
                            P.op("pe", lambda e, kc=kc, sg=sg, pg=pg, gv=gv: e.matmul(
                                pg[:, :], lhsT=gv[:, 0, kc * 128:(kc + 1) * 128], rhs=hnT[:, kc, sg * 512:(sg + 1) * 512],
                                start=(kc == 0), stop=(kc == 7)), r=[(gkey, 0), ("hnT", sg)], w=[pgk])
                        for kc in range(8):
                            P.op("pe", lambda e, kc=kc, sg=sg, pu=pu, gv=gv: e.matmul(
                                pu[:, :], lhsT=gv[:, 1, kc * 128:(kc + 1) * 128], rhs=hnT[:, kc, sg * 512:(sg + 1) * 512],
                                start=(kc == 0), stop=(kc == 7)), r=[(gkey, 1), ("hnT", sg)], w=[puk])
                        st_, stk = silu_t[sg % 2], ("silu", sg % 2)
                        P.op("act", lambda e, pg=pg, st_=st_: e.activation(out=st_, in_=pg[:, :], func=AF.Silu), r=[pgk], w=[stk])
                        P.op("dve", lambda e, pu=pu, st_=st_, fi=fi, sg=sg: e.tensor_tensor(
                            out=h1T[:, fi, sg * 512:(sg + 1) * 512], in0=pu[:, :], in1=st_, op=ALU.mult),
                            r=[puk, stk], w=[("h1T", fi, sg)])
                nfi = len(grp)
                for s in range(NT):
                    for half in range(2):
                        pso, pok = po()
                        for fi in range(nfi):
                            dv, dkey = dslots[fi]
                            P.op("pe", lambda e, fi=fi, s=s, half=half, pso=pso, dv=dv: e.matmul(
                                pso[:, :], lhsT=h1T[:, fi, s * 128:(s + 1) * 128], rhs=dv[:, half * 512:(half + 1) * 512],
                                start=(fi == 0), stop=(fi == nfi - 1)), r=[("h1T", fi, s // 4), dkey], w=[pok])
                        hv = h_t[:, s, half * 512:(half + 1) * 512]
                        if gate_of is None:
                            P.op("dve", lambda e, pso=pso, hv=hv: e.tensor_tensor(out=hv, in0=pso[:, :], in1=hv, op=ALU.add),
                                 r=[pok, hkey(s)], w=[hkey(s)])
                        else:
                            gap, gk = gate_of(s)
                            P.op("dve", lambda e, pso=pso, hv=hv, gap=gap: e.scalar_tensor_tensor(
                                out=hv, in0=pso[:, :], scalar=gap, in1=hv, op0=ALU.mult, op1=ALU.add),
                                r=[pok, hkey(s), gk], w=[hkey(s)])

        def ffn_alloc(r, tag):
            hnT = alloc([8, RANGE], BF16)
            h1T = alloc([6, RANGE], BF16)
            gu = Ring(P, "gu", [alloc([2, 1024], BF16) for _ in range(4)])
            dr = Ring(P, "dr", [alloc([1024], BF16) for _ in range(9)])
            silu_t = [alloc([512], BF16) for _ in range(2)]
            hn_bufs = [(alloc([1024], BF16), ("hn", i)) for i in range(2)]
            junk = alloc([1024], BF16)
            return hnT, h1T, gu, dr, silu_t, hn_bufs, junk

        def ffn_dense(r):
            reset_arena()
            hnT, h1T, gu, dr, silu_t, hn_bufs, junk = ffn_alloc(r, "f")
            for g in range(NG):
                srcs = [(h_t[:, g * 4 + j, :], hkey(g * 4 + j)) for j in range(4)]
                norm_tiles(srcs, 3, hnT[:, :, g * 512:(g + 1) * 512], lambda j, g=g: ("hnT", g), hn_bufs, junk, 0)
            ffn_core(hnT, h1T, gu, dr, silu_t,
                     lambda f: fg_d[f], lambda f: fu_d[f], lambda f: fd_d[f * 128:(f + 1) * 128, :], None)
            P.barrier()

        def gmlp(r):
            reset_arena()
            wu = alloc([8, 1024], BF16)
            wv = alloc([8, 1024], BF16)
            wout = alloc([8, 1024], BF16)
            wsT = alloc([8, 128], BF16)
            bsb = alloc([8, 128], F32)
            lng = alloc([1024], F32)
            lnb = alloc([1024], F32)
            hn_bufs = [(alloc([1024], BF16), ("hn", i)) for i in range(2)]
            junk = alloc([1024], BF16)
            hnT = alloc([8, 512], BF16)
            uT = alloc([8, 512], BF16)
            vg = [alloc([1024], F32) for _ in range(2)]
            vN = alloc([4, 1024], BF16)
            tmp = [alloc([512], F32) for _ in range(2)]
            mT = alloc([8, 512], BF16)
            bst = alloc([4, 8], F32)
            load_resident(wu, sgu_d.rearrange("c p n -> p c n"), "wu")
            load_resident(wv, nat_view(sgv_d), "wv")
            load_resident(wout, nat_view(sgout_d), "wout")
            load_resident(wsT, sgws_d.rearrange("p (g i) -> p g i", g=8), "wsT")
            P.op("sp", lambda e: e.dma_start(out=bsb, in_=sgbs_d.rearrange("p (g i) -> p g i", g=8)), w=["bsb"], chan=c_ph)
            P.op("sp", lambda e: e.dma_start(out=lng, in_=sglng_d), w=["lng"], chan=c_ph)
            P.op("sp", lambda e: e.dma_start(out=lnb, in_=sglnb_d), w=["lnb"], chan=c_ph)
            P.op("dve", lambda e: e.memset(wsT[64:128, :, 0:64], 0.0), r=[], w=["wsT"])
            bstf = bst.rearrange("p a b -> p (a b)")
            for g in range(NG):
                srcs = [(h_t[:, g * 4 + j, :], hkey(g * 4 + j)) for j in range(4)]
                norm_tiles(srcs, 4, hnT, lambda j: "hnT", hn_bufs, junk, 0)
                for s in range(4):
                    vgt, vgk = vg[s % 2], ("vg", s % 2)
                    so = (s % 2) * 16
                    for half in range(2):
                        ps, pk = pm()
                        for kc in range(8):
                            P.op("pe", lambda e, s=s, half=half, kc=kc, ps=ps: e.matmul(
                                ps[:, :], lhsT=hnT[:, kc, s * 128:(s + 1) * 128], rhs=wv[:, kc, half * 512:(half + 1) * 512],
                                start=(kc == 0), stop=(kc == 7)), r=["wv", "hnT"], w=[pk])
                        P.op("act", lambda e, half=half, ps=ps, vgt=vgt: e.activation(
                            out=vgt[:, half * 512:(half + 1) * 512], in_=ps[:, :], func=AF.Gelu_apprx_tanh), r=[pk], w=[vgk])
                        P.op("dve", lambda e, half=half, vgt=vgt, so=so: e.bn_stats(
                            out=bstf[:, so + half * 6:so + half * 6 + 6], in_=vgt[:, half * 512:(half + 1) * 512]),
                            r=[vgk], w=[("bst", s % 2)])
                    P.op("dve", lambda e, so=so: e.bn_aggr(out=bstf[:, so + 12:so + 14], in_=bstf[:, so:so + 12]),
                         r=[("bst", s % 2)], w=[("bst", s % 2)])
                    P.op("act", lambda e, so=so: e.activation(out=bstf[:, so + 14:so + 15], in_=bstf[:, so + 13:so + 14],
                                                              func=AF.Sqrt, bias=eps5[:, 0:1]),
                         r=[("bst", s % 2), "eps5"], w=[("bst", s % 2)])
                    P.op("dve", lambda e, so=so: e.reciprocal(out=bstf[:, so + 15:so + 16], in_=bstf[:, so + 14:so + 15]),
                         r=[("bst", s % 2)], w=[("bst", s % 2)])
                    P.op("dve", lambda e, so=so, vgt=vgt: e.tensor_scalar(
                        out=vgt, in0=vgt, scalar1=bstf[:, so + 12:so + 13], scalar2=bstf[:, so + 15:so + 16],
                        op0=ALU.subtract, op1=ALU.mult), r=[vgk, ("bst", s % 2)], w=[vgk])
                    P.op("pool", lambda e, vgt=vgt: e.tensor_tensor(out=vgt, in0=vgt, in1=lng, op=ALU.mult), r=[vgk, "lng"], w=[vgk])
                    P.op("pool", lambda e, vgt=vgt, s=s: e.tensor_tensor(out=vN[:, s, :], in0=vgt, in1=lnb, op=ALU.add),
                         r=[vgk, "lnb"], w=[("vN", s)])
                for oc in range(8):
                    ps, pk = pm()
                    for kc in range(8):
                        P.op("pe", lambda e, oc=oc, kc=kc, ps=ps: e.matmul(ps[:, :], lhsT=wu[:, oc, kc * 128:(kc + 1) * 128],
                                                                          rhs=hnT[:, kc, :], start=(kc == 0), stop=(kc == 7)),
                             r=["wu", "hnT"], w=[pk])
                    P.op("act", lambda e, oc=oc, ps=ps: e.activation(out=uT[:, oc, :], in_=ps[:, :], func=AF.Gelu_apprx_tanh),
                         r=[pk], w=[("uT", oc)])
                for gg in range(8):
                    ps, pk = pm()
                    for s in range(4):
                        P.op("pe", lambda e, gg=gg, s=s, ps=ps: e.matmul(
                            ps[:, s * 128:(s + 1) * 128], lhsT=vN[:, s, gg * 128:(gg + 1) * 128], rhs=wsT[:, gg, :],
                            start=True, stop=True), r=[("vN", s), "wsT"], w=[pk])
                    tt, tk = tmp[gg % 2], ("tmp", gg % 2)
                    for s in range(4):
                        P.op("dve", lambda e, gg=gg, s=s, ps=ps, tt=tt: e.tensor_tensor(
                            out=tt[:, s * 128:(s + 1) * 128], in0=ps[:, s * 128:(s + 1) * 128], in1=bsb[:, gg, :], op=ALU.add),
                            r=[pk, "bsb"], w=[tk])
                    P.op("dve", lambda e, gg=gg, tt=tt: e.tensor_tensor(out=mT[:, gg, :], in0=tt, in1=uT[:, gg, :], op=ALU.mult),
                         r=[tk, ("uT", gg)], w=[("mT", gg)])
                for s in range(4):
                    for half in range(2):
                        pso, pok = po4()
                        for kc in range(8):
                            P.op("pe", lambda e, kc=kc, s=s, half=half, pso=pso: e.matmul(
                                pso[:, :], lhsT=mT[:, kc, s * 128:(s + 1) * 128], rhs=wout[:, kc, half * 512:(half + 1) * 512],
                                start=(kc == 0), stop=(kc == 7)), r=[("mT", kc), "wout"], w=[pok])
                        ti = g * 4 + s
                        P.op("dve", lambda e, ti=ti, half=half, pso=pso: e.tensor_tensor(
                            out=h_t[:, ti, half * 512:(half + 1) * 512], in0=pso[:, :], in1=h_t[:, ti, half * 512:(half + 1) * 512], op=ALU.add),
                            r=[pok, hkey(ti)], w=[hkey(ti)])
            P.barrier()

        def moe(r):
            reset_arena()
            hnT, h1T, gu, dr, silu_t, hn_bufs, junk = ffn_alloc(r, "m")
            hn32 = [alloc([1024], F32) for _ in range(2)]
            hnT32 = [alloc([8, 128], F32) for _ in range(2)]
            wr = alloc([8, 8], F32)
            gates = alloc([NT, 8], F32)
            rt = alloc([NT, 64], F32)
            P.op("sp", lambda e: e.dma_start(out=wr, in_=mr_d.rearrange("p (k e) -> p k e", k=8)), w=["wr"], chan=c_ph)
            for s in range(NT):
                src, skey = h_t[:, s, :], hkey(s)
                col = (s % 4) * 4
                rstd_of(src, skey, junk, col, eps6, "eps6")
                hb, hbk = hn32[s % 2], ("hn32", s % 2)
                P.op("act", lambda e, src=src, hb=hb, col=col: e.activation(out=hb, in_=src, func=AF.Identity,
                                                                             scale=stat[:, col + 2:col + 3]),
                     r=[skey, ("stat", col + 2)], w=[hbk])
                for kc in range(8):
                    P.op("pe", lambda e, hb=hb, kc=kc: e.transpose(out=psTf[:, kc, :], in_=hb[:, kc * 128:(kc + 1) * 128],
                                                                  identity=identf[:]), r=[hbk, "identf"], w=["psTf"])
                ht, htk = hnT32[s % 2], ("hnT32", s % 2)
                P.op("dve", lambda e, ht=ht: e.tensor_tensor(
                    out=ht, in0=psTf, in1=gcols[:, 56:64].unsqueeze(2).to_broadcast([128, 8, 128]), op=ALU.mult),
                    r=["psTf", "gcols"], w=[htk])
                P.op("act", lambda e, ht=ht, s=s: e.activation(out=hnT[:, :, s * 128:(s + 1) * 128], in_=ht, func=AF.Identity),
                     r=[htk], w=[("hnT", s // 4)])
                ps, pk = pm()
                for kc in range(8):
                    P.op("pe", lambda e, ht=ht, kc=kc, ps=ps: e.matmul(ps[:, 0:8], lhsT=ht[:, kc, :], rhs=wr[:, kc, :],
                                                                      start=(kc == 0), stop=(kc == 7)), r=[htk, "wr"], w=[pk])
                rk = ("rt", s)
                lg = rt[:, s, 0:8]
                srt = rt[:, s, 8:16]
                nm1 = rt[:, s, 16:17]
                ex = rt[:, s, 24:32]
                sel = rt[:, s, 32:40]
                gs = rt[:, s, 40:48]
                den = rt[:, s, 48:49]
                rdn = rt[:, s, 49:50]
                P.op("dve", lambda e, lg=lg, ps=ps: e.tensor_copy(out=lg, in_=ps[:, 0:8]), r=[pk], w=[rk])
                P.op("dve", lambda e, lg=lg, srt=srt: e.max(out=srt, in_=lg), r=[rk], w=[rk])
                P.op("dve", lambda e, srt=srt, nm1=nm1: e.tensor_scalar(out=nm1, in0=srt[:, 0:1], scalar1=-1.0, scalar2=None, op0=ALU.mult),
                     r=[rk], w=[rk])
                P.op("act", lambda e, lg=lg, ex=ex, nm1=nm1: e.activation(out=ex, in_=lg, func=AF.Exp, bias=nm1), r=[rk], w=[rk])
                P.op("dve", lambda e, lg=lg, sel=sel, srt=srt: e.tensor_scalar(out=sel, in0=lg, scalar1=srt[:, 1:2], scalar2=None, op0=ALU.is_ge),
                     r=[rk], w=[rk])
                P.op("dve", lambda e, ex=ex, sel=sel, gs=gs, den=den: e.scalar_tensor_tensor(
                    out=gs, in0=ex, scalar=1.0, in1=sel, op0=ALU.mult, op1=ALU.mult, accum_out=den), r=[rk], w=[rk])
                P.op("dve", lambda e, den=den, rdn=rdn: e.reciprocal(out=rdn, in_=den), r=[rk], w=[rk])
                P.op("dve", lambda e, gs=gs, rdn=rdn, s=s: e.tensor_scalar(out=gates[:, s, :], in0=gs, scalar1=rdn, scalar2=None, op0=ALU.mult),
                     r=[rk], w=[("gates", s)])
            for ex_i in range(NE):
                ffn_core(hnT, h1T, gu, dr, silu_t,
                         lambda f, ex_i=ex_i: mg_d[ex_i * NF + f], lambda f, ex_i=ex_i: mu_d[ex_i * NF + f],
                         lambda f, ex_i=ex_i: md_d[ex_i * DFF + f * 128:ex_i * DFF + (f + 1) * 128, :],
                         lambda s, ex_i=ex_i: (gates[:, s, ex_i:ex_i + 1], ("gates", s)))
            P.barrier()


        def moe_sparse(r):
            BIG = 65536.0
            reset_arena()
            gates = alloc([NT, 8], F32)
            sel = alloc([NT, 8], F32)
            slotA_i = alloc([NT], I32)
            slotB_i = alloc([NT], I32)
            widx = alloc([16, NF], I32)
            base0 = state["off"]
            junk = alloc([1024], BF16)
            hn32 = [alloc([1024], F32) for _ in range(2)]
            hb16 = [alloc([1024], BF16) for _ in range(2)]
            hnT32 = [alloc([8, 128], F32) for _ in range(2)]
            wr = alloc([8, 8], F32)
            rt = alloc([NT, 64], F32)
            c_hn = [P.new_chan(f"hnst{i}") for i in range(2)]
            P.op("sp", lambda e: e.dma_start(out=wr, in_=mr_d.rearrange("p (k e) -> p k e", k=8)), w=["wr"], chan=c_ph)
            for s in range(NT):
                src, skey = h_t[:, s, :], hkey(s)
                col = (s % 4) * 4
                rstd_of(src, skey, junk, col, eps6, "eps6")
                hb, hbk = hn32[s % 2], ("hn32", s % 2)
                P.op("act", lambda e: e.activation(out=hb, in_=src, func=AF.Identity, scale=stat[:, col + 2:col + 3]),
                     r=[skey, ("stat", col + 2)], w=[hbk])
                h16, h16k = hb16[s % 2], ("hb16", s % 2)
                P.op("act", lambda e: e.activation(out=h16, in_=src, func=AF.Identity, scale=stat[:, col + 2:col + 3]),
                     r=[skey, ("stat", col + 2)], w=[h16k])
                P.op("sp", lambda e: e.dma_start(out=hn_d[s * 128:(s + 1) * 128, :], in_=h16), r=[h16k], w=[("hn_d", s)],
                     chan=c_hn[s % 2])
                for kc in range(8):
                    P.op("pe", lambda e: e.transpose(out=psTf[:, kc, :], in_=hb[:, kc * 128:(kc + 1) * 128], identity=identf[:]),
                         r=[hbk, "identf"], w=["psTf"])
                ht, htk = hnT32[s % 2], ("hnT32", s % 2)
                P.op("dve", lambda e: e.tensor_tensor(
                    out=ht, in0=psTf, in1=gcols[:, 56:64].unsqueeze(2).to_broadcast([128, 8, 128]), op=ALU.mult),
                    r=["psTf", "gcols"], w=[htk])
                ps, pk = pm()
                for kc in range(8):
                    P.op("pe", lambda e: e.matmul(ps[:, 0:8], lhsT=ht[:, kc, :], rhs=wr[:, kc, :], start=(kc == 0), stop=(kc == 7)),
                         r=[htk, "wr"], w=[pk])
                rk = ("rt", s)
                lg = rt[:, s, 0:8]
                srt = rt[:, s, 8:16]
                nm1 = rt[:, s, 16:17]
                ex = rt[:, s, 24:32]
                gs = rt[:, s, 40:48]
                den = rt[:, s, 48:49]
                rdn = rt[:, s, 49:50]
                selv = sel[:, s, :]
                P.op("dve", lambda e: e.tensor_copy(out=lg, in_=ps[:, 0:8]), r=[pk], w=[rk])
                P.op("dve", lambda e: e.max(out=srt, in_=lg), r=[rk], w=[rk])
                P.op("dve", lambda e: e.tensor_scalar(out=nm1, in0=srt[:, 0:1], scalar1=-1.0, scalar2=None, op0=ALU.mult), r=[rk], w=[rk])
                P.op("act", lambda e: e.activation(out=ex, in_=lg, func=AF.Exp, bias=nm1), r=[rk], w=[rk])
                P.op("dve", lambda e: e.tensor_scalar(out=selv, in0=lg, scalar1=srt[:, 1:2], scalar2=None, op0=ALU.is_ge), r=[rk], w=[rk, "sel"])
                P.op("dve", lambda e: e.scalar_tensor_tensor(out=gs, in0=ex, scalar=1.0, in1=selv, op0=ALU.mult, op1=ALU.mult, accum_out=den),
                     r=[rk, "sel"], w=[rk])
                P.op("dve", lambda e: e.reciprocal(out=rdn, in_=den), r=[rk], w=[rk])
                P.op("dve", lambda e: e.tensor_scalar(out=gates[:, s, :], in0=gs, scalar1=rdn, scalar2=None, op0=ALU.mult), r=[rk], w=["gates"])
            selb = alloc([128], BF16)
            cs = alloc([NT, 8], F32)
            pre = alloc([NT, 8], F32)
            cnt = alloc([8], F32)
            ntl = alloc([8], F32)
            endt = alloc([8], F32)
            baset = alloc([8], F32)
            slot = alloc([NT, 8], F32)
            m1 = alloc([NT, 8], F32)
            m2 = alloc([NT, 8], F32)
            isA = alloc([NT, 8], F32)
            gtmp = alloc([NT, 8], F32)
            slotA = alloc([NT], F32)
            slotB = alloc([NT], F32)
            gateA = alloc([NT], F32)
            gateB = alloc([NT], F32)
            tokf = alloc([NT], F32)
            rowsA = alloc([NT, 16], F32)
            rowsB = alloc([NT, 16], F32)
            zt = alloc([1024], I32)
            thr = alloc([16], F32)
            te = alloc([16], F32)
            pcol = alloc([1], F32)
            wbase = alloc([16], F32)
            f128 = alloc([NF], F32)
            widxf = alloc([16, NF], F32)
            self_flat = sel.rearrange("p a b -> p (a b)")
            P.op("dve", lambda e: e.tensor_copy(out=selb, in_=self_flat), r=["sel"], w=["selb"])
            prank, prk = pm()
            P.op("pe", lambda e: e.matmul(prank[:, 0:128], lhsT=trib[:], rhs=selb, start=True, stop=True), r=["trib", "selb"], w=[prk])
            pcs, pck = pm()
            P.op("pe", lambda e: e.matmul(pcs[:, 0:128], lhsT=onesb[:], rhs=selb, start=True, stop=True), r=["onesb", "selb"], w=[pck])
            P.op("dve", lambda e: e.tensor_copy(out=cs.rearrange("p a b -> p (a b)"), in_=pcs[:, 0:128]), r=[pck], w=["cs"])
            P.op("dve", lambda e: e.memset(pre[:, 0, :], 0.0), w=["pre"])
            for s in range(1, NT):
                P.op("dve", lambda e: e.tensor_tensor(out=pre[:, s, :], in0=pre[:, s - 1, :], in1=cs[:, s - 1, :], op=ALU.add),
                     r=["pre", "cs"], w=["pre"])
            P.op("dve", lambda e: e.tensor_tensor(out=cnt, in0=pre[:, NT - 1, :], in1=cs[:, NT - 1, :], op=ALU.add), r=["pre", "cs"], w=["cnt"])
            P.op("dve", lambda e: e.tensor_scalar(out=ntl, in0=cnt, scalar1=0.5, scalar2=None, op0=ALU.is_gt), r=["cnt"], w=["ntl"])
            for th in (512.5, 1024.5, 1536.5):
                P.op("dve", lambda e: e.scalar_tensor_tensor(out=ntl, in0=cnt, scalar=th, in1=ntl, op0=ALU.is_gt, op1=ALU.add),
                     r=["cnt", "ntl"], w=["ntl"])
            P.op("dve", lambda e: e.tensor_scalar(out=ntl, in0=ntl, scalar1=512.0, scalar2=None, op0=ALU.mult), r=["ntl"], w=["ntl"])
            P.op("dve", lambda e: e.tensor_copy(out=endt[:, 0:1], in_=ntl[:, 0:1]), r=["ntl"], w=["endt"])
            for ei in range(1, 8):
                P.op("dve", lambda e: e.tensor_tensor(out=endt[:, ei:ei + 1], in0=endt[:, ei - 1:ei], in1=ntl[:, ei:ei + 1], op=ALU.add),
                     r=["endt", "ntl"], w=["endt"])
            P.op("dve", lambda e: e.tensor_tensor(out=baset, in0=endt, in1=ntl, op=ALU.subtract), r=["endt", "ntl"], w=["baset"])
            P.op("dve", lambda e: e.tensor_tensor(out=slot, in0=prank[:, 0:128].rearrange("p (a b) -> p a b", a=NT), in1=pre, op=ALU.add),
                 r=[prk, "pre"], w=["slot"])
            P.op("dve", lambda e: e.tensor_tensor(out=slot, in0=slot, in1=baset.unsqueeze(1).to_broadcast([128, NT, 8]), op=ALU.add),
                 r=["slot", "baset"], w=["slot"])
            P.op("dve", lambda e: e.tensor_tensor(out=m1, in0=slot, in1=sel, op=ALU.mult), r=["slot", "sel"], w=["m1"])
            P.op("dve", lambda e: e.tensor_reduce(out=slotB, in_=m1, axis=AX.X, op=ALU.max), r=["m1"], w=["slotB"])
            P.op("dve", lambda e: e.tensor_scalar(out=m2, in0=sel, scalar1=-BIG, scalar2=BIG, op0=ALU.mult, op1=ALU.add), r=["sel"], w=["m2"])
            P.op("dve", lambda e: e.tensor_tensor(out=m2, in0=m2, in1=m1, op=ALU.add), r=["m2", "m1"], w=["m2"])
            P.op("dve", lambda e: e.tensor_reduce(out=slotA, in_=m2, axis=AX.X, op=ALU.min), r=["m2"], w=["slotA"])
            P.op("dve", lambda e: e.tensor_tensor(out=isA, in0=m2, in1=slotA.unsqueeze(2).to_broadcast([128, NT, 8]), op=ALU.is_equal),
                 r=["m2", "slotA"], w=["isA"])
            P.op("dve", lambda e: e.tensor_tensor(out=gtmp, in0=gates, in1=isA, op=ALU.mult), r=["gates", "isA"], w=["gtmp"])
            P.op("dve", lambda e: e.tensor_reduce(out=gateA, in_=gtmp, axis=AX.X, op=ALU.add), r=["gtmp"], w=["gateA"])
            P.op("dve", lambda e: e.tensor_tensor(out=isA, in0=sel, in1=isA, op=ALU.subtract), r=["sel", "isA"], w=["isA"])
            P.op("dve", lambda e: e.tensor_tensor(out=gtmp, in0=gates, in1=isA, op=ALU.mult), r=["gates", "isA"], w=["gtmp"])
            P.op("dve", lambda e: e.tensor_reduce(out=gateB, in_=gtmp, axis=AX.X, op=ALU.add), r=["gtmp"], w=["gateB"])
            P.op("dve", lambda e: e.tensor_copy(out=slotA_i, in_=slotA), r=["slotA"], w=["slotA_i"])
            P.op("dve", lambda e: e.tensor_copy(out=slotB_i, in_=slotB), r=["slotB"], w=["slotB_i"])
            P.op("pool", lambda e: e.iota(tokf, pattern=[[128, NT]], base=0, channel_multiplier=1, allow_small_or_imprecise_dtypes=True), w=["tokf"])
            P.op("pool", lambda e: e.iota(thr, pattern=[[512, 16]], base=0, channel_multiplier=0, allow_small_or_imprecise_dtypes=True), w=["thr"])
            P.op("pool", lambda e: e.iota(pcol, pattern=[[0, 1]], base=0, channel_multiplier=1, allow_small_or_imprecise_dtypes=True), w=["pcol"])
            P.op("pool", lambda e: e.iota(f128, pattern=[[128, NF]], base=0, channel_multiplier=0, allow_small_or_imprecise_dtypes=True), w=["f128"])
            P.op("pool", lambda e: e.memset(zt, 0), w=["zt"])
            for rows, gt_, nm in ((rowsA, gateA, "A"), (rowsB, gateB, "B")):
                rows_i = rows.bitcast(I32)
                P.op("dve", lambda e: e.memset(rows, 0.0), w=["rows" + nm])
                P.op("dve", lambda e: e.tensor_copy(out=rows_i[:, :, 0], in_=tokf), r=["tokf"], w=["rows" + nm])
                P.op("dve", lambda e: e.tensor_copy(out=rows[:, :, 1], in_=gt_), r=["gate" + nm], w=["rows" + nm])
            c_sc = P.new_chan("scat")
            c_z = P.new_chan("tabzero")
            P.op("pool", lambda e: e.dma_start(out=slot_tab_d.rearrange("(p a) c -> p (a c)", p=128), in_=zt), r=["zt"], w=["tabz"], chan=c_z)
            for s in range(NT):
                for rows, si, nm in ((rowsA, slotA_i, "A"), (rowsB, slotB_i, "B")):
                    rows_i = rows.bitcast(I32)
                    P.op("pool", lambda e: e.indirect_dma_start(
                        out=slot_tab_d, out_offset=bass.IndirectOffsetOnAxis(ap=si[:, s:s + 1], axis=0),
                        in_=rows_i[:, s, :], in_offset=None),
                        r=["rows" + nm, "slot" + nm + "_i", "tabz"], w=[("tab", nm, s)], chan=c_sc)
            tabkeys = [("tab", nm, s) for nm in "AB" for s in range(NT)]
            P.op("dve", lambda e: e.memset(te, 0.0), w=["te"])
            for ei in range(8):
                P.op("dve", lambda e: e.scalar_tensor_tensor(out=te, in0=thr, scalar=endt[:, ei:ei + 1], in1=te, op0=ALU.is_ge, op1=ALU.add),
                     r=["thr", "endt", "te"], w=["te"])
            P.op("dve", lambda e: e.tensor_scalar(out=te, in0=te, scalar1=7.0, scalar2=None, op0=ALU.min), r=["te"], w=["te"])
            P.op("dve", lambda e: e.tensor_scalar(out=wbase, in0=te, scalar1=float(DFF), scalar2=pcol[:, 0:1], op0=ALU.mult, op1=ALU.add),
                 r=["te", "pcol"], w=["wbase"])
            for i in range(16):
                P.op("dve", lambda e: e.tensor_scalar(out=widxf[:, i, :], in0=f128, scalar1=wbase[:, i:i + 1], scalar2=None, op0=ALU.add),
                     r=["f128", "wbase"], w=["widxf"])
            P.op("dve", lambda e: e.tensor_copy(out=widx, in_=widxf), r=["widxf"], w=["widx"])
            P.barrier()
            reset_arena(base0)
            gu = Ring(P, "gu", [alloc([2, 1024], BF16) for _ in range(6)])
            dr = Ring(P, "dr", [alloc([1024], BF16) for _ in range(10)])
            silu_t = [alloc([512], BF16) for _ in range(2)]
            NH1 = 8
            h1T = alloc([NH1, 512], BF16)
            hrows2 = [alloc([4, 1024], BF16) for _ in range(2)]
            hnTt = [alloc([8, 512], BF16) for _ in range(2)]
            acc = [alloc([4, 1024], F32) for _ in range(2)]
            tabt = [alloc([4, 16], F32) for _ in range(2)]
            c_tab = [P.new_chan(f"tabld{i}") for i in range(2)]
            c_hg = [P.new_chan(f"hgath{i}") for i in range(2)]
            c_ys = [P.new_chan(f"yst{i}") for i in range(2)]
            hnkeys = [("hn_d", s) for s in range(NT)]

            def prefetch(i):
                b = i % 2
                tb_i = tabt[b].bitcast(I32)
                P.op("sp", lambda e: e.dma_start(out=tb_i, in_=slot_tab_d[i * 512:(i + 1) * 512, :].rearrange("(j p) c -> p j c", p=128)),
                     r=tabkeys, w=[("tabt", b)], chan=c_tab[b])
                for j in range(4):
                    P.op("pool", lambda e: e.indirect_dma_start(
                        out=hrows2[b][:, j, :], out_offset=None, in_=hn_d,
                        in_offset=bass.IndirectOffsetOnAxis(ap=tb_i[:, j, 0:1], axis=0)),
                        r=[("tabt", b)] + hnkeys, w=[("hrows", b, j)], chan=c_hg[b])

            def transposes(i):
                b = i % 2
                hT = hnTt[b]
                for j0 in (0, 2):
                    for jj in range(2):
                        j = j0 + jj
                        for kc in range(8):
                            P.op("pe", lambda e: e.transpose(out=psTb[:, kc, jj * 128:(jj + 1) * 128],
                                                             in_=hrows2[b][:, j, kc * 128:(kc + 1) * 128], identity=identb[:]),
                                 r=[("hrows", b, j), "identb"], w=[("psT", jj)])
                    c0 = j0 * 128
                    P.op("dve", lambda e: e.tensor_tensor(
                        out=hT[:, :, c0:c0 + 256], in0=psTb[:, :, 0:256],
                        in1=gcols[:, 56:64].unsqueeze(2).to_broadcast([128, 8, 256]), op=ALU.mult),
                        r=[("psT", 0), ("psT", 1), "gcols"], w=[("hnTt", b)])

            prefetch(0)
            transposes(0)
            for i in range(16):
                b = i % 2
                tb = tabt[b]
                tbk = ("tabt", b)
                hT = hnTt[b]
                hTk = ("hnTt", b)
                ac = acc[b]
                ack = ("acc", b)
                if i + 1 < 16:
                    prefetch(i + 1)
                cstate = {"c": 0}
                info = {}

                def emit_gu(f):
                    cidx = cstate["c"]
                    cstate["c"] += 1
                    hs = cidx % NH1
                    _, gv, gch, gkey = gu.next()
                    gch2 = gu.last2
                    P.op("pool", lambda e: e.indirect_dma_start(
                        out=gv[:, 0, :], out_offset=None, in_=mg_flat,
                        in_offset=bass.IndirectOffsetOnAxis(ap=widx[:, i, f:f + 1], axis=0)), r=["widx"], w=[(gkey, 0)], chan=gch)
                    P.op("pool", lambda e: e.indirect_dma_start(
                        out=gv[:, 1, :], out_offset=None, in_=mu_flat,
                        in_offset=bass.IndirectOffsetOnAxis(ap=widx[:, i, f:f + 1], axis=0)), r=["widx"], w=[(gkey, 1)], chan=gch2)
                    _, dv, dch, dkey = dr.next()
                    P.op("pool", lambda e: e.indirect_dma_start(
                        out=dv, out_offset=None, in_=md_d,
                        in_offset=bass.IndirectOffsetOnAxis(ap=widx[:, i, f:f + 1], axis=0)), r=["widx"], w=[dkey], chan=dch)
                    info[f] = (hs, dv, dkey)
                    (pg, pgk), (pu, puk) = pm_pair()
                    for kc in range(8):
                        P.op("pe", lambda e: e.matmul(pg[:, :], lhsT=gv[:, 0, kc * 128:(kc + 1) * 128], rhs=hT[:, kc, :],
                                                      start=(kc == 0), stop=(kc == 7)), r=[(gkey, 0), hTk], w=[pgk])
                    for kc in range(8):
                        P.op("pe", lambda e: e.matmul(pu[:, :], lhsT=gv[:, 1, kc * 128:(kc + 1) * 128], rhs=hT[:, kc, :],
                                                      start=(kc == 0), stop=(kc == 7)), r=[(gkey, 1), hTk], w=[puk])
                    st_, stk = silu_t[cidx % 2], ("silu", cidx % 2)
                    P.op("act", lambda e: e.activation(out=st_, in_=pg[:, :], func=AF.Silu), r=[pgk], w=[stk])
                    P.op("dve", lambda e: e.tensor_tensor(out=h1T[:, hs, :], in0=pu[:, :], in1=st_, op=ALU.mult),
                         r=[puk, stk], w=[("h1T", hs)])

                def emit_down(gi, grp):
                    nfi = len(grp)
                    for j in range(4):
                        for half in range(2):
                            pso, pok = po()
                            for fi, f in enumerate(grp):
                                hs, dv, dkey = info[f]
                                P.op("pe", lambda e: e.matmul(pso[:, :], lhsT=h1T[:, hs, j * 128:(j + 1) * 128],
                                                              rhs=dv[:, half * 512:(half + 1) * 512],
                                                              start=(fi == 0), stop=(fi == nfi - 1)), r=[("h1T", hs), dkey], w=[pok])
                            av = ac[:, j, half * 512:(half + 1) * 512]
                            gap = tb[:, j, 1:2]
                            if gi == 0:
                                P.op("dve", lambda e: e.tensor_scalar(out=av, in0=pso[:, :], scalar1=gap, scalar2=None, op0=ALU.mult),
                                     r=[pok, tbk], w=[ack])
                            else:
                                P.op("dve", lambda e: e.scalar_tensor_tensor(out=av, in0=pso[:, :], scalar=gap, in1=av,
                                                                             op0=ALU.mult, op1=ALU.add), r=[pok, tbk, ack], w=[ack])

                for gi, grp in enumerate(FGROUPS):
                    for k, f in enumerate(grp):
                        if k == 0 and gi > 0:
                            continue
                        emit_gu(f)
                    if gi + 1 < len(FGROUPS):
                        emit_gu(FGROUPS[gi + 1][0])
                    if gi == 1 and i + 1 < 16:
                        transposes(i + 1)
                    emit_down(gi, grp)
                P.op("sp", lambda e: e.dma_start(out=yslot_d[i * 512:(i + 1) * 512, :].rearrange("(j p) d -> p j d", p=128), in_=ac),
                     r=[ack], w=[("yslot", i)], chan=c_ys[b])
            P.barrier()
            reset_arena(base0)
            ya = [alloc([1024], F32) for _ in range(2)]
            yb = [alloc([1024], F32) for _ in range(2)]
            c_ya = [P.new_chan(f"ya{i}") for i in range(2)]
            c_yb = [P.new_chan(f"yb{i}") for i in range(2)]
            for s in range(NT):
                b = s % 2
                for yy, si, cc, nm in ((ya[b], slotA_i, c_ya[b], "ya"), (yb[b], slotB_i, c_yb[b], "yb")):
                    P.op("pool", lambda e: e.indirect_dma_start(
                        out=yy, out_offset=None, in_=yslot_d, in_offset=bass.IndirectOffsetOnAxis(ap=si[:, s:s + 1], axis=0)),
                        r=["slotA_i", "slotB_i"], w=[(nm, b)], chan=cc)
                    P.op("dve", lambda e: e.tensor_tensor(out=h_t[:, s, :], in0=h_t[:, s, :], in1=yy, op=ALU.add),
                         r=[(nm, b), hkey(s)], w=[hkey(s)])
            P.barrier()

        def moe2_stage1(r):
            gates, sel = gates_g, sel_g
            reset_arena()
            junk = alloc([1024], BF16)
            hn32 = [alloc([1024], F32) for _ in range(2)]
            hb16 = [alloc([1024], BF16) for _ in range(2)]
            hnT32 = [alloc([8, 128], F32) for _ in range(2)]
            wr = alloc([8, 8], F32)
            rt = alloc([NT, 64], F32)
            c_hn = [P.new_chan(f"hnst{i}") for i in range(2)]
            P.op("sp", lambda e: e.dma_start(out=wr, in_=mr_d.rearrange("p (k e) -> p k e", k=8)), w=["wr"], chan=c_ph)
            for s in range(NT):
                src, skey = h_t[:, s, :], hkey(s)
                P.op("act", lambda e: e.activation(out=junk, in_=src, func=AF.Square, accum_out=stat[:, s:s + 1]),
                     r=[skey], w=["junk", ("mss", s)])
            P.op("act", lambda e: e.activation(out=stat[:, 16:32], in_=stat[:, 0:16], func=AF.Sqrt, scale=1.0 / D, bias=eps6[:, 0:1]),
                 r=[("mss", s) for s in range(NT)] + ["eps6"], w=["msd"])
            P.op("dve", lambda e: e.reciprocal(out=stat[:, 32:48], in_=stat[:, 16:32]), r=["msd"], w=["mrs"])
            for s in range(NT):
                src, skey = h_t[:, s, :], hkey(s)
                col = 30 + s
                hb, hbk = hn32[s % 2], ("hn32", s % 2)
                P.op("act", lambda e: e.activation(out=hb, in_=src, func=AF.Identity, scale=stat[:, col + 2:col + 3]),
                     r=[skey, "mrs"], w=[hbk])
                h16, h16k = hb16[s % 2], ("hb16", s % 2)
                P.op("act", lambda e: e.activation(out=h16, in_=src, func=AF.Identity, scale=stat[:, col + 2:col + 3]),
                     r=[skey, "mrs"], w=[h16k])
                P.op("sp", lambda e: e.dma_start(out=hn_d[(r * NT + s) * 128:(r * NT + s + 1) * 128, :], in_=h16), r=[h16k], w=[("hn_d", r * NT + s)],
                     chan=c_hn[s % 2])
                for kc in range(8):
                    P.op("pe", lambda e: e.transpose(out=psTf[:, kc, :], in_=hb[:, kc * 128:(kc + 1) * 128], identity=identf[:]),
                         r=[hbk, "identf"], w=["psTf"])
                ht, htk = hnT32[s % 2], ("hnT32", s % 2)
                P.op("dve", lambda e: e.tensor_tensor(
                    out=ht, in0=psTf, in1=gcols[:, 56:64].unsqueeze(2).to_broadcast([128, 8, 128]), op=ALU.mult),
                    r=["psTf", "gcols"], w=[htk])
                ps, pk = pm()
                for kc in range(8):
                    P.op("pe", lambda e: e.matmul(ps[:, 0:8], lhsT=ht[:, kc, :], rhs=wr[:, kc, :], start=(kc == 0), stop=(kc == 7)),
                         r=[htk, "wr"], w=[pk])
                rk = ("rt", s)
                lg = rt[:, s, 0:8]
                srt = rt[:, s, 8:16]
                nm1 = rt[:, s, 16:17]
                ex = rt[:, s, 24:32]
                gs = rt[:, s, 40:48]
                den = rt[:, s, 48:49]
                rdn = rt[:, s, 49:50]
                selv = sel[:, r * NT + s, :]
                P.op("dve", lambda e: e.tensor_copy(out=lg, in_=ps[:, 0:8]), r=[pk], w=[rk])
                P.op("dve", lambda e: e.max(out=srt, in_=lg), r=[rk], w=[rk])
                P.op("dve", lambda e: e.tensor_scalar(out=nm1, in0=srt[:, 0:1], scalar1=-1.0, scalar2=None, op0=ALU.mult), r=[rk], w=[rk])
                P.op("act", lambda e: e.activation(out=ex, in_=lg, func=AF.Exp, bias=nm1), r=[rk], w=[rk])
                P.op("dve", lambda e: e.tensor_scalar(out=selv, in0=lg, scalar1=srt[:, 1:2], scalar2=None, op0=ALU.is_ge), r=[rk], w=[rk, "sel"])
                P.op("dve", lambda e: e.scalar_tensor_tensor(out=gs, in0=ex, scalar=1.0, in1=selv, op0=ALU.mult, op1=ALU.mult, accum_out=den),
                     r=[rk, "sel"], w=[rk])
                P.op("dve", lambda e: e.reciprocal(out=rdn, in_=den), r=[rk], w=[rk])
                P.op("dve", lambda e: e.tensor_scalar(out=gates[:, r * NT + s, :], in0=gs, scalar1=rdn, scalar2=None, op0=ALU.mult), r=[rk], w=["gates"])
            if r < nranges - 1:
                dst = hpark_d[r * RANGE:(r + 1) * RANGE, :].rearrange("(s p) d -> p s d", p=128)
                for q in range(4):
                    P.op("sp", lambda e: e.dma_start(out=dst[:, q * 4:(q + 1) * 4, :], in_=h_t[:, q * 4:(q + 1) * 4, :]),
                         r=[hkey(s) for s in range(q * 4, q * 4 + 4)], w=[("hpark", r, q)], chan=c_xs[q])
            P.barrier()

        def moe2_rest(nr):
            BIG = 65536.0
            NTT = NT * nr
            NTL = 8 * nr + 8
            gates, sel, slotA_i, slotB_i, widx = gates_g, sel_g, slotA_ig, slotB_ig, widx_g
            reset_arena()
            base0 = state["off"]
            selb = alloc([NTT * 8], BF16)
            cs = alloc([NTT, 8], F32)
            pre = alloc([NTT, 8], F32)
            cnt = alloc([8], F32)
            ntl = alloc([8], F32)
            endt = alloc([8], F32)
            baset = alloc([8], F32)
            slot = alloc([NTT, 8], F32)
            m1 = alloc([NTT, 8], F32)
            m2 = alloc([NTT, 8], F32)
            isA = alloc([NTT, 8], F32)
            gtmp = alloc([NTT, 8], F32)
            slotA = alloc([NTT], F32)
            slotB = alloc([NTT], F32)
            gateA = alloc([NTT], F32)
            gateB = alloc([NTT], F32)
            tokf = alloc([NTT], F32)
            rowsA = alloc([NTT, 16], F32)
            rowsB = alloc([NTT, 16], F32)
            zt = alloc([NTL * 64], I32)
            thr = alloc([NTL], F32)
            te = alloc([NTL], F32)
            pcol = alloc([1], F32)
            wbase = alloc([NTL], F32)
            f128 = alloc([NF], F32)
            widxf = alloc([NTL, NF], F32)
            self_flat = sel.rearrange("p a b -> p (a b)")
            P.op("dve", lambda e: e.tensor_copy(out=selb, in_=self_flat), r=["sel"], w=["selb"])
            prank, prk = pm()
            P.op("pe", lambda e: e.matmul(prank[:, 0:NTT * 8], lhsT=trib[:], rhs=selb, start=True, stop=True), r=["trib", "selb"], w=[prk])
            pcs, pck = pm()
            P.op("pe", lambda e: e.matmul(pcs[:, 0:NTT * 8], lhsT=onesb[:], rhs=selb, start=True, stop=True), r=["onesb", "selb"], w=[pck])
            P.op("dve", lambda e: e.tensor_copy(out=cs.rearrange("p a b -> p (a b)"), in_=pcs[:, 0:NTT * 8]), r=[pck], w=["cs"])
            P.op("dve", lambda e: e.memset(pre[:, 0, :], 0.0), w=["pre"])
            for s in range(1, NTT):
                P.op("dve", lambda e: e.tensor_tensor(out=pre[:, s, :], in0=pre[:, s - 1, :], in1=cs[:, s - 1, :], op=ALU.add),
                     r=["pre", "cs"], w=["pre"])
            P.op("dve", lambda e: e.tensor_tensor(out=cnt, in0=pre[:, NTT - 1, :], in1=cs[:, NTT - 1, :], op=ALU.add), r=["pre", "cs"], w=["cnt"])
            P.op("dve", lambda e: e.tensor_scalar(out=ntl, in0=cnt, scalar1=0.5, scalar2=None, op0=ALU.is_gt), r=["cnt"], w=["ntl"])
            for th in [512.0 * k + 0.5 for k in range(1, NTT // 4)]:
                P.op("dve", lambda e: e.scalar_tensor_tensor(out=ntl, in0=cnt, scalar=th, in1=ntl, op0=ALU.is_gt, op1=ALU.add),
                     r=["cnt", "ntl"], w=["ntl"])
            P.op("dve", lambda e: e.tensor_scalar(out=ntl, in0=ntl, scalar1=512.0, scalar2=None, op0=ALU.mult), r=["ntl"], w=["ntl"])
            P.op("dve", lambda e: e.tensor_copy(out=endt[:, 0:1], in_=ntl[:, 0:1]), r=["ntl"], w=["endt"])
            for ei in range(1, 8):
                P.op("dve", lambda e: e.tensor_tensor(out=endt[:, ei:ei + 1], in0=endt[:, ei - 1:ei], in1=ntl[:, ei:ei + 1], op=ALU.add),
                     r=["endt", "ntl"], w=["endt"])
            P.op("dve", lambda e: e.tensor_tensor(out=baset, in0=endt, in1=ntl, op=ALU.subtract), r=["endt", "ntl"], w=["baset"])
            P.op("dve", lambda e: e.tensor_tensor(out=slot, in0=prank[:, 0:NTT * 8].rearrange("p (a b) -> p a b", a=NTT), in1=pre, op=ALU.add),
                 r=[prk, "pre"], w=["slot"])
            P.op("dve", lambda e: e.tensor_tensor(out=slot, in0=slot, in1=baset.unsqueeze(1).to_broadcast([128, NTT, 8]), op=ALU.add),
                 r=["slot", "baset"], w=["slot"])
            P.op("dve", lambda e: e.tensor_tensor(out=m1, in0=slot, in1=sel, op=ALU.mult), r=["slot", "sel"], w=["m1"])
            P.op("dve", lambda e: e.tensor_reduce(out=slotB, in_=m1, axis=AX.X, op=ALU.max), r=["m1"], w=["slotB"])
            P.op("dve", lambda e: e.tensor_scalar(out=m2, in0=sel, scalar1=-BIG, scalar2=BIG, op0=ALU.mult, op1=ALU.add), r=["sel"], w=["m2"])
            P.op("dve", lambda e: e.tensor_tensor(out=m2, in0=m2, in1=m1, op=ALU.add), r=["m2", "m1"], w=["m2"])
            P.op("dve", lambda e: e.tensor_reduce(out=slotA, in_=m2, axis=AX.X, op=ALU.min), r=["m2"], w=["slotA"])
            P.op("dve", lambda e: e.tensor_tensor(out=isA, in0=m2, in1=slotA.unsqueeze(2).to_broadcast([128, NTT, 8]), op=ALU.is_equal),
                 r=["m2", "slotA"], w=["isA"])
            P.op("dve", lambda e: e.tensor_tensor(out=gtmp, in0=gates, in1=isA, op=ALU.mult), r=["gates", "isA"], w=["gtmp"])
            P.op("dve", lambda e: e.tensor_reduce(out=gateA, in_=gtmp, axis=AX.X, op=ALU.add), r=["gtmp"], w=["gateA"])
            P.op("dve", lambda e: e.tensor_tensor(out=isA, in0=sel, in1=isA, op=ALU.subtract), r=["sel", "isA"], w=["isA"])
            P.op("dve", lambda e: e.tensor_tensor(out=gtmp, in0=gates, in1=isA, op=ALU.mult), r=["gates", "isA"], w=["gtmp"])
            P.op("dve", lambda e: e.tensor_reduce(out=gateB, in_=gtmp, axis=AX.X, op=ALU.add), r=["gtmp"], w=["gateB"])
            P.op("dve", lambda e: e.tensor_copy(out=slotA_i, in_=slotA), r=["slotA"], w=["slotA_i"])
            P.op("dve", lambda e: e.tensor_copy(out=slotB_i, in_=slotB), r=["slotB"], w=["slotB_i"])
            P.op("pool", lambda e: e.iota(tokf, pattern=[[128, NTT]], base=0, channel_multiplier=1, allow_small_or_imprecise_dtypes=True), w=["tokf"])
            P.op("pool", lambda e: e.iota(thr, pattern=[[512, NTL]], base=0, channel_multiplier=0, allow_small_or_imprecise_dtypes=True), w=["thr"])
            P.op("pool", lambda e: e.iota(pcol, pattern=[[0, 1]], base=0, channel_multiplier=1, allow_small_or_imprecise_dtypes=True), w=["pcol"])
            P.op("pool", lambda e: e.iota(f128, pattern=[[128, NF]], base=0, channel_multiplier=0, allow_small_or_imprecise_dtypes=True), w=["f128"])
            P.op("pool", lambda e: e.memset(zt, 0), w=["zt"])
            for rows, gt_, nm in ((rowsA, gateA, "A"), (rowsB, gateB, "B")):
                rows_i = rows.bitcast(I32)
                P.op("dve", lambda e: e.memset(rows, 0.0), w=["rows" + nm])
                P.op("dve", lambda e: e.tensor_copy(out=rows_i[:, :, 0], in_=tokf), r=["tokf"], w=["rows" + nm])
                P.op("dve", lambda e: e.tensor_copy(out=rows[:, :, 1], in_=gt_), r=["gate" + nm], w=["rows" + nm])
            c_sc = P.new_chan("scat")
            c_z = P.new_chan("tabzero")
            P.op("pool", lambda e: e.dma_start(out=slot_tab_d.rearrange("(p a) c -> p (a c)", p=128), in_=zt), r=["zt"], w=["tabz"], chan=c_z)
            for s in range(NTT):
                for rows, si, nm in ((rowsA, slotA_i, "A"), (rowsB, slotB_i, "B")):
                    rows_i = rows.bitcast(I32)
                    P.op("pool", lambda e: e.indirect_dma_start(
                        out=slot_tab_d, out_offset=bass.IndirectOffsetOnAxis(ap=si[:, s:s + 1], axis=0),
                        in_=rows_i[:, s, :], in_offset=None),
                        r=["rows" + nm, "slot" + nm + "_i", "tabz"], w=[("tab", nm, s)], chan=c_sc)
            tabkeys = [("tab", nm, s) for nm in "AB" for s in range(NTT)]
            P.op("dve", lambda e: e.memset(te, 0.0), w=["te"])
            for ei in range(8):
                P.op("dve", lambda e: e.scalar_tensor_tensor(out=te, in0=thr, scalar=endt[:, ei:ei + 1], in1=te, op0=ALU.is_ge, op1=ALU.add),
                     r=["thr", "endt", "te"], w=["te"])
            P.op("dve", lambda e: e.tensor_scalar(out=te, in0=te, scalar1=7.0, scalar2=None, op0=ALU.min), r=["te"], w=["te"])
            P.op("dve", lambda e: e.tensor_scalar(out=wbase, in0=te, scalar1=float(DFF), scalar2=pcol[:, 0:1], op0=ALU.mult, op1=ALU.add),
                 r=["te", "pcol"], w=["wbase"])
            for i in range(NTL):
                P.op("dve", lambda e: e.tensor_scalar(out=widxf[:, i, :], in0=f128, scalar1=wbase[:, i:i + 1], scalar2=None, op0=ALU.add),
                     r=["f128", "wbase"], w=["widxf"])
            P.op("dve", lambda e: e.tensor_copy(out=widx, in_=widxf), r=["widxf"], w=["widx"])
            P.barrier()
            reset_arena(base0)
            gu = Ring(P, "gu", [alloc([2, 1024], BF16) for _ in range(6)])
            dr = Ring(P, "dr", [alloc([1024], BF16) for _ in range(10)])
            silu_t = [alloc([512], BF16) for _ in range(2)]
            NH1 = 8
            h1T = alloc([NH1, 512], BF16)
            hrows2 = [alloc([4, 1024], BF16) for _ in range(2)]
            hnTt = [alloc([8, 512], BF16) for _ in range(2)]
            acc = [alloc([4, 1024], F32) for _ in range(2)]
            tabt = [alloc([4, 16], F32) for _ in range(2)]
            c_tab = [P.new_chan(f"tabld{i}") for i in range(2)]
            c_hg = [P.new_chan(f"hgath{i}") for i in range(2)]
            c_ys = [P.new_chan(f"yst{i}") for i in range(2)]
            hnkeys = [("hn_d", s) for s in range(NTT)]

            def prefetch(i):
                b = i % 2
                tb_i = tabt[b].bitcast(I32)
                P.op("sp", lambda e: e.dma_start(out=tb_i, in_=slot_tab_d[i * 512:(i + 1) * 512, :].rearrange("(j p) c -> p j c", p=128)),
                     r=tabkeys, w=[("tabt", b)], chan=c_tab[b])
                for j in range(4):
                    P.op("pool", lambda e: e.indirect_dma_start(
                        out=hrows2[b][:, j, :], out_offset=None, in_=hn_d,
                        in_offset=bass.IndirectOffsetOnAxis(ap=tb_i[:, j, 0:1], axis=0)),
                        r=[("tabt", b)] + hnkeys, w=[("hrows", b, j)], chan=c_hg[b])

            def transposes(i):
                b = i % 2
                hT = hnTt[b]
                for j0 in (0, 2):
                    for jj in range(2):
                        j = j0 + jj
                        for kc in range(8):
                            P.op("pe", lambda e: e.transpose(out=psTb[:, kc, jj * 128:(jj + 1) * 128],
                                                             in_=hrows2[b][:, j, kc * 128:(kc + 1) * 128], identity=identb[:]),
                                 r=[("hrows", b, j), "identb"], w=[("psT", jj)])
                    c0 = j0 * 128
                    P.op("dve", lambda e: e.tensor_tensor(
                        out=hT[:, :, c0:c0 + 256], in0=psTb[:, :, 0:256],
                        in1=gcols[:, 56:64].unsqueeze(2).to_broadcast([128, 8, 256]), op=ALU.mult),
                        r=[("psT", 0), ("psT", 1), "gcols"], w=[("hnTt", b)])

            prefetch(0)
            transposes(0)
            for i in range(NTL):
                b = i % 2
                tb = tabt[b]
                tbk = ("tabt", b)
                hT = hnTt[b]
                hTk = ("hnTt", b)
                ac = acc[b]
                ack = ("acc", b)
                if i + 1 < NTL:
                    prefetch(i + 1)
                cstate = {"c": 0}
                info = {}

                def emit_gu(f):
                    cidx = cstate["c"]
                    cstate["c"] += 1
                    hs = cidx % NH1
                    _, gv, gch, gkey = gu.next()
                    gch2 = gu.last2
                    P.op("pool", lambda e: e.indirect_dma_start(
                        out=gv[:, 0, :], out_offset=None, in_=mg_flat,
                        in_offset=bass.IndirectOffsetOnAxis(ap=widx[:, i, f:f + 1], axis=0)), r=["widx"], w=[(gkey, 0)], chan=gch)
                    P.op("pool", lambda e: e.indirect_dma_start(
                        out=gv[:, 1, :], out_offset=None, in_=mu_flat,
                        in_offset=bass.IndirectOffsetOnAxis(ap=widx[:, i, f:f + 1], axis=0)), r=["widx"], w=[(gkey, 1)], chan=gch2)
                    _, dv, dch, dkey = dr.next()
                    P.op("pool", lambda e: e.indirect_dma_start(
                        out=dv, out_offset=None, in_=md_d,
                        in_offset=bass.IndirectOffsetOnAxis(ap=widx[:, i, f:f + 1], axis=0)), r=["widx"], w=[dkey], chan=dch)
                    info[f] = (hs, dv, dkey)
                    (pg, pgk), (pu, puk) = pm_pair()
                    for kc in range(8):
                        P.op("pe", lambda e: e.matmul(pg[:, :], lhsT=gv[:, 0, kc * 128:(kc + 1) * 128], rhs=hT[:, kc, :],
                                                      start=(kc == 0), stop=(kc == 7)), r=[(gkey, 0), hTk], w=[pgk])
                    for kc in range(8):
                        P.op("pe", lambda e: e.matmul(pu[:, :], lhsT=gv[:, 1, kc * 128:(kc + 1) * 128], rhs=hT[:, kc, :],
                                                      start=(kc == 0), stop=(kc == 7)), r=[(gkey, 1), hTk], w=[puk])
                    st_, stk = silu_t[cidx % 2], ("silu", cidx % 2)
                    P.op("act", lambda e: e.activation(out=st_, in_=pg[:, :], func=AF.Silu), r=[pgk], w=[stk])
                    P.op("dve", lambda e: e.tensor_tensor(out=h1T[:, hs, :], in0=pu[:, :], in1=st_, op=ALU.mult),
                         r=[puk, stk], w=[("h1T", hs)])

                def emit_down(gi, grp):
                    nfi = len(grp)
                    for j in range(4):
                        for half in range(2):
                            pso, pok = po()
                            for fi, f in enumerate(grp):
                                hs, dv, dkey = info[f]
                                P.op("pe", lambda e: e.matmul(pso[:, :], lhsT=h1T[:, hs, j * 128:(j + 1) * 128],
                                                              rhs=dv[:, half * 512:(half + 1) * 512],
                                                              start=(fi == 0), stop=(fi == nfi - 1)), r=[("h1T", hs), dkey], w=[pok])
                            av = ac[:, j, half * 512:(half + 1) * 512]
                            gap = tb[:, j, 1:2]
                            if gi == 0:
                                P.op("dve", lambda e: e.tensor_scalar(out=av, in0=pso[:, :], scalar1=gap, scalar2=None, op0=ALU.mult),
                                     r=[pok, tbk], w=[ack])
                            else:
                                P.op("dve", lambda e: e.scalar_tensor_tensor(out=av, in0=pso[:, :], scalar=gap, in1=av,
                                                                             op0=ALU.mult, op1=ALU.add), r=[pok, tbk, ack], w=[ack])

                for gi, grp in enumerate(FGROUPS):
                    for k, f in enumerate(grp):
                        if k == 0 and gi > 0:
                            continue
                        emit_gu(f)
                    if gi + 1 < len(FGROUPS):
                        emit_gu(FGROUPS[gi + 1][0])
                    if gi == 1 and i + 1 < NTL:
                        transposes(i + 1)
                    emit_down(gi, grp)
                P.op("sp", lambda e: e.dma_start(out=yslot_d[i * 512:(i + 1) * 512, :].rearrange("(j p) d -> p j d", p=128), in_=ac),
                     r=[ack], w=[("yslot", i)], chan=c_ys[b])
            P.barrier()
            for r in [nr - 1] + list(range(nr - 1)):
                reset_arena(base0)
                ya = [alloc([1024], F32) for _ in range(2)]
                yb = [alloc([1024], F32) for _ in range(2)]
                c_ya = [P.new_chan(f"ya{i}") for i in range(2)]
                c_yb = [P.new_chan(f"yb{i}") for i in range(2)]
                srcp = hpark_d[r * RANGE:(r + 1) * RANGE, :].rearrange("(s p) d -> p s d", p=128)
                for q in range(4):
                    if r == nr - 1:
                        break
                    P.op("sp", lambda e: e.dma_start(out=h_t[:, q * 4:(q + 1) * 4, :], in_=srcp[:, q * 4:(q + 1) * 4, :]),
                         w=[hkey(s) for s in range(q * 4, q * 4 + 4)], chan=c_xs[q])
                for s in range(NT):
                    b = s % 2
                    S = r * NT + s
                    for yy, si, cc, nm in ((ya[b], slotA_i, c_ya[b], "ya"), (yb[b], slotB_i, c_yb[b], "yb")):
                        P.op("pool", lambda e: e.indirect_dma_start(
                            out=yy, out_offset=None, in_=yslot_d, in_offset=bass.IndirectOffsetOnAxis(ap=si[:, S:S + 1], axis=0)),
                            r=["slotA_i", "slotB_i"], w=[(nm, b)], chan=cc)
                        P.op("dve", lambda e: e.tensor_tensor(out=h_t[:, s, :], in0=h_t[:, s, :], in1=yy, op=ALU.add),
                             r=[(nm, b), hkey(s)], w=[hkey(s)])
                P.barrier()
                final_norm(r)

        def final_norm(r):
            reset_arena()
            junk = alloc([1024], BF16)
            ot = [alloc([1024], F32) for _ in range(2)]
            for s in range(NT):
                src, skey = h_t[:, s, :], hkey(s)
                col = (s % 4) * 4
                rstd_of(src, skey, junk, col, eps6, "eps6")
                o, ok = ot[s % 2], ("ot", s % 2)
                P.op("act", lambda e, src=src, o=o, col=col: e.activation(out=o, in_=src, func=AF.Identity, scale=stat[:, col + 2:col + 3]),
                     r=[skey, ("stat", col + 2)], w=[ok])
                P.op("dve", lambda e, o=o: e.tensor_tensor(out=o, in0=o, in1=gfin[:], op=ALU.mult), r=[ok, "gfin"], w=[ok])
                row = r * RANGE + s * 128
                P.op("sp", lambda e, o=o, row=row: e.dma_start(out=y_d[row:row + 128, :], in_=o), r=[ok], chan=c_outs[s % 2])
            P.barrier()

        def dump_h(r):
            dst = y_d[r * RANGE:(r + 1) * RANGE, :].rearrange("(s p) d -> p s d", p=128)
            for q in range(4):
                P.op("sp", lambda e, q=q: e.dma_start(out=dst[:, q * 4:(q + 1) * 4, :], in_=h_t[:, q * 4:(q + 1) * 4, :]),
                     r=[hkey(s) for s in range(q * 4, q * 4 + 4)], chan=c_out)
            P.barrier()

        phases = [("mix0", lambda r: conv_mixer(r)), ("xa0", lambda r: xattn(r, 0)), ("ffn0", lambda r: ffn_dense(r)),
                  ("mix1", lambda r: gmlp(r)), ("xa1", lambda r: xattn(r, 1)), ("ffn1", lambda r: (moe_sparse(r) if SPARSE else moe(r)))]
        P.barrier()
        combined = SPARSE and COMBINED and stop is None and only is None
        for r in range(nranges):
            load_x(r)
            stopped = False
            for name, fn in phases:
                if only is not None and name not in only:
                    continue
                if combined and name == "ffn1":
                    moe2_stage1(r)
                    continue
                fn(r)
                if stop == name:
                    stopped = True
                    break
            if combined:
                continue
            if stopped:
                dump_h(r)
            else:
                final_norm(r)
        if combined:
            moe2_rest(nranges)

        with nc.Block() as block:
            @block.tensor
            def _(e):
                P.replay("pe", e)

            @block.scalar
            def _(e):
                P.replay("act", e)

            @block.vector
            def _(e):
                P.replay("dve", e)

            @block.gpsimd
            def _(e):
                P.replay("pool", e)

            @block.sync
            def _(e):
                P.replay("sp", e)
    return nc


def _chunks(W):
    K, N = W.shape
    kc, ncn = K // 128, N // 128
    return np.ascontiguousarray(W.reshape(kc, 128, ncn, 128).transpose(2, 1, 0, 3)).reshape(ncn, 128, kc * 128)


def _cols(v):
    return np.ascontiguousarray(v.reshape(-1, 128).T)


def _rep(v):
    v = np.asarray(v).reshape(-1)
    return np.ascontiguousarray(np.broadcast_to(v[None, :], (128, v.shape[0])))


def prepare_inputs(inp):
    f = lambda a: np.ascontiguousarray(np.asarray(a, dtype=np.float32))
    x = f(inp["x"])
    mem = f(inp["mem"])
    shared = {}
    g = [inp["norm_mix_g"][0], inp["norm_xattn_g"][0], inp["norm_mem_g"][0], inp["norm_ffn_g"][0],
         inp["norm_mix_g"][1], inp["norm_xattn_g"][1], inp["norm_mem_g"][1], inp["norm_ffn_g"][1]]
    shared["gcols"] = np.ascontiguousarray(np.concatenate([_cols(f(v)) for v in g], axis=1))
    shared["gfin"] = _rep(f(inp["final_norm_g"]))
    shared["ident"] = np.eye(128, dtype=np.float32)
    shared["tri"] = np.triu(np.ones((128, 128), np.float32), 1)
    shared["cvin"] = _chunks(f(inp["cv_w_in"][0]))
    aw = f(inp["cv_a_conv_w"][0])
    bw = f(inp["cv_b_conv_w"][0])
    cvsm = np.zeros((128, 148), np.float32)
    cvsm[:, 0:124] = aw.reshape(31, 4, 128).transpose(2, 1, 0).reshape(128, 124)
    cvsm[:, 124:128] = _cols(f(inp["cv_a_conv_b"][0]))
    cvsm[:, 128:132] = _cols(f(inp["cv_a_ln_g"][0]))
    cvsm[:, 132:136] = _cols(f(inp["cv_a_ln_b"][0]))
    cvsm[:, 136:148] = bw.reshape(3, 4, 128).transpose(2, 1, 0).reshape(128, 12)
    shared["cvsm"] = cvsm
    shared["cvout"] = f(inp["cv_w_out"][0])
    for i in range(2):
        shared[f"xq{i}"] = _chunks(f(inp["xa_w_q"][i]))
        shared[f"xk{i}"] = _chunks(f(inp["xa_w_k"][i]))
        shared[f"xv{i}"] = f(inp["xa_w_v"][i])
        shared[f"xo{i}"] = f(inp["xa_w_o"][i])
    shared["fg"] = _chunks(f(inp["ffn_w_gate"][0]))
    shared["fu"] = _chunks(f(inp["ffn_w_up"][0]))
    shared["fd"] = f(inp["ffn_w_down"][0])
    sgin = f(inp["sg_w_in"][0])
    shared["sgu"] = _chunks(sgin[:, 0:1024])
    shared["sgv"] = np.ascontiguousarray(sgin[:, 1024:2048])
    shared["sglng"] = _rep(f(inp["sg_ln_g"][0]))
    shared["sglnb"] = _rep(f(inp["sg_ln_b"][0]))
    ws = f(inp["sg_w_s"][0])
    shared["sgws"] = np.ascontiguousarray(ws.transpose(2, 0, 1)).reshape(128, 1024)
    shared["sgbs"] = _rep(f(inp["sg_b_s"][0]))
    shared["sgout"] = f(inp["sg_w_out"][0])
    shared["mr"] = np.ascontiguousarray(f(inp["moe_w_router"][0]).reshape(8, 128, 8).transpose(1, 0, 2)).reshape(128, 64)
    mg = f(inp["moe_w_gate"][0])
    mu = f(inp["moe_w_up"][0])
    shared["mg"] = np.concatenate([_chunks(mg[e]) for e in range(NE)], axis=0)
    shared["mu"] = np.concatenate([_chunks(mu[e]) for e in range(NE)], axis=0)
    shared["md"] = f(inp["moe_w_down"][0]).reshape(NE * DFF, D)
    in_maps = []
    for c in range(NCORES):
        b, hf = c // 2, c % 2
        t0 = hf * TOK_CORE
        m = dict(shared)
        m["x"] = np.ascontiguousarray(x[b, t0:t0 + TOK_CORE])
        xh = np.zeros((2, 128, D), np.float32)
        for r in range(2):
            s = t0 + r * RANGE
            if s >= 128:
                xh[r] = x[b, s - 128:s]
        m["xh"] = xh
        m["mem"] = mem[b]
        in_maps.append(m)
    return in_maps


_NC_CACHE = {}


def kernel(**inputs):
    in_maps = prepare_inputs(inputs)
    if "nc" not in _NC_CACHE:
        _NC_CACHE["nc"] = build_program()
    nc = _NC_CACHE["nc"]
    res = run_bass_kernel_spmd(nc, in_maps, core_ids=list(range(NCORES)))
    out = np.empty((4, SEQ, D), np.float32)
    for c in range(NCORES):
        b, hf = c // 2, c % 2
        out[b, hf * TOK_CORE:(hf + 1) * TOK_CORE] = res.results[c]["y"]
    return out
```

```python
import contextlib
import types
import numpy as np
import concourse.bass as bass
import concourse.mybir as mybir
from concourse.bass_utils import run_bass_kernel_spmd

F32 = mybir.dt.float32
BF16 = mybir.dt.bfloat16
I32 = mybir.dt.int32
AX = mybir.AxisListType
AF = mybir.ActivationFunctionType
ALU = mybir.AluOpType

NCORES = 8
D = 1024
SEQ = 8192
TOK_CORE = 4096
RANGE = 2048
NT = RANGE // 128
NG = RANGE // 512
DFF = 2816
NF = DFF // 128
NE = 8
FGROUPS = [list(range(0, 6)), list(range(6, 12)), list(range(12, 17)), list(range(17, 22))]
HALO = 128
SPARSE = True
COMBINED = True
ARENA = 65536

ENGS = ("pe", "act", "dve", "pool", "sp")


def _snap(fn):
    if fn is None or fn.__closure__ is None:
        return fn
    cells = []
    for c in fn.__closure__:
        try:
            cells.append(types.CellType(c.cell_contents))
        except ValueError:
            cells.append(c)
    return types.FunctionType(fn.__code__, fn.__globals__, fn.__name__, fn.__defaults__, tuple(cells))


class Chan:
    def __init__(self, h):
        self.h = h
        self.count = 0


class Prog:
    def __init__(self, nc, stack):
        self.nc = nc
        self.stack = stack
        self.streams = {e: [] for e in ENGS}
        self.chan = {e: Chan(stack.enter_context(nc.semaphore("c_" + e))) for e in ENGS}
        self.allchans = list(self.chan.values())
        self.engchans = set(self.chan.values())
        self.seen = {e: {} for e in ENGS}
        self.lastw = {}
        self.readers = {}
        self.nsem = 0
        self.named = {}

    def new_chan(self, name):
        if name in self.named:
            return self.named[name]
        c = Chan(self.stack.enter_context(self.nc.semaphore("d_" + name)))
        self.allchans.append(c)
        self.named[name] = c
        return c

    def op(self, eng, fn, r=(), w=(), chan=None):
        need = {}

        def add(ch, val):
            if eng == "pe" and ch is self.chan["pe"]:
                return
            if ch not in self.engchans:
                val = ch.count
            if self.seen[eng].get(ch, 0) >= val:
                return
            if need.get(ch, 0) < val:
                need[ch] = val

        for k in r:
            t = self.lastw.get(k)
            if t:
                add(*t)
        for k in w:
            t = self.lastw.get(k)
            if t:
                add(*t)
            for ch, val in self.readers.get(k, {}).items():
                add(ch, val)
        for ch, val in need.items():
            self.seen[eng][ch] = val
        if chan is None:
            ch = self.chan[eng]
            ch.count += 1
            inc = 1
        else:
            ch = chan
            ch.count += 16
            inc = 16
        tok = (ch, ch.count)
        self.streams[eng].append((list(need.items()), _snap(fn), ch.h, inc))
        for k in r:
            d = self.readers.setdefault(k, {})
            d[ch] = ch.count
        for k in w:
            self.lastw[k] = tok
            self.readers[k] = {}
        return tok

    def barrier(self):
        for e in ENGS:
            waits = []
            for ch in self.allchans:
                if ch.count > 0 and self.seen[e].get(ch, 0) < ch.count:
                    if e == "pe" and ch is self.chan["pe"]:
                        continue
                    waits.append((ch, ch.count))
                    self.seen[e][ch] = ch.count
            if waits:
                self.streams[e].append((waits, None, None, 0))
        self.lastw = {}
        self.readers = {}

    def replay(self, eng, e):
        for waits, fn, semh, inc in self.streams[eng]:
            for ch, val in waits:
                e.wait_ge(ch.h, val)
            if fn is not None:
                fn(e).then_inc(semh, inc)


class Ring:
    def __init__(self, P, name, views):
        self.views = views
        self.n = len(views)
        self.chans = [P.new_chan(f"{name}{i}") for i in range(self.n)]
        self.chans2 = [P.new_chan(f"{name}b{i}") for i in range(self.n)]
        self.i = 0
        self.name = name

    def next(self):
        s = self.i % self.n
        self.i += 1
        self.last2 = self.chans2[s]
        return s, self.views[s], self.chans[s], (self.name, s)


def build_program(stop=None, nranges=2, only=None):
    nc = bass.Bass("TRN2", target_bir_lowering=False)

    def din(name, shape):
        return nc.dram_tensor(name, list(shape), F32, kind="ExternalInput").ap()

    x_d = din("x", [TOK_CORE, D])
    xh_d = din("xh", [2, 128, D])
    mem_d = din("mem", [256, D])
    gcols_d = din("gcols", [128, 64])
    gfin_d = din("gfin", [128, D])
    ident_d = din("ident", [128, 128])
    tri_d = din("tri", [128, 128])
    cvin_d = din("cvin", [20, 128, 1024])
    cvsm_d = din("cvsm", [128, 148])
    cvout_d = din("cvout", [D, D])
    xq_d = [din(f"xq{i}", [8, 128, 1024]) for i in range(2)]
    xk_d = [din(f"xk{i}", [8, 128, 1024]) for i in range(2)]
    xv_d = [din(f"xv{i}", [D, D]) for i in range(2)]
    xo_d = [din(f"xo{i}", [D, D]) for i in range(2)]
    fg_d = din("fg", [NF, 128, 1024])
    fu_d = din("fu", [NF, 128, 1024])
    fd_d = din("fd", [DFF, D])
    sgu_d = din("sgu", [8, 128, 1024])
    sgv_d = din("sgv", [D, D])
    sglng_d = din("sglng", [128, D])
    sglnb_d = din("sglnb", [128, D])
    sgws_d = din("sgws", [128, 1024])
    sgbs_d = din("sgbs", [128, 1024])
    sgout_d = din("sgout", [D, D])
    mr_d = din("mr", [128, 64])
    mg_d = din("mg", [NE * NF, 128, 1024])
    mu_d = din("mu", [NE * NF, 128, 1024])
    md_d = din("md", [NE * DFF, D])
    y_d = nc.dram_tensor("y", [TOK_CORE, D], F32, kind="ExternalOutput").ap()
    NSLOT = 12288
    kt_d = [nc.dram_tensor(f"kt_scr{i}", [128, 2048], BF16, kind="Internal").ap() for i in range(2)]
    v_d = [nc.dram_tensor(f"v_scr{i}", [128, 2048], BF16, kind="Internal").ap() for i in range(2)]
    hn_d = nc.dram_tensor("hn_scr", [TOK_CORE, D], BF16, kind="Internal").ap()
    hpark_d = nc.dram_tensor("hpark", [TOK_CORE, D], F32, kind="Internal").ap()
    slot_tab_d = nc.dram_tensor("slot_tab", [NSLOT, 16], I32, kind="Internal").ap()
    yslot_d = nc.dram_tensor("yslot", [NSLOT, D], F32, kind="Internal").ap()
    mg_flat = mg_d.rearrange("c p n -> (c p) n")
    mu_flat = mu_d.rearrange("c p n -> (c p) n")

    stack = contextlib.ExitStack()
    with stack:
        def sb(name, shape, dt):
            return stack.enter_context(nc.sbuf_tensor(name, list(shape), dt))

        h_t = sb("h", [128, NT, D], F32)
        arena = sb("arena", [128, ARENA], BF16)
        identb = sb("identb", [128, 128], BF16)
        identf = sb("identf", [128, 128], F32)
        onesb = sb("onesb", [128, 128], BF16)
        onesm = sb("onesm", [128, 128], BF16)
        trib = sb("trib", [128, 128], BF16)
        gcols = sb("gcols_s", [128, 64], F32)
        gfin = sb("gfin_s", [128, D], F32)
        eps6 = sb("eps6", [128, 1], F32)
        eps5 = sb("eps5", [128, 1], F32)
        stat = sb("stat", [128, 64], F32)
        gates_g = sb("gates_g", [128, 2 * NT, 8], F32)[:, :, :]
        sel_g = sb("sel_g", [128, 2 * NT, 8], F32)[:, :, :]
        slotA_ig = sb("slotA_ig", [128, 2 * NT], I32)[:, :]
        slotB_ig = sb("slotB_ig", [128, 2 * NT], I32)[:, :]
        widx_g = sb("widx_g", [128, 24, NF], I32)[:, :, :]
        psT_t = stack.enter_context(nc.psum_tensor("psT", [128, 1024], F32))
        psM_t = [stack.enter_context(nc.psum_tensor(f"psM{i}", [128, 512], F32)) for i in range(4)]
        psO_t = [stack.enter_context(nc.psum_tensor(f"psO{i}", [128, 512], F32)) for i in range(2)]

        P = Prog(nc, stack)
        c_const = P.new_chan("const")
        c_constsw = P.new_chan("constsw")
        c_xs = [P.new_chan(f"x{q}") for q in range(4)]
        c_outs = [P.new_chan(f"out{q}") for q in range(2)]
        c_out = c_outs[0]
        c_ph = P.new_chan("ph")
        c_phsw = P.new_chan("phsw")

        psTb = psT_t[:, :].bitcast(BF16).rearrange("p (a b) -> p a b", a=8)
        psTf = psT_t[:, :].rearrange("p (a b) -> p a b", a=8)

        state = {"off": 0, "pm": 0, "po": 0, "pp": 0, "nrm": 0, "po4": 0}

        def reset_arena(off=0):
            state["off"] = off

        def alloc(shape, dt):
            n = int(np.prod(shape))
            wide = dt in (F32, I32)
            ne = n * (2 if wide else 1)
            ne = (ne + 31) // 32 * 32
            off = state["off"]
            assert off + ne <= ARENA, f"arena overflow {off}+{ne}"
            state["off"] = off + ne
            v = arena[:, off:off + ne]
            if wide:
                v = v.bitcast(dt)
            v = v[:, 0:n]
            if len(shape) == 1:
                return v
            if len(shape) == 2:
                return v.rearrange("p (a b) -> p a b", a=shape[0])
            if len(shape) == 3:
                return v.rearrange("p (a b c) -> p a b c", a=shape[0], b=shape[1])
            raise ValueError

        def pm():
            i = state["pm"] % 4
            state["pm"] += 1
            return psM_t[i], ("psM", i)

        def pm_pair():
            i = 2 * (state["pp"] % 2)
            state["pp"] += 1
            return (psM_t[i], ("psM", i)), (psM_t[i + 1], ("psM", i + 1))

        def po():
            i = state["po"] % 2
            state["po"] += 1
            return psO_t[i], ("psO", i)

        def po4():
            i = state["po4"] % 4
            state["po4"] += 1
            if i < 2:
                return psO_t[i], ("psO", i)
            return psM_t[i], ("psM", i)

        P.op("pool", lambda e: e.dma_start(out=identb[:], in_=ident_d), w=["identb"], chan=c_constsw)
        P.op("sp", lambda e: e.dma_start(out=identf[:], in_=ident_d), w=["identf"], chan=c_const)
        P.op("pool", lambda e: e.dma_start(out=trib[:], in_=tri_d), w=["trib"], chan=c_constsw)
        P.op("sp", lambda e: e.dma_start(out=gcols[:], in_=gcols_d), w=["gcols"], chan=c_const)
        P.op("sp", lambda e: e.dma_start(out=gfin[:], in_=gfin_d), w=["gfin"], chan=c_const)
        P.op("dve", lambda e: e.memset(onesb[:], 1.0), w=["onesb"])
        P.op("dve", lambda e: e.memset(onesm[:], 1.0 / 512.0), w=["onesm"])
        P.op("dve", lambda e: e.memset(eps6[:], 1e-6), w=["eps6"])
        P.op("dve", lambda e: e.memset(eps5[:], 1e-5), w=["eps5"])

        def hkey(s):
            return ("h", s)

        def rstd_of(src_ap, src_key, junk, col, eps_t, eps_key):
            P.op("act", lambda e: e.activation(out=junk, in_=src_ap, func=AF.Square,
                                               accum_out=stat[:, col:col + 1]),
                 r=[src_key], w=["junk", ("stat", col)])
            P.op("act", lambda e: e.activation(out=stat[:, col + 1:col + 2], in_=stat[:, col:col + 1],
                                               func=AF.Sqrt, scale=1.0 / D, bias=eps_t[:, 0:1]),
                 r=[("stat", col), eps_key], w=[("stat", col + 1)])
            P.op("dve", lambda e: e.reciprocal(out=stat[:, col + 2:col + 3], in_=stat[:, col + 1:col + 2]),
                 r=[("stat", col + 1)], w=[("stat", col + 2)])

        def norm_tiles(srcs, gidx, hnT, hnT_keyf, hn_bufs, junk, col0):
            n = len(srcs)
            base = 32 * (state["nrm"] % 2)
            state["nrm"] += 1
            sk = ("statg", base)
            for j in range(n):
                src, skey = srcs[j]
                P.op("act", lambda e: e.activation(out=junk, in_=src, func=AF.Square, accum_out=stat[:, base + j:base + j + 1]),
                     r=[skey], w=["junk", (sk, "ss", j)])
            P.op("act", lambda e: e.activation(out=stat[:, base + 8:base + 8 + n], in_=stat[:, base:base + n],
                                               func=AF.Sqrt, scale=1.0 / D, bias=eps6[:, 0:1]),
                 r=[(sk, "ss", j) for j in range(n)] + ["eps6"], w=[(sk, "sd")])
            P.op("dve", lambda e: e.reciprocal(out=stat[:, base + 16:base + 16 + n], in_=stat[:, base + 8:base + 8 + n]),
                 r=[(sk, "sd")], w=[(sk, "rs")])
            for j0 in range(0, n, 2):
                pair = list(range(j0, min(j0 + 2, n)))
                for j in pair:
                    src, skey = srcs[j]
                    hb, hbk = hn_bufs[j % 2]
                    P.op("act", lambda e: e.activation(out=hb, in_=src, func=AF.Identity, scale=stat[:, base + 16 + j:base + 17 + j]),
                         r=[skey, (sk, "rs")], w=[hbk])
                    for kc in range(8):
                        jj = j - j0
                        P.op("pe", lambda e: e.transpose(out=psTb[:, kc, jj * 128:(jj + 1) * 128], in_=hb[:, kc * 128:(kc + 1) * 128],
                                                         identity=identb[:]), r=[hbk, "identb"], w=[("psT", jj)])
                w = len(pair) * 128
                c0 = j0 * 128
                P.op("dve", lambda e: e.tensor_tensor(
                    out=hnT[:, :, c0:c0 + w], in0=psTb[:, :, 0:w],
                    in1=gcols[:, gidx * 8:(gidx + 1) * 8].unsqueeze(2).to_broadcast([128, 8, w]), op=ALU.mult),
                    r=[("psT", jj) for jj in range(len(pair))] + ["gcols"],
                    w=[hnT_keyf(j) for j in pair])

        def load_resident(dst, src_ap, key):
            P.op("pool", lambda e: e.dma_start(out=dst, in_=src_ap), w=[key], chan=c_phsw)

        def nat_view(w_d, kc=8):
            return w_d.rearrange("(kc p) n -> p kc n", p=128)

        def load_x(r):
            src = x_d[r * RANGE:(r + 1) * RANGE, :].rearrange("(s p) d -> p s d", p=128)
            for q in range(4):
                P.op("sp", lambda e, q=q: e.dma_start(out=h_t[:, q * 4:(q + 1) * 4, :], in_=src[:, q * 4:(q + 1) * 4, :]),
                     w=[hkey(s) for s in range(q * 4, q * 4 + 4)], chan=c_xs[q])

        def conv_mixer(r):
            reset_arena()
            wout = alloc([8, 1024], BF16)
            diagA = alloc([4, 31, 128], BF16)
            diagB = alloc([4, 3, 128], BF16)
            wc = Ring(P, "wc", [alloc([128 * 8], BF16) for _ in range(4)])
            hn_bufs = [(alloc([1024], BF16), ("hn", i)) for i in range(2)]
            junk = alloc([1024], BF16)
            hnT = alloc([8, 512], BF16)
            aT = alloc([4, HALO + 512], BF16)
            pT = alloc([4, HALO + 512], BF16)
            gbT = alloc([4, 512], BF16)
            sig = [alloc([512], F32) for _ in range(2)]
            gct = [alloc([512], F32) for _ in range(2)]
            cA = alloc([4, 512], BF16)
            sq = alloc([4, 512], BF16)
            mean_sb = alloc([512], F32)
            var_sb = alloc([512], F32)
            rstd_sb = alloc([512], F32)
            t1 = [alloc([512], F32) for _ in range(2)]
            mixT = alloc([8, 512], BF16)
            xht = alloc([1024], F32)
            cvsm = alloc([148], F32)
            OFF_AW, OFF_AB, OFF_LG, OFF_LB, OFF_BW = 0, 124, 128, 132, 136

            P.op("sp", lambda e: e.dma_start(out=cvsm, in_=cvsm_d), w=["cvsm"], chan=c_ph)
            P.op("sp", lambda e: e.dma_start(out=xht, in_=xh_d[r]), w=["xht"], chan=c_ph)
            load_resident(wout, nat_view(cvout_d), "wout")
            def build_diags():
                for c in range(4):
                    for k in range(31):
                        P.op("dve", lambda e, c=c, k=k: e.tensor_scalar(
                            out=diagA[:, c, k, :], in0=identf[:], scalar1=cvsm[:, OFF_AW + c * 31 + k:OFF_AW + c * 31 + k + 1],
                            scalar2=None, op0=ALU.mult), r=["identf", "cvsm"], w=[("diagA", c)])
                    for k in range(3):
                        P.op("dve", lambda e, c=c, k=k: e.tensor_scalar(
                            out=diagB[:, c, k, :], in0=identf[:], scalar1=cvsm[:, OFF_BW + c * 3 + k:OFF_BW + c * 3 + k + 1],
                            scalar2=None, op0=ALU.mult), r=["identf", "cvsm"], w=[("diagB", c)])

            def in_chunk(ci, ntok):
                s, view, ch, key = wc.next()
                P.op("pool", lambda e: e.dma_start(out=view, in_=cvin_d[ci]), w=[key], chan=ch)
                ps, pk = pm()
                rk = [("hnTt", j) for j in range(max(1, ntok // 128))]
                for kc in range(8):
                    P.op("pe", lambda e, kc=kc: e.matmul(ps[:, 0:ntok], lhsT=view[:, kc * 128:(kc + 1) * 128],
                                                         rhs=hnT[:, kc, 0:ntok], start=(kc == 0), stop=(kc == 7)),
                         r=[key] + rk, w=[pk])
                return ps, pk

            def in_proj(ntok, col0, with_gb):
                for c in range(4):
                    pv, pvk = in_chunk(c, ntok)
                    pg, pgk = in_chunk(4 + c, ntok)
                    sg_, sgk = sig[c % 2], ("sig", c % 2)
                    P.op("act", lambda e, pg=pg, sg_=sg_: e.activation(out=sg_[:, 0:ntok], in_=pg[:, 0:ntok], func=AF.Sigmoid),
                         r=[pgk], w=[sgk])
                    P.op("dve", lambda e, pv=pv, sg_=sg_, c=c: e.tensor_tensor(
                        out=aT[:, c, col0:col0 + ntok], in0=pv[:, 0:ntok], in1=sg_[:, 0:ntok], op=ALU.mult),
                        r=[pvk, sgk], w=[("aT", c)])
                for c in range(4):
                    pgc, pgck = in_chunk(12 + c, ntok)
                    phb, phbk = in_chunk(16 + c, ntok)
                    g_, gk = gct[c % 2], ("gct", c % 2)
                    P.op("act", lambda e, pgc=pgc, g_=g_: e.activation(out=g_[:, 0:ntok], in_=pgc[:, 0:ntok], func=AF.Identity),
                         r=[pgck], w=[gk])
                    P.op("dve", lambda e, phb=phb, g_=g_, c=c: e.tensor_tensor(
                        out=pT[:, c, col0:col0 + ntok], in0=phb[:, 0:ntok], in1=g_[:, 0:ntok], op=ALU.mult),
                        r=[phbk, gk], w=[("pT", c)])
                if with_gb:
                    for c in range(4):
                        pgb, pgbk = in_chunk(8 + c, ntok)
                        P.op("act", lambda e, pgb=pgb, c=c: e.activation(out=gbT[:, c, 0:ntok], in_=pgb[:, 0:ntok], func=AF.Identity),
                             r=[pgbk], w=[("gbT", c)])

            def norm_into(srcs):
                norm_tiles(srcs, 0, hnT, lambda j: ("hnTt", j), hn_bufs, junk, 0)

            norm_into([(xht, "xht")])
            in_proj(128, 0, False)

            def normin(g):
                srcs = [(h_t[:, g * 4 + j, :], hkey(g * 4 + j)) for j in range(4)]
                norm_into(srcs)
                in_proj(512, HALO, True)

            normin(0)
            build_diags()
            for g in range(NG):
                for c in range(4):
                    ps, pk = pm()
                    for k in range(31):
                        o = HALO - 30 + k
                        P.op("pe", lambda e, c=c, k=k, o=o, ps=ps: e.matmul(
                            ps[:, :], lhsT=diagA[:, c, k, :], rhs=aT[:, c, o:o + 512], start=(k == 0), stop=(k == 30)),
                            r=[("diagA", c), ("aT", c)], w=[pk])
                    P.op("act", lambda e, c=c, ps=ps: e.activation(out=cA[:, c, :], in_=ps[:, :], func=AF.Identity,
                                                                   bias=cvsm[:, OFF_AB + c:OFF_AB + c + 1]),
                         r=[pk, "cvsm"], w=[("cA", c)])
                    P.op("act", lambda e, c=c, ps=ps: e.activation(out=sq[:, c, :], in_=ps[:, :], func=AF.Square,
                                                                   bias=cvsm[:, OFF_AB + c:OFF_AB + c + 1]),
                         r=[pk, "cvsm"], w=[("sq", c)])
                pmean, pmk = pm()
                for c in range(4):
                    P.op("pe", lambda e, c=c: e.matmul(pmean[:, :], lhsT=onesm[:], rhs=cA[:, c, :], start=(c == 0), stop=(c == 3)),
                         r=["onesm", ("cA", c)], w=[pmk])
                pex, pexk = pm()
                for c in range(4):
                    P.op("pe", lambda e, c=c: e.matmul(pex[:, :], lhsT=onesm[:], rhs=sq[:, c, :], start=(c == 0), stop=(c == 3)),
                         r=["onesm", ("sq", c)], w=[pexk])
                P.op("act", lambda e: e.activation(out=mean_sb, in_=pmean[:, :], func=AF.Identity), r=[pmk], w=["mean_sb"])
                P.op("dve", lambda e: e.tensor_tensor(out=var_sb, in0=mean_sb, in1=mean_sb, op=ALU.mult), r=["mean_sb"], w=["var_sb"])
                P.op("dve", lambda e: e.tensor_tensor(out=var_sb, in0=pex[:, :], in1=var_sb, op=ALU.subtract), r=[pexk, "var_sb"], w=["var_sb"])
                P.op("act", lambda e: e.activation(out=var_sb, in_=var_sb, func=AF.Sqrt, bias=eps5[:, 0:1]), r=["var_sb", "eps5"], w=["var_sb"])
                P.op("dve", lambda e: e.reciprocal(out=rstd_sb, in_=var_sb), r=["var_sb"], w=["rstd_sb"])
                for c in range(4):
                    tt, tk = t1[c % 2], ("t1", c % 2)
                    P.op("dve", lambda e, c=c, tt=tt: e.tensor_tensor(out=tt, in0=cA[:, c, :], in1=mean_sb, op=ALU.subtract),
                         r=[("cA", c), "mean_sb"], w=[tk])
                    P.op("dve", lambda e, tt=tt: e.tensor_tensor(out=tt, in0=tt, in1=rstd_sb, op=ALU.mult), r=[tk, "rstd_sb"], w=[tk])
                    P.op("act", lambda e, c=c, tt=tt: e.activation(out=mixT[:, c, :], in_=tt, func=AF.Silu,
                                                                   scale=cvsm[:, OFF_LG + c:OFF_LG + c + 1],
                                                                   bias=cvsm[:, OFF_LB + c:OFF_LB + c + 1]),
                         r=[tk, "cvsm"], w=[("mixT", c)])
                for c in range(4):
                    ps, pk = pm()
                    for k in range(3):
                        o = HALO - 2 + k
                        P.op("pe", lambda e, c=c, k=k, o=o, ps=ps: e.matmul(
                            ps[:, :], lhsT=diagB[:, c, k, :], rhs=pT[:, c, o:o + 512], start=(k == 0), stop=(k == 2)),
                            r=[("diagB", c), ("pT", c)], w=[pk])
                    P.op("dve", lambda e, c=c, ps=ps: e.tensor_tensor(out=mixT[:, 4 + c, :], in0=ps[:, :], in1=gbT[:, c, :], op=ALU.mult),
                         r=[pk, ("gbT", c)], w=[("mixT", 4 + c)])
                for c in range(4):
                    P.op("dve", lambda e, c=c: e.tensor_copy(out=aT[:, c, 0:HALO], in_=aT[:, c, 512:512 + HALO]), r=[("aT", c)], w=[("aT", c)])
                    P.op("dve", lambda e, c=c: e.tensor_copy(out=pT[:, c, 0:HALO], in_=pT[:, c, 512:512 + HALO]), r=[("pT", c)], w=[("pT", c)])
                if g + 1 < NG:
                    normin(g + 1)
                for s in range(4):
                    for half in range(2):
                        pso, pok = po4()
                        for kc in range(8):
                            P.op("pe", lambda e, kc=kc, s=s, half=half, pso=pso: e.matmul(
                                pso[:, :], lhsT=mixT[:, kc, s * 128:(s + 1) * 128], rhs=wout[:, kc, half * 512:(half + 1) * 512],
                                start=(kc == 0), stop=(kc == 7)), r=[("mixT", kc), "wout"], w=[pok])
                        ti = g * 4 + s
                        P.op("dve", lambda e, ti=ti, half=half, pso=pso: e.tensor_tensor(
                            out=h_t[:, ti, half * 512:(half + 1) * 512], in0=pso[:, :], in1=h_t[:, ti, half * 512:(half + 1) * 512], op=ALU.add),
                            r=[pok, hkey(ti)], w=[hkey(ti)])
            P.barrier()

        def xattn(r, li):
            reset_arena()
            KT = alloc([8, 256], BF16)
            V = alloc([2, 1024], BF16)
            base = state["off"]
            gi_x, gi_m = (1, 2) if li == 0 else (5, 6)
            if r == 0:
                wk = alloc([8, 1024], BF16)
                wv = alloc([8, 1024], BF16)
                memx = alloc([2, 1024], F32)
                hn_bufs = [(alloc([1024], BF16), ("hn", i)) for i in range(2)]
                junk = alloc([1024], BF16)
                memT = alloc([8, 256], BF16)
                P.op("sp", lambda e: e.dma_start(out=memx, in_=mem_d.rearrange("(s p) d -> p s d", p=128)), w=["memx"], chan=c_ph)
                load_resident(wk, xk_d[li].rearrange("c p n -> p c n"), "wk")
                load_resident(wv, nat_view(xv_d[li]), "wv")
                norm_tiles([(memx[:, j, :], "memx") for j in range(2)], gi_m, memT, lambda j: "memT", hn_bufs, junk, 0)
                for oc in range(8):
                    ps, pk = pm()
                    for kc in range(8):
                        P.op("pe", lambda e, oc=oc, kc=kc, ps=ps: e.matmul(ps[:, 0:256], lhsT=wk[:, oc, kc * 128:(kc + 1) * 128],
                                                                          rhs=memT[:, kc, :], start=(kc == 0), stop=(kc == 7)),
                             r=["wk", "memT"], w=[pk])
                    P.op("act", lambda e, oc=oc, ps=ps: e.activation(out=KT[:, oc, :], in_=ps[:, 0:256], func=AF.Identity), r=[pk], w=["KT"])
                for mt in range(2):
                    for half in range(2):
                        ps, pk = pm()
                        for kc in range(8):
                            P.op("pe", lambda e, mt=mt, half=half, kc=kc, ps=ps: e.matmul(
                                ps[:, :], lhsT=memT[:, kc, mt * 128:(mt + 1) * 128], rhs=wv[:, kc, half * 512:(half + 1) * 512],
                                start=(kc == 0), stop=(kc == 7)), r=["wv", "memT"], w=[pk])
                        P.op("dve", lambda e, mt=mt, half=half, ps=ps: e.tensor_copy(out=V[:, mt, half * 512:(half + 1) * 512], in_=ps[:, :]),
                             r=[pk], w=["V"])
                P.op("sp", lambda e: e.dma_start(out=kt_d[li].rearrange("p (a b) -> p a b", a=8), in_=KT), r=["KT"], w=[("kt_d", li)], chan=c_ph)
                P.op("sp", lambda e: e.dma_start(out=v_d[li].rearrange("p (a b) -> p a b", a=2), in_=V), r=["V"], w=[("v_d", li)], chan=c_ph)
                P.barrier()
            else:
                P.op("sp", lambda e: e.dma_start(out=KT, in_=kt_d[li].rearrange("p (a b) -> p a b", a=8)), w=["KT"], chan=c_ph)
                P.op("sp", lambda e: e.dma_start(out=V, in_=v_d[li].rearrange("p (a b) -> p a b", a=2)), w=["V"], chan=c_ph)
            reset_arena(base)
            wq = alloc([8, 1024], BF16)
            wo = alloc([8, 1024], BF16)
            hn_bufs = [(alloc([1024], BF16), ("hn", i)) for i in range(2)]
            junk = alloc([1024], BF16)
            hnT2 = [alloc([8, 512], BF16) for _ in range(2)]
            qT2 = [alloc([8, 512], BF16) for _ in range(2)]
            ET = alloc([4, 2, 512], BF16)
            rden = [alloc([512], F32) for _ in range(4)]
            oT = alloc([8, 512], BF16)
            load_resident(wq, xq_d[li].rearrange("c p n -> p c n"), "wq")
            load_resident(wo, nat_view(xo_d[li]), "wo")

            def stage_a(g):
                hnT, qT, pb = hnT2[g % 2], qT2[g % 2], g % 2
                srcs = [(h_t[:, g * 4 + j, :], hkey(g * 4 + j)) for j in range(4)]
                norm_tiles(srcs, gi_x, hnT, lambda j: ("hnT", pb), hn_bufs, junk, 0)
                for oc in range(8):
                    ps, pk = pm()
                    for kc in range(8):
                        P.op("pe", lambda e: e.matmul(ps[:, :], lhsT=wq[:, oc, kc * 128:(kc + 1) * 128],
                                                      rhs=hnT[:, kc, :], start=(kc == 0), stop=(kc == 7)),
                             r=["wq", ("hnT", pb)], w=[pk])
                    P.op("act", lambda e: e.activation(out=qT[:, oc, :], in_=ps[:, :], func=AF.Identity, scale=0.0625),
                         r=[pk], w=[("qT", pb, oc)])

            def stage_b(g):
                qT, pb = qT2[g % 2], g % 2
                for hh in range(4):
                    for mc in range(2):
                        ps, pk = pm()
                        for dc in range(2):
                            P.op("pe", lambda e: e.matmul(
                                ps[:, :], lhsT=KT[:, 2 * hh + dc, mc * 128:(mc + 1) * 128], rhs=qT[:, 2 * hh + dc, :],
                                start=(dc == 0), stop=(dc == 1)), r=["KT", ("qT", pb, 2 * hh + dc)], w=[pk])
                        P.op("act", lambda e: e.activation(out=ET[:, hh, mc, :], in_=ps[:, :], func=AF.Exp),
                             r=[pk], w=[("ET", hh, mc)])
                for hh in range(4):
                    ps, pk = pm()
                    for mc in range(2):
                        P.op("pe", lambda e: e.matmul(ps[:, :], lhsT=onesb[:], rhs=ET[:, hh, mc, :],
                                                      start=(mc == 0), stop=(mc == 1)),
                             r=["onesb", ("ET", hh, mc)], w=[pk])
                    rd, rdk = rden[hh], ("rden", hh)
                    P.op("dve", lambda e: e.reciprocal(out=rd, in_=ps[:, :]), r=[pk], w=[rdk])
                for hh in range(4):
                    rd, rdk = rden[hh], ("rden", hh)
                    for dc in range(2):
                        ps2, pk2 = pm()
                        oc = 2 * hh + dc
                        for mc in range(2):
                            P.op("pe", lambda e: e.matmul(
                                ps2[:, :], lhsT=V[:, mc, oc * 128:(oc + 1) * 128], rhs=ET[:, hh, mc, :],
                                start=(mc == 0), stop=(mc == 1)), r=["V", ("ET", hh, mc)], w=[pk2])
                        P.op("dve", lambda e: e.tensor_tensor(out=oT[:, oc, :], in0=ps2[:, :], in1=rd, op=ALU.mult),
                             r=[pk2, rdk], w=[("oT", oc)])
                for s in range(4):
                    for half in range(2):
                        pso, pok = po4()
                        for kc in range(8):
                            P.op("pe", lambda e: e.matmul(
                                pso[:, :], lhsT=oT[:, kc, s * 128:(s + 1) * 128], rhs=wo[:, kc, half * 512:(half + 1) * 512],
                                start=(kc == 0), stop=(kc == 7)), r=[("oT", kc), "wo"], w=[pok])
                        ti = g * 4 + s
                        hv = h_t[:, ti, half * 512:(half + 1) * 512]
                        P.op("dve", lambda e: e.tensor_tensor(out=hv, in0=pso[:, :], in1=hv, op=ALU.add),
                             r=[pok, hkey(ti)], w=[hkey(ti)])

            stage_a(0)
            for g in range(NG):
                if g + 1 < NG:
                    stage_a(g + 1)
                stage_b(g)
            P.barrier()

        def ffn_core(hnT, h1T, gu, dr, silu_t, wg_of, wu_of, wd_of, gate_of):
            for grp in FGROUPS:
                dslots = {}
                for fi, f in enumerate(grp):
                    s, gv, gch, gkey = gu.next()
                    P.op("pool", lambda e, gv=gv, f=f: e.dma_start(out=gv[:, 0, :], in_=wg_of(f)), w=[(gkey, 0)], chan=gch)
                    P.op("pool", lambda e, gv=gv, f=f: e.dma_start(out=gv[:, 1, :], in_=wu_of(f)), w=[(gkey, 1)], chan=gu.last2)
                    s2, dv, dch, dkey = dr.next()
                    P.op("pool", lambda e, dv=dv, f=f: e.dma_start(out=dv, in_=wd_of(f)), w=[dkey], chan=dch)
                    dslots[fi] = (dv, dkey)
                    for sg in range(NG):
                        (pg, pgk), (pu, puk) = pm_pair()
                        for kc in range(8):
                            P.op("pe", lambda e, kc=kc, sg=sg, pg=pg, gv=gv: e.matmul(
                                pg[:, :], lhsT=gv[:, 0, kc * 128:(kc + 1) * 128], rhs=hnT[:, kc, sg * 512:(sg + 1) * 512],
                                start=(kc == 0), stop=(kc == 7)), r=[(gkey, 0), ("hnT", sg)], w=[pgk])
                        for kc in range(8):
                            P.op("pe", lambda e, kc=kc, sg=sg, pu=pu, gv=gv: e.matmul(
                                pu[:, :], lhsT=gv[:, 1, kc * 128:(kc + 1) * 128], rhs=hnT[:, kc, sg * 512:(sg + 1) * 512],
                                start=(kc == 0), stop=(kc == 7)), r=[(gkey, 1), ("hnT", sg)], w=[puk])
                        st_, stk = silu_t[sg % 2], ("silu", sg % 2)
                        P.op("act", lambda e, pg=pg, st_=st_: e.activation(out=st_, in_=pg[:, :], func=AF.Silu), r=[pgk], w=[stk])
                        P.op("dve", lambda e, pu=pu, st_=st_, fi=fi, sg=sg: e.tensor_tensor(
                            out=h1T[:, fi, sg * 512:(sg + 1) * 512], in0=pu[:, :], in1=st_, op=ALU.mult),
                            r=[puk, stk], w=[("h1T", fi, sg)])
                nfi = len(grp)
                for s in range(NT):
                    for half in range(2):
                        pso, pok = po()
                        for fi in range(nfi):
                            dv, dkey = dslots[fi]
                            P.op("pe", lambda e, fi=fi, s=s, half=half, pso=pso, dv=dv: e.matmul(
                                pso[:, :], lhsT=h1T[:, fi, s * 128:(s + 1) * 128], rhs=dv[:, half * 512:(half + 1) * 512],
                                start=(fi == 0), stop=(fi == nfi - 1)), r=[("h1T", fi, s // 4), dkey], w=[pok])
                        hv = h_t[:, s, half * 512:(half + 1) * 512]
                        if gate_of is None:
                            P.op("dve", lambda e, pso=pso, hv=hv: e.tensor_tensor(out=hv, in0=pso[:, :], in1=hv, op=ALU.add),
                                 r=[pok, hkey(s)], w=[hkey(s)])
                        else:
                            gap, gk = gate_of(s)
                            P.op("dve", lambda e, pso=pso, hv=hv, gap=gap: e.scalar_tensor_tensor(
                                out=hv, in0=pso[:, :], scalar=gap, in1=hv, op0=ALU.mult, op1=ALU.add),
                                r=[pok, hkey(s), gk], w=[hkey(s)])

        def ffn_alloc(r, tag):
            hnT = alloc([8, RANGE], BF16)
            h1T = alloc([6, RANGE], BF16)
            gu = Ring(P, "gu", [alloc([2, 1024], BF16) for _ in range(4)])
            dr = Ring(P, "dr", [alloc([1024], BF16) for _ in range(9)])
            silu_t = [alloc([512], BF16) for _ in range(2)]
            hn_bufs = [(alloc([1024], BF16), ("hn", i)) for i in range(2)]
            junk = alloc([1024], BF16)
            return hnT, h1T, gu, dr, silu_t, hn_bufs, junk

        def ffn_dense(r):
            reset_arena()
            hnT, h1T, gu, dr, silu_t, hn_bufs, junk = ffn_alloc(r, "f")
            for g in range(NG):
                srcs = [(h_t[:, g * 4 + j, :], hkey(g * 4 + j)) for j in range(4)]
                norm_tiles(srcs, 3, hnT[:, :, g * 512:(g + 1) * 512], lambda j, g=g: ("hnT", g), hn_bufs, junk, 0)
            ffn_core(hnT, h1T, gu, dr, silu_t,
                     lambda f: fg_d[f], lambda f: fu_d[f], lambda f: fd_d[f * 128:(f + 1) * 128, :], None)
            P.barrier()

        def gmlp(r):
            reset_arena()
            wu = alloc([8, 1024], BF16)
            wv = alloc([8, 1024], BF16)
            wout = alloc([8, 1024], BF16)
            wsT = alloc([8, 128], BF16)
            bsb = alloc([8, 128], F32)
            lng = alloc([1024], F32)
            lnb = alloc([1024], F32)
            hn_bufs = [(alloc([1024], BF16), ("hn", i)) for i in range(2)]
            junk = alloc([1024], BF16)
            hnT = alloc([8, 512], BF16)
            uT = alloc([8, 512], BF16)
            vg = [alloc([1024], F32) for _ in range(2)]
            vN = alloc([4, 1024], BF16)
            tmp = [alloc([512], F32) for _ in range(2)]
            mT = alloc([8, 512], BF16)
            bst = alloc([4, 8], F32)
            load_resident(wu, sgu_d.rearrange("c p n -> p c n"), "wu")
            load_resident(wv, nat_view(sgv_d), "wv")
            load_resident(wout, nat_view(sgout_d), "wout")
            load_resident(wsT, sgws_d.rearrange("p (g i) -> p g i", g=8), "wsT")
            P.op("sp", lambda e: e.dma_start(out=bsb, in_=sgbs_d.rearrange("p (g i) -> p g i", g=8)), w=["bsb"], chan=c_ph)
            P.op("sp", lambda e: e.dma_start(out=lng, in_=sglng_d), w=["lng"], chan=c_ph)
            P.op("sp", lambda e: e.dma_start(out=lnb, in_=sglnb_d), w=["lnb"], chan=c_ph)
            P.op("dve", lambda e: e.memset(wsT[64:128, :, 0:64], 0.0), r=[], w=["wsT"])
            bstf = bst.rearrange("p a b -> p (a b)")
            for g in range(NG):
                srcs = [(h_t[:, g * 4 + j, :], hkey(g * 4 + j)) for j in range(4)]
                norm_tiles(srcs, 4, hnT, lambda j: "hnT", hn_bufs, junk, 0)
                for s in range(4):
                    vgt, vgk = vg[s % 2], ("vg", s % 2)
                    so = (s % 2) * 16
                    for half in range(2):
                        ps, pk = pm()
                        for kc in range(8):
                            P.op("pe", lambda e, s=s, half=half, kc=kc, ps=ps: e.matmul(
                                ps[:, :], lhsT=hnT[:, kc, s * 128:(s + 1) * 128], rhs=wv[:, kc, half * 512:(half + 1) * 512],
                                start=(kc == 0), stop=(kc == 7)), r=["wv", "hnT"], w=[pk])
                        P.op("act", lambda e, half=half, ps=ps, vgt=vgt: e.activation(
                            out=vgt[:, half * 512:(half + 1) * 512], in_=ps[:, :], func=AF.Gelu_apprx_tanh), r=[pk], w=[vgk])
                        P.op("dve", lambda e, half=half, vgt=vgt, so=so: e.bn_stats(
                            out=bstf[:, so + half * 6:so + half * 6 + 6], in_=vgt[:, half * 512:(half + 1) * 512]),
                            r=[vgk], w=[("bst", s % 2)])
                    P.op("dve", lambda e, so=so: e.bn_aggr(out=bstf[:, so + 12:so + 14], in_=bstf[:, so:so + 12]),
                         r=[("bst", s % 2)], w=[("bst", s % 2)])
                    P.op("act", lambda e, so=so: e.activation(out=bstf[:, so + 14:so + 15], in_=bstf[:, so + 13:so + 14],
                                                              func=AF.Sqrt, bias=eps5[:, 0:1]),
                         r=[("bst", s % 2), "eps5"], w=[("bst", s % 2)])
                    P.op("dve", lambda e, so=so: e.reciprocal(out=bstf[:, so + 15:so + 16], in_=bstf[:, so + 14:so + 15]),
                         r=[("bst", s % 2)], w=[("bst", s % 2)])
                    P.op("dve", lambda e, so=so, vgt=vgt: e.tensor_scalar(
                        out=vgt, in0=vgt, scalar1=bstf[:, so + 12:so + 13], scalar2=bstf[:, so + 15:so + 16],
                        op0=ALU.subtract, op1=ALU.mult), r=[vgk, ("bst", s % 2)], w=[vgk])
                    P.op("pool", lambda e, vgt=vgt: e.tensor_tensor(out=vgt, in0=vgt, in1=lng, op=ALU.mult), r=[vgk, "lng"], w=[vgk])
                    P.op("pool", lambda e, vgt=vgt, s=s: e.tensor_tensor(out=vN[:, s, :], in0=vgt, in1=lnb, op=ALU.add),
                         r=[vgk, "lnb"], w=[("vN", s)])
                for oc in range(8):
                    ps, pk = pm()
                    for kc in range(8):
                        P.op("pe", lambda e, oc=oc, kc=kc, ps=ps: e.matmul(ps[:, :], lhsT=wu[:, oc, kc * 128:(kc + 1) * 128],
                                                                          rhs=hnT[:, kc, :], start=(kc == 0), stop=(kc == 7)),
                             r=["wu", "hnT"], w=[pk])
                    P.op("act", lambda e, oc=oc, ps=ps: e.activation(out=uT[:, oc, :], in_=ps[:, :], func=AF.Gelu_apprx_tanh),
                         r=[pk], w=[("uT", oc)])
                for gg in range(8):
                    ps, pk = pm()
                    for s in range(4):
                        P.op("pe", lambda e, gg=gg, s=s, ps=ps: e.matmul(
                            ps[:, s * 128:(s + 1) * 128], lhsT=vN[:, s, gg * 128:(gg + 1) * 128], rhs=wsT[:, gg, :],
                            start=True, stop=True), r=[("vN", s), "wsT"], w=[pk])
                    tt, tk = tmp[gg % 2], ("tmp", gg % 2)
                    for s in range(4):
                        P.op("dve", lambda e, gg=gg, s=s, ps=ps, tt=tt: e.tensor_tensor(
                            out=tt[:, s * 128:(s + 1) * 128], in0=ps[:, s * 128:(s + 1) * 128], in1=bsb[:, gg, :], op=ALU.add),
                            r=[pk, "bsb"], w=[tk])
                    P.op("dve", lambda e, gg=gg, tt=tt: e.tensor_tensor(out=mT[:, gg, :], in0=tt, in1=uT[:, gg, :], op=ALU.mult),
                         r=[tk, ("uT", gg)], w=[("mT", gg)])
                for s in range(4):
                    for half in range(2):
                        pso, pok = po4()
                        for kc in range(8):
                            P.op("pe", lambda e, kc=kc, s=s, half=half, pso=pso: e.matmul(
                                pso[:, :], lhsT=mT[:, kc, s * 128:(s + 1) * 128], rhs=wout[:, kc, half * 512:(half + 1) * 512],
                                start=(kc == 0), stop=(kc == 7)), r=[("mT", kc), "wout"], w=[pok])
                        ti = g * 4 + s
                        P.op("dve", lambda e, ti=ti, half=half, pso=pso: e.tensor_tensor(
                            out=h_t[:, ti, half * 512:(half + 1) * 512], in0=pso[:, :], in1=h_t[:, ti, half * 512:(half + 1) * 512], op=ALU.add),
                            r=[pok, hkey(ti)], w=[hkey(ti)])
            P.barrier()

        def moe(r):
            reset_arena()
            hnT, h1T, gu, dr, silu_t, hn_bufs, junk = ffn_alloc(r, "m")
            hn32 = [alloc([1024], F32) for _ in range(2)]
            hnT32 = [alloc([8, 128], F32) for _ in range(2)]
            wr = alloc([8, 8], F32)
            gates = alloc([NT, 8], F32)
            rt = alloc([NT, 64], F32)
            P.op("sp", lambda e: e.dma_start(out=wr, in_=mr_d.rearrange("p (k e) -> p k e", k=8)), w=["wr"], chan=c_ph)
            for s in range(NT):
                src, skey = h_t[:, s, :], hkey(s)
                col = (s % 4) * 4
                rstd_of(src, skey, junk, col, eps6, "eps6")
                hb, hbk = hn32[s % 2], ("hn32", s % 2)
                P.op("act", lambda e, src=src, hb=hb, col=col: e.activation(out=hb, in_=src, func=AF.Identity,
                                                                             scale=stat[:, col + 2:col + 3]),
                     r=[skey, ("stat", col + 2)], w=[hbk])
                for kc in range(8):
                    P.op("pe", lambda e, hb=hb, kc=kc: e.transpose(out=psTf[:, kc, :], in_=hb[:, kc * 128:(kc + 1) * 128],
                                                                  identity=identf[:]), r=[hbk, "identf"], w=["psTf"])
                ht, htk = hnT32[s % 2], ("hnT32", s % 2)
                P.op("dve", lambda e, ht=ht: e.tensor_tensor(
                    out=ht, in0=psTf, in1=gcols[:, 56:64].unsqueeze(2).to_broadcast([128, 8, 128]), op=ALU.mult),
                    r=["psTf", "gcols"], w=[htk])
                P.op("act", lambda e, ht=ht, s=s: e.activation(out=hnT[:, :, s * 128:(s + 1) * 128], in_=ht, func=AF.Identity),
                     r=[htk], w=[("hnT", s // 4)])
                ps, pk = pm()
                for kc in range(8):
                    P.op("pe", lambda e, ht=ht, kc=kc, ps=ps: e.matmul(ps[:, 0:8], lhsT=ht[:, kc, :], rhs=wr[:, kc, :],
                                                                      start=(kc == 0), stop=(kc == 7)), r=[htk, "wr"], w=[pk])
                rk = ("rt", s)
                lg = rt[:, s, 0:8]
                srt = rt[:, s, 8:16]
                nm1 = rt[:, s, 16:17]
                ex = rt[:, s, 24:32]
                sel = rt[:, s, 32:40]
                gs = rt[:, s, 40:48]
                den = rt[:, s, 48:49]
                rdn = rt[:, s, 49:50]
                P.op("dve", lambda e, lg=lg, ps=ps: e.tensor_copy(out=lg, in_=ps[:, 0:8]), r=[pk], w=[rk])
                P.op("dve", lambda e, lg=lg, srt=srt: e.max(out=srt, in_=lg), r=[rk], w=[rk])
                P.op("dve", lambda e, srt=srt, nm1=nm1: e.tensor_scalar(out=nm1, in0=srt[:, 0:1], scalar1=-1.0, scalar2=None, op0=ALU.mult),
                     r=[rk], w=[rk])
                P.op("act", lambda e, lg=lg, ex=ex, nm1=nm1: e.activation(out=ex, in_=lg, func=AF.Exp, bias=nm1), r=[rk], w=[rk])
                P.op("dve", lambda e, lg=lg, sel=sel, srt=srt: e.tensor_scalar(out=sel, in0=lg, scalar1=srt[:, 1:2], scalar2=None, op0=ALU.is_ge),
                     r=[rk], w=[rk])
                P.op("dve", lambda e, ex=ex, sel=sel, gs=gs, den=den: e.scalar_tensor_tensor(
                    out=gs, in0=ex, scalar=1.0, in1=sel, op0=ALU.mult, op1=ALU.mult, accum_out=den), r=[rk], w=[rk])
                P.op("dve", lambda e, den=den, rdn=rdn: e.reciprocal(out=rdn, in_=den), r=[rk], w=[rk])
                P.op("dve", lambda e, gs=gs, rdn=rdn, s=s: e.tensor_scalar(out=gates[:, s, :], in0=gs, scalar1=rdn, scalar2=None, op0=ALU.mult),
                     r=[rk], w=[("gates", s)])
            for ex_i in range(NE):
                ffn_core(hnT, h1T, gu, dr, silu_t,
                         lambda f, ex_i=ex_i: mg_d[ex_i * NF + f], lambda f, ex_i=ex_i: mu_d[ex_i * NF + f],
                         lambda f, ex_i=ex_i: md_d[ex_i * DFF + f * 128:ex_i * DFF + (f + 1) * 128, :],
                         lambda s, ex_i=ex_i: (gates[:, s, ex_i:ex_i + 1], ("gates", s)))
            P.barrier()


        def moe_sparse(r):
            BIG = 65536.0
            reset_arena()
            gates = alloc([NT, 8], F32)
            sel = alloc([NT, 8], F32)
            slotA_i = alloc([NT], I32)
            slotB_i = alloc([NT], I32)
            widx = alloc([16, NF], I32)
            base0 = state["off"]
            junk = alloc([1024], BF16)
            hn32 = [alloc([1024], F32) for _ in range(2)]
            hb16 = [alloc([1024], BF16) for _ in range(2)]
            hnT32 = [alloc([8, 128], F32) for _ in range(2)]
            wr = alloc([8, 8], F32)
            rt = alloc([NT, 64], F32)
            c_hn = [P.new_chan(f"hnst{i}") for i in range(2)]
            P.op("sp", lambda e: e.dma_start(out=wr, in_=mr_d.rearrange("p (k e) -> p k e", k=8)), w=["wr"], chan=c_ph)
            for s in range(NT):
                src, skey = h_t[:, s, :], hkey(s)
                col = (s % 4) * 4
                rstd_of(src, skey, junk, col, eps6, "eps6")
                hb, hbk = hn32[s % 2], ("hn32", s % 2)
                P.op("act", lambda e: e.activation(out=hb, in_=src, func=AF.Identity, scale=stat[:, col + 2:col + 3]),
                     r=[skey, ("stat", col + 2)], w=[hbk])
                h16, h16k = hb16[s % 2], ("hb16", s % 2)
                P.op("act", lambda e: e.activation(out=h16, in_=src, func=AF.Identity, scale=stat[:, col + 2:col + 3]),
                     r=[skey, ("stat", col + 2)], w=[h16k])
                P.op("sp", lambda e: e.dma_start(out=hn_d[s * 128:(s + 1) * 128, :], in_=h16), r=[h16k], w=[("hn_d", s)],
                     chan=c_hn[s % 2])
                for kc in range(8):
                    P.op("pe", lambda e: e.transpose(out=psTf[:, kc, :], in_=hb[:, kc * 128:(kc + 1) * 128], identity=identf[:]),
                         r=[hbk, "identf"], w=["psTf"])
                ht, htk = hnT32[s % 2], ("hnT32", s % 2)
                P.op("dve", lambda e: e.tensor_tensor(
                    out=ht, in0=psTf, in1=gcols[:, 56:64].unsqueeze(2).to_broadcast([128, 8, 128]), op=ALU.mult),
                    r=["psTf", "gcols"], w=[htk])
                ps, pk = pm()
                for kc in range(8):
                    P.op("pe", lambda e: e.matmul(ps[:, 0:8], lhsT=ht[:, kc, :], rhs=wr[:, kc, :], start=(kc == 0), stop=(kc == 7)),
                         r=[htk, "wr"], w=[pk])
                rk = ("rt", s)
                lg = rt[:, s, 0:8]
                srt = rt[:, s, 8:16]
                nm1 = rt[:, s, 16:17]
                ex = rt[:, s, 24:32]
                gs = rt[:, s, 40:48]
                den = rt[:, s, 48:49]
                rdn = rt[:, s, 49:50]
                selv = sel[:, s, :]
                P.op("dve", lambda e: e.tensor_copy(out=lg, in_=ps[:, 0:8]), r=[pk], w=[rk])
                P.op("dve", lambda e: e.max(out=srt, in_=lg), r=[rk], w=[rk])
                P.op("dve", lambda e: e.tensor_scalar(out=nm1, in0=srt[:, 0:1], scalar1=-1.0, scalar2=None, op0=ALU.mult), r=[rk], w=[rk])
                P.op("act", lambda e: e.activation(out=ex, in_=lg, func=AF.Exp, bias=nm1), r=[rk], w=[rk])
                P.op("dve", lambda e: e.tensor_scalar(out=selv, in0=lg, scalar1=srt[:, 1:2], scalar2=None, op0=ALU.is_ge), r=[rk], w=[rk, "sel"])
                P.op("dve", lambda e: e.scalar_tensor_tensor(out=gs, in0=ex, scalar=1.0, in1=selv, op0=ALU.mult, op1=ALU.mult, accum_out=den),
                     r=[rk, "sel"], w=[rk])
                P.op("dve", lambda e: e.reciprocal(out=rdn, in_=den), r=[rk], w=[rk])
                P.op("dve", lambda e: e.tensor_scalar(out=gates[:, s, :], in0=gs, scalar1=rdn, scalar2=None, op0=ALU.mult), r=[rk], w=["gates"])
            selb = alloc([128], BF16)
            cs = alloc([NT, 8], F32)
            pre = alloc([NT, 8], F32)
            cnt = alloc([8], F32)
            ntl = alloc([8], F32)
            endt = alloc([8], F32)
            baset = alloc([8], F32)
            slot = alloc([NT, 8], F32)
            m1 = alloc([NT, 8], F32)
            m2 = alloc([NT, 8], F32)
            isA = alloc([NT, 8], F32)
            gtmp = alloc([NT, 8], F32)
            slotA = alloc([NT], F32)
            slotB = alloc([NT], F32)
            gateA = alloc([NT], F32)
            gateB = alloc([NT], F32)
            tokf = alloc([NT], F32)
            rowsA = alloc([NT, 16], F32)
            rowsB = alloc([NT, 16], F32)
            zt = alloc([1024], I32)
            thr = alloc([16], F32)
            te = alloc([16], F32)
            pcol = alloc([1], F32)
            wbase = alloc([16], F32)
            f128 = alloc([NF], F32)
            widxf = alloc([16, NF], F32)
            self_flat = sel.rearrange("p a b -> p (a b)")
            P.op("dve", lambda e: e.tensor_copy(out=selb, in_=self_flat), r=["sel"], w=["selb"])
            prank, prk = pm()
            P.op("pe", lambda e: e.matmul(prank[:, 0:128], lhsT=trib[:], rhs=selb, start=True, stop=True), r=["trib", "selb"], w=[prk])
            pcs, pck = pm()
            P.op("pe", lambda e: e.matmul(pcs[:, 0:128], lhsT=onesb[:], rhs=selb, start=True, stop=True), r=["onesb", "selb"], w=[pck])
            P.op("dve", lambda e: e.tensor_copy(out=cs.rearrange("p a b -> p (a b)"), in_=pcs[:, 0:128]), r=[pck], w=["cs"])
            P.op("dve", lambda e: e.memset(pre[:, 0, :], 0.0), w=["pre"])
            for s in range(1, NT):
                P.op("dve", lambda e: e.tensor_tensor(out=pre[:, s, :], in0=pre[:, s - 1, :], in1=cs[:, s - 1, :], op=ALU.add),
                     r=["pre", "cs"], w=["pre"])
            P.op("dve", lambda e: e.tensor_tensor(out=cnt, in0=pre[:, NT - 1, :], in1=cs[:, NT - 1, :], op=ALU.add), r=["pre", "cs"], w=["cnt"])
            P.op("dve", lambda e: e.tensor_scalar(out=ntl, in0=cnt, scalar1=0.5, scalar2=None, op0=ALU.is_gt), r=["cnt"], w=["ntl"])
            for th in (512.5, 1024.5, 1536.5):
                P.op("dve", lambda e: e.scalar_tensor_tensor(out=ntl, in0=cnt, scalar=th, in1=ntl, op0=ALU.is_gt, op1=ALU.add),
                     r=["cnt", "ntl"], w=["ntl"])
            P.op("dve", lambda e: e.tensor_scalar(out=ntl, in0=ntl, scalar1=512.0, scalar2=None, op0=ALU.mult), r=["ntl"], w=["ntl"])
            P.op("dve", lambda e: e.tensor_copy(out=endt[:, 0:1], in_=ntl[:, 0:1]), r=["ntl"], w=["endt"])
            for ei in range(1, 8):
                P.op("dve", lambda e: e.tensor_tensor(out=endt[:, ei:ei + 1], in0=endt[:, ei - 1:ei], in1=ntl[:, ei:ei + 1], op=ALU.add),
                     r=["endt", "ntl"], w=["endt"])
            P.op("dve", lambda e: e.tensor_tensor(out=baset, in0=endt, in1=ntl, op=ALU.subtract), r=["endt", "ntl"], w=["baset"])
            P.op("dve", lambda e: e.tensor_tensor(out=slot, in0=prank[:, 0:128].rearrange("p (a b) -> p a b", a=NT), in1=pre, op=ALU.add),
                 r=[prk, "pre"], w=["slot"])
            P.op("dve", lambda e: e.tensor_tensor(out=slot, in0=slot, in1=baset.unsqueeze(1).to_broadcast([128, NT, 8]), op=ALU.add),
                 r=["slot", "baset"], w=["slot"])
            P.op("dve", lambda e: e.tensor_tensor(out=m1, in0=slot, in1=sel, op=ALU.mult), r=["slot", "sel"], w=["m1"])
            P.op("dve", lambda e: e.tensor_reduce(out=slotB, in_=m1, axis=AX.X, op=ALU.max), r=["m1"], w=["slotB"])
            P.op("dve", lambda e: e.tensor_scalar(out=m2, in0=sel, scalar1=-BIG, scalar2=BIG, op0=ALU.mult, op1=ALU.add), r=["sel"], w=["m2"])
            P.op("dve", lambda e: e.tensor_tensor(out=m2, in0=m2, in1=m1, op=ALU.add), r=["m2", "m1"], w=["m2"])
            P.op("dve", lambda e: e.tensor_reduce(out=slotA, in_=m2, axis=AX.X, op=ALU.min), r=["m2"], w=["slotA"])
            P.op("dve", lambda e: e.tensor_tensor(out=isA, in0=m2, in1=slotA.unsqueeze(2).to_broadcast([128, NT, 8]), op=ALU.is_equal),
                 r=["m2", "slotA"], w=["isA"])
            P.op("dve", lambda e: e.tensor_tensor(out=gtmp, in0=gates, in1=isA, op=ALU.mult), r=["gates", "isA"], w=["gtmp"])
            P.op("dve", lambda e: e.tensor_reduce(out=gateA, in_=gtmp, axis=AX.X, op=ALU.add), r=["gtmp"], w=["gateA"])
            P.op("dve", lambda e: e.tensor_tensor(out=isA, in0=sel, in1=isA, op=ALU.subtract), r=["sel", "isA"], w=["isA"])
            P.op("dve", lambda e: e.tensor_tensor(out=gtmp, in0=gates, in1=isA, op=ALU.mult), r=["gates", "isA"], w=["gtmp"])
            P.op("dve", lambda e: e.tensor_reduce(out=gateB, in_=gtmp, axis=AX.X, op=ALU.add), r=["gtmp"], w=["gateB"])
            P.op("dve", lambda e: e.tensor_copy(out=slotA_i, in_=slotA), r=["slotA"], w=["slotA_i"])
            P.op("dve", lambda e: e.tensor_copy(out=slotB_i, in_=slotB), r=["slotB"], w=["slotB_i"])
            P.op("pool", lambda e: e.iota(tokf, pattern=[[128, NT]], base=0, channel_multiplier=1, allow_small_or_imprecise_dtypes=True), w=["tokf"])
            P.op("pool", lambda e: e.iota(thr, pattern=[[512, 16]], base=0, channel_multiplier=0, allow_small_or_imprecise_dtypes=True), w=["thr"])
            P.op("pool", lambda e: e.iota(pcol, pattern=[[0, 1]], base=0, channel_multiplier=1, allow_small_or_imprecise_dtypes=True), w=["pcol"])
            P.op("pool", lambda e: e.iota(f128, pattern=[[128, NF]], base=0, channel_multiplier=0, allow_small_or_imprecise_dtypes=True), w=["f128"])
            P.op("pool", lambda e: e.memset(zt, 0), w=["zt"])
            for rows, gt_, nm in ((rowsA, gateA, "A"), (rowsB, gateB, "B")):
                rows_i = rows.bitcast(I32)
                P.op("dve", lambda e: e.memset(rows, 0.0), w=["rows" + nm])
                P.op("dve", lambda e: e.tensor_copy(out=rows_i[:, :, 0], in_=tokf), r=["tokf"], w=["rows" + nm])
                P.op("dve", lambda e: e.tensor_copy(out=rows[:, :, 1], in_=gt_), r=["gate" + nm], w=["rows" + nm])
            c_sc = P.new_chan("scat")
            c_z = P.new_chan("tabzero")
            P.op("pool", lambda e: e.dma_start(out=slot_tab_d.rearrange("(p a) c -> p (a c)", p=128), in_=zt), r=["zt"], w=["tabz"], chan=c_z)
            for s in range(NT):
                for rows, si, nm in ((rowsA, slotA_i, "A"), (rowsB, slotB_i, "B")):
                    rows_i = rows.bitcast(I32)
                    P.op("pool", lambda e: e.indirect_dma_start(
                        out=slot_tab_d, out_offset=bass.IndirectOffsetOnAxis(ap=si[:, s:s + 1], axis=0),
                        in_=rows_i[:, s, :], in_offset=None),
                        r=["rows" + nm, "slot" + nm + "_i", "tabz"], w=[("tab", nm, s)], chan=c_sc)
            tabkeys = [("tab", nm, s) for nm in "AB" for s in range(NT)]
            P.op("dve", lambda e: e.memset(te, 0.0), w=["te"])
            for ei in range(8):
                P.op("dve", lambda e: e.scalar_tensor_tensor(out=te, in0=thr, scalar=endt[:, ei:ei + 1], in1=te, op0=ALU.is_ge, op1=ALU.add),
                     r=["thr", "endt", "te"], w=["te"])
            P.op("dve", lambda e: e.tensor_scalar(out=te, in0=te, scalar1=7.0, scalar2=None, op0=ALU.min), r=["te"], w=["te"])
            P.op("dve", lambda e: e.tensor_scalar(out=wbase, in0=te, scalar1=float(DFF), scalar2=pcol[:, 0:1], op0=ALU.mult, op1=ALU.add),
                 r=["te", "pcol"], w=["wbase"])
            for i in range(16):
                P.op("dve", lambda e: e.tensor_scalar(out=widxf[:, i, :], in0=f128, scalar1=wbase[:, i:i + 1], scalar2=None, op0=ALU.add),
                     r=["f128", "wbase"], w=["widxf"])
            P.op("dve", lambda e: e.tensor_copy(out=widx, in_=widxf), r=["widxf"], w=["widx"])
            P.barrier()
            reset_arena(base0)
            gu = Ring(P, "gu", [alloc([2, 1024], BF16) for _ in range(6)])
            dr = Ring(P, "dr", [alloc([1024], BF16) for _ in range(10)])
            silu_t = [alloc([512], BF16) for _ in range(2)]
            NH1 = 8
            h1T = alloc([NH1, 512], BF16)
            hrows2 = [alloc([4, 1024], BF16) for _ in range(2)]
            hnTt = [alloc([8, 512], BF16) for _ in range(2)]
            acc = [alloc([4, 1024], F32) for _ in range(2)]
            tabt = [alloc([4, 16], F32) for _ in range(2)]
            c_tab = [P.new_chan(f"tabld{i}") for i in range(2)]
            c_hg = [P.new_chan(f"hgath{i}") for i in range(2)]
            c_ys = [P.new_chan(f"yst{i}") for i in range(2)]
            hnkeys = [("hn_d", s) for s in range(NT)]

            def prefetch(i):
                b = i % 2
                tb_i = tabt[b].bitcast(I32)
                P.op("sp", lambda e: e.dma_start(out=tb_i, in_=slot_tab_d[i * 512:(i + 1) * 512, :].rearrange("(j p) c -> p j c", p=128)),
                     r=tabkeys, w=[("tabt", b)], chan=c_tab[b])
                for j in range(4):
                    P.op("pool", lambda e: e.indirect_dma_start(
                        out=hrows2[b][:, j, :], out_offset=None, in_=hn_d,
                        in_offset=bass.IndirectOffsetOnAxis(ap=tb_i[:, j, 0:1], axis=0)),
                        r=[("tabt", b)] + hnkeys, w=[("hrows", b, j)], chan=c_hg[b])

            def transposes(i):
                b = i % 2
                hT = hnTt[b]
                for j0 in (0, 2):
                    for jj in range(2):
                        j = j0 + jj
                        for kc in range(8):
                            P.op("pe", lambda e: e.transpose(out=psTb[:, kc, jj * 128:(jj + 1) * 128],
                                                             in_=hrows2[b][:, j, kc * 128:(kc + 1) * 128], identity=identb[:]),
                                 r=[("hrows", b, j), "identb"], w=[("psT", jj)])
                    c0 = j0 * 128
                    P.op("dve", lambda e: e.tensor_tensor(
                        out=hT[:, :, c0:c0 + 256], in0=psTb[:, :, 0:256],
                        in1=gcols[:, 56:64].unsqueeze(2).to_broadcast([128, 8, 256]), op=ALU.mult),
                        r=[("psT", 0), ("psT", 1), "gcols"], w=[("hnTt", b)])

            prefetch(0)
            transposes(0)
            for i in range(16):
                b = i % 2
                tb = tabt[b]
                tbk = ("tabt", b)
                hT = hnTt[b]
                hTk = ("hnTt", b)
                ac = acc[b]
                ack = ("acc", b)
                if i + 1 < 16:
                    prefetch(i + 1)
                cstate = {"c": 0}
                info = {}

                def emit_gu(f):
                    cidx = cstate["c"]
                    cstate["c"] += 1
                    hs = cidx % NH1
                    _, gv, gch, gkey = gu.next()
                    gch2 = gu.last2
                    P.op("pool", lambda e: e.indirect_dma_start(
                        out=gv[:, 0, :], out_offset=None, in_=mg_flat,
                        in_offset=bass.IndirectOffsetOnAxis(ap=widx[:, i, f:f + 1], axis=0)), r=["widx"], w=[(gkey, 0)], chan=gch)
                    P.op("pool", lambda e: e.indirect_dma_start(
                        out=gv[:, 1, :], out_offset=None, in_=mu_flat,
                        in_offset=bass.IndirectOffsetOnAxis(ap=widx[:, i, f:f + 1], axis=0)), r=["widx"], w=[(gkey, 1)], chan=gch2)
                    _, dv, dch, dkey = dr.next()
                    P.op("pool", lambda e: e.indirect_dma_start(
                        out=dv, out_offset=None, in_=md_d,
                        in_offset=bass.IndirectOffsetOnAxis(ap=widx[:, i, f:f + 1], axis=0)), r=["widx"], w=[dkey], chan=dch)
                    info[f] = (hs, dv, dkey)
                    (pg, pgk), (pu, puk) = pm_pair()
                    for kc in range(8):
                        P.op("pe", lambda e: e.matmul(pg[:, :], lhsT=gv[:, 0, kc * 128:(kc + 1) * 128], rhs=hT[:, kc, :],
                                                      start=(kc == 0), stop=(kc == 7)), r=[(gkey, 0), hTk], w=[pgk])
                    for kc in range(8):
                        P.op("pe", lambda e: e.matmul(pu[:, :], lhsT=gv[:, 1, kc * 128:(kc + 1) * 128], rhs=hT[:, kc, :],
                                                      start=(kc == 0), stop=(kc == 7)), r=[(gkey, 1), hTk], w=[puk])
                    st_, stk = silu_t[cidx % 2], ("silu", cidx % 2)
                    P.op("act", lambda e: e.activation(out=st_, in_=pg[:, :], func=AF.Silu), r=[pgk], w=[stk])
                    P.op("dve", lambda e: e.tensor_tensor(out=h1T[:, hs, :], in0=pu[:, :], in1=st_, op=ALU.mult),
                         r=[puk, stk], w=[("h1T", hs)])

                def emit_down(gi, grp):
                    nfi = len(grp)
                    for j in range(4):
                        for half in range(2):
                            pso, pok = po()
                            for fi, f in enumerate(grp):
                                hs, dv, dkey = info[f]
                                P.op("pe", lambda e: e.matmul(pso[:, :], lhsT=h1T[:, hs, j * 128:(j + 1) * 128],
                                                              rhs=dv[:, half * 512:(half + 1) * 512],
                                                              start=(fi == 0), stop=(fi == nfi - 1)), r=[("h1T", hs), dkey], w=[pok])
                            av = ac[:, j, half * 512:(half + 1) * 512]
                            gap = tb[:, j, 1:2]
                            if gi == 0:
                                P.op("dve", lambda e: e.tensor_scalar(out=av, in0=pso[:, :], scalar1=gap, scalar2=None, op0=ALU.mult),
                                     r=[pok, tbk], w=[ack])
                            else:
                                P.op("dve", lambda e: e.scalar_tensor_tensor(out=av, in0=pso[:, :], scalar=gap, in1=av,
                                                                             op0=ALU.mult, op1=ALU.add), r=[pok, tbk, ack], w=[ack])

                for gi, grp in enumerate(FGROUPS):
                    for k, f in enumerate(grp):
                        if k == 0 and gi > 0:
                            continue
                        emit_gu(f)
                    if gi + 1 < len(FGROUPS):
                        emit_gu(FGROUPS[gi + 1][0])
                    if gi == 1 and i + 1 < 16:
                        transposes(i + 1)
                    emit_down(gi, grp)
                P.op("sp", lambda e: e.dma_start(out=yslot_d[i * 512:(i + 1) * 512, :].rearrange("(j p) d -> p j d", p=128), in_=ac),
                     r=[ack], w=[("yslot", i)], chan=c_ys[b])
            P.barrier()
            reset_arena(base0)
            ya = [alloc([1024], F32) for _ in range(2)]
            yb = [alloc([1024], F32) for _ in range(2)]
            c_ya = [P.new_chan(f"ya{i}") for i in range(2)]
            c_yb = [P.new_chan(f"yb{i}") for i in range(2)]
            for s in range(NT):
                b = s % 2
                for yy, si, cc, nm in ((ya[b], slotA_i, c_ya[b], "ya"), (yb[b], slotB_i, c_yb[b], "yb")):
                    P.op("pool", lambda e: e.indirect_dma_start(
                        out=yy, out_offset=None, in_=yslot_d, in_offset=bass.IndirectOffsetOnAxis(ap=si[:, s:s + 1], axis=0)),
                        r=["slotA_i", "slotB_i"], w=[(nm, b)], chan=cc)
                    P.op("dve", lambda e: e.tensor_tensor(out=h_t[:, s, :], in0=h_t[:, s, :], in1=yy, op=ALU.add),
                         r=[(nm, b), hkey(s)], w=[hkey(s)])
            P.barrier()

        def moe2_stage1(r):
            gates, sel = gates_g, sel_g
            reset_arena()
            junk = alloc([1024], BF16)
            hn32 = [alloc([1024], F32) for _ in range(2)]
            hb16 = [alloc([1024], BF16) for _ in range(2)]
            hnT32 = [alloc([8, 128], F32) for _ in range(2)]
            wr = alloc([8, 8], F32)
            rt = alloc([NT, 64], F32)
            c_hn = [P.new_chan(f"hnst{i}") for i in range(2)]
            P.op("sp", lambda e: e.dma_start(out=wr, in_=mr_d.rearrange("p (k e) -> p k e", k=8)), w=["wr"], chan=c_ph)
            for s in range(NT):
                src, skey = h_t[:, s, :], hkey(s)
                P.op("act", lambda e: e.activation(out=junk, in_=src, func=AF.Square, accum_out=stat[:, s:s + 1]),
                     r=[skey], w=["junk", ("mss", s)])
            P.op("act", lambda e: e.activation(out=stat[:, 16:32], in_=stat[:, 0:16], func=AF.Sqrt, scale=1.0 / D, bias=eps6[:, 0:1]),
                 r=[("mss", s) for s in range(NT)] + ["eps6"], w=["msd"])
            P.op("dve", lambda e: e.reciprocal(out=stat[:, 32:48], in_=stat[:, 16:32]), r=["msd"], w=["mrs"])
            for s in range(NT):
                src, skey = h_t[:, s, :], hkey(s)
                col = 30 + s
                hb, hbk = hn32[s % 2], ("hn32", s % 2)
                P.op("act", lambda e: e.activation(out=hb, in_=src, func=AF.Identity, scale=stat[:, col + 2:col + 3]),
                     r=[skey, "mrs"], w=[hbk])
                h16, h16k = hb16[s % 2], ("hb16", s % 2)
                P.op("act", lambda e: e.activation(out=h16, in_=src, func=AF.Identity, scale=stat[:, col + 2:col + 3]),
                     r=[skey, "mrs"], w=[h16k])
                P.op("sp", lambda e: e.dma_start(out=hn_d[(r * NT + s) * 128:(r * NT + s + 1) * 128, :], in_=h16), r=[h16k], w=[("hn_d", r * NT + s)],
                     chan=c_hn[s % 2])
                for kc in range(8):
                    P.op("pe", lambda e: e.transpose(out=psTf[:, kc, :], in_=hb[:, kc * 128:(kc + 1) * 128], identity=identf[:]),
                         r=[hbk, "identf"], w=["psTf"])
                ht, htk = hnT32[s % 2], ("hnT32", s % 2)
                P.op("dve", lambda e: e.tensor_tensor(
                    out=ht, in0=psTf, in1=gcols[:, 56:64].unsqueeze(2).to_broadcast([128, 8, 128]), op=ALU.mult),
                    r=["psTf", "gcols"], w=[htk])
                ps, pk = pm()
                for kc in range(8):
                    P.op("pe", lambda e: e.matmul(ps[:, 0:8], lhsT=ht[:, kc, :], rhs=wr[:, kc, :], start=(kc == 0), stop=(kc == 7)),
                         r=[htk, "wr"], w=[pk])
                rk = ("rt", s)
                lg = rt[:, s, 0:8]
                srt = rt[:, s, 8:16]
                nm1 = rt[:, s, 16:17]
                ex = rt[:, s, 24:32]
                gs = rt[:, s, 40:48]
                den = rt[:, s, 48:49]
                rdn = rt[:, s, 49:50]
                selv = sel[:, r * NT + s, :]
                P.op("dve", lambda e: e.tensor_copy(out=lg, in_=ps[:, 0:8]), r=[pk], w=[rk])
                P.op("dve", lambda e: e.max(out=srt, in_=lg), r=[rk], w=[rk])
                P.op("dve", lambda e: e.tensor_scalar(out=nm1, in0=srt[:, 0:1], scalar1=-1.0, scalar2=None, op0=ALU.mult), r=[rk], w=[rk])
                P.op("act", lambda e: e.activation(out=ex, in_=lg, func=AF.Exp, bias=nm1), r=[rk], w=[rk])
                P.op("dve", lambda e: e.tensor_scalar(out=selv, in0=lg, scalar1=srt[:, 1:2], scalar2=None, op0=ALU.is_ge), r=[rk], w=[rk, "sel"])
                P.op("dve", lambda e: e.scalar_tensor_tensor(out=gs, in0=ex, scalar=1.0, in1=selv, op0=ALU.mult, op1=ALU.mult, accum_out=den),
                     r=[rk, "sel"], w=[rk])
                P.op("dve", lambda e: e.reciprocal(out=rdn, in_=den), r=[rk], w=[rk])
                P.op("dve", lambda e: e.tensor_scalar(out=gates[:, r * NT + s, :], in0=gs, scalar1=rdn, scalar2=None, op0=ALU.mult), r=[rk], w=["gates"])
            if r < nranges - 1:
                dst = hpark_d[r * RANGE:(r + 1) * RANGE, :].rearrange("(s p) d -> p s d", p=128)
                for q in range(4):
                    P.op("sp", lambda e: e.dma_start(out=dst[:, q * 4:(q + 1) * 4, :], in_=h_t[:, q * 4:(q + 1) * 4, :]),
                         r=[hkey(s) for s in range(q * 4, q * 4 + 4)], w=[("hpark", r, q)], chan=c_xs[q])
            P.barrier()

        def moe2_rest(nr):
            BIG = 65536.0
            NTT = NT * nr
            NTL = 8 * nr + 8
            gates, sel, slotA_i, slotB_i, widx = gates_g, sel_g, slotA_ig, slotB_ig, widx_g
            reset_arena()
            base0 = state["off"]
            selb = alloc([NTT * 8], BF16)
            cs = alloc([NTT, 8], F32)
            pre = alloc([NTT, 8], F32)
            cnt = alloc([8], F32)
            ntl = alloc([8], F32)
            endt = alloc([8], F32)
            baset = alloc([8], F32)
            slot = alloc([NTT, 8], F32)
            m1 = alloc([NTT, 8], F32)
            m2 = alloc([NTT, 8], F32)
            isA = alloc([NTT, 8], F32)
            gtmp = alloc([NTT, 8], F32)
            slotA = alloc([NTT], F32)
            slotB = alloc([NTT], F32)
            gateA = alloc([NTT], F32)
            gateB = alloc([NTT], F32)
            tokf = alloc([NTT], F32)
            rowsA = alloc([NTT, 16], F32)
            rowsB = alloc([NTT, 16], F32)
            zt = alloc([NTL * 64], I32)
            thr = alloc([NTL], F32)
            te = alloc([NTL], F32)
            pcol = alloc([1], F32)
            wbase = alloc([NTL], F32)
            f128 = alloc([NF], F32)
            widxf = alloc([NTL, NF], F32)
            self_flat = sel.rearrange("p a b -> p (a b)")
            P.op("dve", lambda e: e.tensor_copy(out=selb, in_=self_flat), r=["sel"], w=["selb"])
            prank, prk = pm()
            P.op("pe", lambda e: e.matmul(prank[:, 0:NTT * 8], lhsT=trib[:], rhs=selb, start=True, stop=True), r=["trib", "selb"], w=[prk])
            pcs, pck = pm()
            P.op("pe", lambda e: e.matmul(pcs[:, 0:NTT * 8], lhsT=onesb[:], rhs=selb, start=True, stop=True), r=["onesb", "selb"], w=[pck])
            P.op("dve", lambda e: e.tensor_copy(out=cs.rearrange("p a b -> p (a b)"), in_=pcs[:, 0:NTT * 8]), r=[pck], w=["cs"])
            P.op("dve", lambda e: e.memset(pre[:, 0, :], 0.0), w=["pre"])
            for s in range(1, NTT):
                P.op("dve", lambda e: e.tensor_tensor(out=pre[:, s, :], in0=pre[:, s - 1, :], in1=cs[:, s - 1, :], op=ALU.add),
                     r=["pre", "cs"], w=["pre"])
            P.op("dve", lambda e: e.tensor_tensor(out=cnt, in0=pre[:, NTT - 1, :], in1=cs[:, NTT - 1, :], op=ALU.add), r=["pre", "cs"], w=["cnt"])
            P.op("dve", lambda e: e.tensor_scalar(out=ntl, in0=cnt, scalar1=0.5, scalar2=None, op0=ALU.is_gt), r=["cnt"], w=["ntl"])
            for th in [512.0 * k + 0.5 for k in range(1, NTT // 4)]:
                P.op("dve", lambda e: e.scalar_tensor_tensor(out=ntl, in0=cnt, scalar=th, in1=ntl, op0=ALU.is_gt, op1=ALU.add),
                     r=["cnt", "ntl"], w=["ntl"])
            P.op("dve", lambda e: e.tensor_scalar(out=ntl, in0=ntl, scalar1=512.0, scalar2=None, op0=ALU.mult), r=["ntl"], w=["ntl"])
            P.op("dve", lambda e: e.tensor_copy(out=endt[:, 0:1], in_=ntl[:, 0:1]), r=["ntl"], w=["endt"])
            for ei in range(1, 8):
                P.op("dve", lambda e: e.tensor_tensor(out=endt[:, ei:ei + 1], in0=endt[:, ei - 1:ei], in1=ntl[:, ei:ei + 1], op=ALU.add),
                     r=["endt", "ntl"], w=["endt"])
            P.op("dve", lambda e: e.tensor_tensor(out=baset, in0=endt, in1=ntl, op=ALU.subtract), r=["endt", "ntl"], w=["baset"])
            P.op("dve", lambda e: e.tensor_tensor(out=slot, in0=prank[:, 0:NTT * 8].rearrange("p (a b) -> p a b", a=NTT), in1=pre, op=ALU.add),
                 r=[prk, "pre"], w=["slot"])
            P.op("dve", lambda e: e.tensor_tensor(out=slot, in0=slot, in1=baset.unsqueeze(1).to_broadcast([128, NTT, 8]), op=ALU.add),
                 r=["slot", "baset"], w=["slot"])
            P.op("dve", lambda e: e.tensor_tensor(out=m1, in0=slot, in1=sel, op=ALU.mult), r=["slot", "sel"], w=["m1"])
            P.op("dve", lambda e: e.tensor_reduce(out=slotB, in_=m1, axis=AX.X, op=ALU.max), r=["m1"], w=["slotB"])
            P.op("dve", lambda e: e.tensor_scalar(out=m2, in0=sel, scalar1=-BIG, scalar2=BIG, op0=ALU.mult, op1=ALU.add), r=["sel"], w=["m2"])
            P.op("dve", lambda e: e.tensor_tensor(out=m2, in0=m2, in1=m1, op=ALU.add), r=["m2", "m1"], w=["m2"])
            P.op("dve", lambda e: e.tensor_reduce(out=slotA, in_=m2, axis=AX.X, op=ALU.min), r=["m2"], w=["slotA"])
            P.op("dve", lambda e: e.tensor_tensor(out=isA, in0=m2, in1=slotA.unsqueeze(2).to_broadcast([128, NTT, 8]), op=ALU.is_equal),
                 r=["m2", "slotA"], w=["isA"])
            P.op("dve", lambda e: e.tensor_tensor(out=gtmp, in0=gates, in1=isA, op=ALU.mult), r=["gates", "isA"], w=["gtmp"])
            P.op("dve", lambda e: e.tensor_reduce(out=gateA, in_=gtmp, axis=AX.X, op=ALU.add), r=["gtmp"], w=["gateA"])
            P.op("dve", lambda e: e.tensor_tensor(out=isA, in0=sel, in1=isA, op=ALU.subtract), r=["sel", "isA"], w=["isA"])
            P.op("dve", lambda e: e.tensor_tensor(out=gtmp, in0=gates, in1=isA, op=ALU.mult), r=["gates", "isA"], w=["gtmp"])
            P.op("dve", lambda e: e.tensor_reduce(out=gateB, in_=gtmp, axis=AX.X, op=ALU.add), r=["gtmp"], w=["gateB"])
            for sl_, nm_ in ((slotA, "slotA"), (slotB, "slotB")):
                P.op("dve", lambda e: e.tensor_scalar(out=sl_, in0=sl_, scalar1=0.0, scalar2=float(NTL * 512 - 1),
                                                      op0=ALU.max, op1=ALU.min), r=[nm_], w=[nm_])
            P.op("dve", lambda e: e.tensor_copy(out=slotA_i, in_=slotA), r=["slotA"], w=["slotA_i"])
            P.op("dve", lambda e: e.tensor_copy(out=slotB_i, in_=slotB), r=["slotB"], w=["slotB_i"])
            P.op("pool", lambda e: e.iota(tokf, pattern=[[128, NTT]], base=0, channel_multiplier=1, allow_small_or_imprecise_dtypes=True), w=["tokf"])
            P.op("pool", lambda e: e.iota(thr, pattern=[[512, NTL]], base=0, channel_multiplier=0, allow_small_or_imprecise_dtypes=True), w=["thr"])
            P.op("pool", lambda e: e.iota(pcol, pattern=[[0, 1]], base=0, channel_multiplier=1, allow_small_or_imprecise_dtypes=True), w=["pcol"])
            P.op("pool", lambda e: e.iota(f128, pattern=[[128, NF]], base=0, channel_multiplier=0, allow_small_or_imprecise_dtypes=True), w=["f128"])
            P.op("pool", lambda e: e.memset(zt, 0), w=["zt"])
            for rows, gt_, nm in ((rowsA, gateA, "A"), (rowsB, gateB, "B")):
                rows_i = rows.bitcast(I32)
                P.op("dve", lambda e: e.memset(rows, 0.0), w=["rows" + nm])
                P.op("dve", lambda e: e.tensor_copy(out=rows_i[:, :, 0], in_=tokf), r=["tokf"], w=["rows" + nm])
                P.op("dve", lambda e: e.tensor_copy(out=rows[:, :, 1], in_=gt_), r=["gate" + nm], w=["rows" + nm])
            c_scs = [P.new_chan(f"scat{i}") for i in range(4)]
            c_z = P.new_chan("tabzero")
            P.op("pool", lambda e: e.dma_start(out=slot_tab_d.rearrange("(p a) c -> p (a c)", p=128), in_=zt), r=["zt"], w=["tabz"], chan=c_z)
            for s in range(NTT):
                for rows, si, nm in ((rowsA, slotA_i, "A"), (rowsB, slotB_i, "B")):
                    rows_i = rows.bitcast(I32)
                    P.op("pool", lambda e: e.indirect_dma_start(
                        out=slot_tab_d, out_offset=bass.IndirectOffsetOnAxis(ap=si[:, s:s + 1], axis=0),
                        in_=rows_i[:, s, :], in_offset=None),
                        r=["rows" + nm, "slot" + nm + "_i", "tabz", ("scq", (2 * s + (nm == "B")) % 4)],
                        w=[("tab", nm, s), ("scq", (2 * s + (nm == "B")) % 4)], chan=c_scs[(2 * s + (nm == "B")) % 4])
            tabkeys = [("tab", nm, s) for nm in "AB" for s in range(NTT)]
            P.op("dve", lambda e: e.memset(te, 0.0), w=["te"])
            for ei in range(8):
                P.op("dve", lambda e: e.scalar_tensor_tensor(out=te, in0=thr, scalar=endt[:, ei:ei + 1], in1=te, op0=ALU.is_ge, op1=ALU.add),
                     r=["thr", "endt", "te"], w=["te"])
            P.op("dve", lambda e: e.tensor_scalar(out=te, in0=te, scalar1=7.0, scalar2=0.0, op0=ALU.min, op1=ALU.max), r=["te"], w=["te"])
            P.op("dve", lambda e: e.tensor_scalar(out=wbase, in0=te, scalar1=float(DFF), scalar2=pcol[:, 0:1], op0=ALU.mult, op1=ALU.add),
                 r=["te", "pcol"], w=["wbase"])
            for i in range(NTL):
                P.op("dve", lambda e: e.tensor_scalar(out=widxf[:, i, :], in0=f128, scalar1=wbase[:, i:i + 1], scalar2=None, op0=ALU.add),
                     r=["f128", "wbase"], w=["widxf"])
            P.op("dve", lambda e: e.tensor_copy(out=widx, in_=widxf), r=["widxf"], w=["widx"])
            P.barrier()
            reset_arena(base0)
            gu = Ring(P, "gu", [alloc([2, 1024], BF16) for _ in range(6)])
            dr = Ring(P, "dr", [alloc([1024], BF16) for _ in range(10)])
            silu_t = [alloc([512], BF16) for _ in range(2)]
            NH1 = 8
            h1T = alloc([NH1, 512], BF16)
            hrows2 = [alloc([4, 1024], BF16) for _ in range(2)]
            hnTt = [alloc([8, 512], BF16) for _ in range(2)]
            acc = [alloc([4, 1024], F32) for _ in range(2)]
            tabt = [alloc([4, 16], F32) for _ in range(2)]
            c_tab = [P.new_chan(f"tabld{i}") for i in range(2)]
            c_hg = [P.new_chan(f"hgath{i}") for i in range(2)]
            c_ys = [P.new_chan(f"yst{i}") for i in range(2)]
            hnkeys = [("hn_d", s) for s in range(NTT)]

            def prefetch(i):
                b = i % 2
                tb_i = tabt[b].bitcast(I32)
                P.op("sp", lambda e: e.dma_start(out=tb_i, in_=slot_tab_d[i * 512:(i + 1) * 512, :].rearrange("(j p) c -> p j c", p=128)),
                     r=tabkeys, w=[("tabt", b)], chan=c_tab[b])
                for j in range(4):
                    P.op("pool", lambda e: e.indirect_dma_start(
                        out=hrows2[b][:, j, :], out_offset=None, in_=hn_d,
                        in_offset=bass.IndirectOffsetOnAxis(ap=tb_i[:, j, 0:1], axis=0)),
                        r=[("tabt", b)] + hnkeys, w=[("hrows", b, j)], chan=c_hg[b])

            def transposes(i):
                b = i % 2
                hT = hnTt[b]
                for j0 in (0, 2):
                    for jj in range(2):
                        j = j0 + jj
                        for kc in range(8):
                            P.op("pe", lambda e: e.transpose(out=psTb[:, kc, jj * 128:(jj + 1) * 128],
                                                             in_=hrows2[b][:, j, kc * 128:(kc + 1) * 128], identity=identb[:]),
                                 r=[("hrows", b, j), "identb"], w=[("psT", jj)])
                    c0 = j0 * 128
                    P.op("dve", lambda e: e.tensor_tensor(
                        out=hT[:, :, c0:c0 + 256], in0=psTb[:, :, 0:256],
                        in1=gcols[:, 56:64].unsqueeze(2).to_broadcast([128, 8, 256]), op=ALU.mult),
                        r=[("psT", 0), ("psT", 1), "gcols"], w=[("hnTt", b)])

            prefetch(0)
            transposes(0)
            for i in range(NTL):
                b = i % 2
                tb = tabt[b]
                tbk = ("tabt", b)
                hT = hnTt[b]
                hTk = ("hnTt", b)
                ac = acc[b]
                ack = ("acc", b)
                if i + 1 < NTL:
                    prefetch(i + 1)
                cstate = {"c": 0}
                info = {}

                def emit_gu(f):
                    cidx = cstate["c"]
                    cstate["c"] += 1
                    hs = cidx % NH1
                    _, gv, gch, gkey = gu.next()
                    gch2 = gu.last2
                    P.op("pool", lambda e: e.indirect_dma_start(
                        out=gv[:, 0, :], out_offset=None, in_=mg_flat,
                        in_offset=bass.IndirectOffsetOnAxis(ap=widx[:, i, f:f + 1], axis=0)), r=["widx"], w=[(gkey, 0)], chan=gch)
                    P.op("pool", lambda e: e.indirect_dma_start(
                        out=gv[:, 1, :], out_offset=None, in_=mu_flat,
                        in_offset=bass.IndirectOffsetOnAxis(ap=widx[:, i, f:f + 1], axis=0)), r=["widx"], w=[(gkey, 1)], chan=gch2)
                    _, dv, dch, dkey = dr.next()
                    P.op("pool", lambda e: e.indirect_dma_start(
                        out=dv, out_offset=None, in_=md_d,
                        in_offset=bass.IndirectOffsetOnAxis(ap=widx[:, i, f:f + 1], axis=0)), r=["widx"], w=[dkey], chan=dch)
                    info[f] = (hs, dv, dkey)
                    (pg, pgk), (pu, puk) = pm_pair()
                    for kc in range(8):
                        P.op("pe", lambda e: e.matmul(pg[:, :], lhsT=gv[:, 0, kc * 128:(kc + 1) * 128], rhs=hT[:, kc, :],
                                                      start=(kc == 0), stop=(kc == 7)), r=[(gkey, 0), hTk], w=[pgk])
                    for kc in range(8):
                        P.op("pe", lambda e: e.matmul(pu[:, :], lhsT=gv[:, 1, kc * 128:(kc + 1) * 128], rhs=hT[:, kc, :],
                                                      start=(kc == 0), stop=(kc == 7)), r=[(gkey, 1), hTk], w=[puk])
                    st_, stk = silu_t[cidx % 2], ("silu", cidx % 2)
                    P.op("act", lambda e: e.activation(out=st_, in_=pg[:, :], func=AF.Silu), r=[pgk], w=[stk])
                    P.op("dve", lambda e: e.tensor_tensor(out=h1T[:, hs, :], in0=pu[:, :], in1=st_, op=ALU.mult),
                         r=[puk, stk], w=[("h1T", hs)])

                def emit_down(gi, grp):
                    nfi = len(grp)
                    for j in range(4):
                        for half in range(2):
                            pso, pok = po()
                            for fi, f in enumerate(grp):
                                hs, dv, dkey = info[f]
                                P.op("pe", lambda e: e.matmul(pso[:, :], lhsT=h1T[:, hs, j * 128:(j + 1) * 128],
                                                              rhs=dv[:, half * 512:(half + 1) * 512],
                                                              start=(fi == 0), stop=(fi == nfi - 1)), r=[("h1T", hs), dkey], w=[pok])
                            av = ac[:, j, half * 512:(half + 1) * 512]
                            gap = tb[:, j, 1:2]
                            if gi == 0:
                                P.op("dve", lambda e: e.tensor_scalar(out=av, in0=pso[:, :], scalar1=gap, scalar2=None, op0=ALU.mult),
                                     r=[pok, tbk], w=[ack])
                            else:
                                P.op("dve", lambda e: e.scalar_tensor_tensor(out=av, in0=pso[:, :], scalar=gap, in1=av,
                                                                             op0=ALU.mult, op1=ALU.add), r=[pok, tbk, ack], w=[ack])

                for gi, grp in enumerate(FGROUPS):
                    for k, f in enumerate(grp):
                        if k == 0 and gi > 0:
                            continue
                        emit_gu(f)
                    if gi + 1 < len(FGROUPS):
                        emit_gu(FGROUPS[gi + 1][0])
                    if gi == 1 and i + 1 < NTL:
                        transposes(i + 1)
                    emit_down(gi, grp)
                P.op("sp", lambda e: e.dma_start(out=yslot_d[i * 512:(i + 1) * 512, :].rearrange("(j p) d -> p j d", p=128), in_=ac),
                     r=[ack], w=[("yslot", i)], chan=c_ys[b])
            P.barrier()
            for r in [nr - 1] + list(range(nr - 1)):
                reset_arena(base0)
                ya = [alloc([1024], F32) for _ in range(2)]
                yb = [alloc([1024], F32) for _ in range(2)]
                c_ya = [P.new_chan(f"ya{i}") for i in range(2)]
                c_yb = [P.new_chan(f"yb{i}") for i in range(2)]
                srcp = hpark_d[r * RANGE:(r + 1) * RANGE, :].rearrange("(s p) d -> p s d", p=128)
                for q in range(4):
                    if r == nr - 1:
                        break
                    P.op("sp", lambda e: e.dma_start(out=h_t[:, q * 4:(q + 1) * 4, :], in_=srcp[:, q * 4:(q + 1) * 4, :]),
                         w=[hkey(s) for s in range(q * 4, q * 4 + 4)], chan=c_xs[q])
                for s in range(NT):
                    b = s % 2
                    S = r * NT + s
                    for yy, si, cc, nm in ((ya[b], slotA_i, c_ya[b], "ya"), (yb[b], slotB_i, c_yb[b], "yb")):
                        P.op("pool", lambda e: e.indirect_dma_start(
                            out=yy, out_offset=None, in_=yslot_d, in_offset=bass.IndirectOffsetOnAxis(ap=si[:, S:S + 1], axis=0)),
                            r=["slotA_i", "slotB_i"], w=[(nm, b)], chan=cc)
                        P.op("dve", lambda e: e.tensor_tensor(out=h_t[:, s, :], in0=h_t[:, s, :], in1=yy, op=ALU.add),
                             r=[(nm, b), hkey(s)], w=[hkey(s)])
                P.barrier()
                final_norm(r)

        def final_norm(r):
            reset_arena()
            junk = alloc([1024], BF16)
            ot = [alloc([1024], F32) for _ in range(2)]
            for s in range(NT):
                src, skey = h_t[:, s, :], hkey(s)
                col = (s % 4) * 4
                rstd_of(src, skey, junk, col, eps6, "eps6")
                o, ok = ot[s % 2], ("ot", s % 2)
                P.op("act", lambda e, src=src, o=o, col=col: e.activation(out=o, in_=src, func=AF.Identity, scale=stat[:, col + 2:col + 3]),
                     r=[skey, ("stat", col + 2)], w=[ok])
                P.op("dve", lambda e, o=o: e.tensor_tensor(out=o, in0=o, in1=gfin[:], op=ALU.mult), r=[ok, "gfin"], w=[ok])
                row = r * RANGE + s * 128
                P.op("sp", lambda e, o=o, row=row: e.dma_start(out=y_d[row:row + 128, :], in_=o), r=[ok], chan=c_outs[s % 2])
            P.barrier()

        def dump_h(r):
            dst = y_d[r * RANGE:(r + 1) * RANGE, :].rearrange("(s p) d -> p s d", p=128)
            for q in range(4):
                P.op("sp", lambda e, q=q: e.dma_start(out=dst[:, q * 4:(q + 1) * 4, :], in_=h_t[:, q * 4:(q + 1) * 4, :]),
                     r=[hkey(s) for s in range(q * 4, q * 4 + 4)], chan=c_out)
            P.barrier()

        phases = [("mix0", lambda r: conv_mixer(r)), ("xa0", lambda r: xattn(r, 0)), ("ffn0", lambda r: ffn_dense(r)),
                  ("mix1", lambda r: gmlp(r)), ("xa1", lambda r: xattn(r, 1)), ("ffn1", lambda r: (moe_sparse(r) if SPARSE else moe(r)))]
        P.barrier()
        combined = SPARSE and COMBINED and stop is None and only is None
        for r in range(nranges):
            load_x(r)
            stopped = False
            for name, fn in phases:
                if only is not None and name not in only:
                    continue
                if combined and name == "ffn1":
                    moe2_stage1(r)
                    continue
                fn(r)
                if stop == name:
                    stopped = True
                    break
            if combined:
                continue
            if stopped:
                dump_h(r)
            else:
                final_norm(r)
        if combined:
            moe2_rest(nranges)

        with nc.Block() as block:
            @block.tensor
            def _(e):
                P.replay("pe", e)

            @block.scalar
            def _(e):
                P.replay("act", e)

            @block.vector
            def _(e):
                P.replay("dve", e)

            @block.gpsimd
            def _(e):
                P.replay("pool", e)

            @block.sync
            def _(e):
                P.replay("sp", e)
    return nc


def _chunks(W):
    K, N = W.shape
    kc, ncn = K // 128, N // 128
    return np.ascontiguousarray(W.reshape(kc, 128, ncn, 128).transpose(2, 1, 0, 3)).reshape(ncn, 128, kc * 128)


def _cols(v):
    return np.ascontiguousarray(v.reshape(-1, 128).T)


def _rep(v):
    v = np.asarray(v).reshape(-1)
    return np.ascontiguousarray(np.broadcast_to(v[None, :], (128, v.shape[0])))


def prepare_inputs(inp):
    f = lambda a: np.ascontiguousarray(np.asarray(a, dtype=np.float32))
    x = f(inp["x"])
    mem = f(inp["mem"])
    shared = {}
    g = [inp["norm_mix_g"][0], inp["norm_xattn_g"][0], inp["norm_mem_g"][0], inp["norm_ffn_g"][0],
         inp["norm_mix_g"][1], inp["norm_xattn_g"][1], inp["norm_mem_g"][1], inp["norm_ffn_g"][1]]
    shared["gcols"] = np.ascontiguousarray(np.concatenate([_cols(f(v)) for v in g], axis=1))
    shared["gfin"] = _rep(f(inp["final_norm_g"]))
    shared["ident"] = np.eye(128, dtype=np.float32)
    shared["tri"] = np.triu(np.ones((128, 128), np.float32), 1)
    shared["cvin"] = _chunks(f(inp["cv_w_in"][0]))
    aw = f(inp["cv_a_conv_w"][0])
    bw = f(inp["cv_b_conv_w"][0])
    cvsm = np.zeros((128, 148), np.float32)
    cvsm[:, 0:124] = aw.reshape(31, 4, 128).transpose(2, 1, 0).reshape(128, 124)
    cvsm[:, 124:128] = _cols(f(inp["cv_a_conv_b"][0]))
    cvsm[:, 128:132] = _cols(f(inp["cv_a_ln_g"][0]))
    cvsm[:, 132:136] = _cols(f(inp["cv_a_ln_b"][0]))
    cvsm[:, 136:148] = bw.reshape(3, 4, 128).transpose(2, 1, 0).reshape(128, 12)
    shared["cvsm"] = cvsm
    shared["cvout"] = f(inp["cv_w_out"][0])
    for i in range(2):
        shared[f"xq{i}"] = _chunks(f(inp["xa_w_q"][i]))
        shared[f"xk{i}"] = _chunks(f(inp["xa_w_k"][i]))
        shared[f"xv{i}"] = f(inp["xa_w_v"][i])
        shared[f"xo{i}"] = f(inp["xa_w_o"][i])
    shared["fg"] = _chunks(f(inp["ffn_w_gate"][0]))
    shared["fu"] = _chunks(f(inp["ffn_w_up"][0]))
    shared["fd"] = f(inp["ffn_w_down"][0])
    sgin = f(inp["sg_w_in"][0])
    shared["sgu"] = _chunks(sgin[:, 0:1024])
    shared["sgv"] = np.ascontiguousarray(sgin[:, 1024:2048])
    shared["sglng"] = _rep(f(inp["sg_ln_g"][0]))
    shared["sglnb"] = _rep(f(inp["sg_ln_b"][0]))
    ws = f(inp["sg_w_s"][0])
    shared["sgws"] = np.ascontiguousarray(ws.transpose(2, 0, 1)).reshape(128, 1024)
    shared["sgbs"] = _rep(f(inp["sg_b_s"][0]))
    shared["sgout"] = f(inp["sg_w_out"][0])
    shared["mr"] = np.ascontiguousarray(f(inp["moe_w_router"][0]).reshape(8, 128, 8).transpose(1, 0, 2)).reshape(128, 64)
    mg = f(inp["moe_w_gate"][0])
    mu = f(inp["moe_w_up"][0])
    shared["mg"] = np.concatenate([_chunks(mg[e]) for e in range(NE)], axis=0)
    shared["mu"] = np.concatenate([_chunks(mu[e]) for e in range(NE)], axis=0)
    shared["md"] = f(inp["moe_w_down"][0]).reshape(NE * DFF, D)
    in_maps = []
    for c in range(NCORES):
        b, hf = c // 2, c % 2
        t0 = hf * TOK_CORE
        m = dict(shared)
        m["x"] = np.ascontiguousarray(x[b, t0:t0 + TOK_CORE])
        xh = np.zeros((2, 128, D), np.float32)
        for r in range(2):
            s = t0 + r * RANGE
            if s >= 128:
                xh[r] = x[b, s - 128:s]
        m["xh"] = xh
        m["mem"] = mem[b]
        in_maps.append(m)
    return in_maps


_NC_CACHE = {}


def kernel(**inputs):
    in_maps = prepare_inputs(inputs)
    if "nc" not in _NC_CACHE:
        _NC_CACHE["nc"] = build_program()
    nc = _NC_CACHE["nc"]
    res = run_bass_kernel_spmd(nc, in_maps, core_ids=list(range(NCORES)))
    out = np.empty((4, SEQ, D), np.float32)
    for c in range(NCORES):
        b, hf = c // 2, c % 2
        out[b, hf * TOK_CORE:(hf + 1) * TOK_CORE] = res.results[c]["y"]
    return out
```

```python
import contextlib
import types
import numpy as np
import concourse.bass as bass
import concourse.mybir as mybir
from concourse.bass_utils import run_bass_kernel_spmd

F32 = mybir.dt.float32
BF16 = mybir.dt.bfloat16
I32 = mybir.dt.int32
AX = mybir.AxisListType
AF = mybir.ActivationFunctionType
ALU = mybir.AluOpType

NCORES = 8
D = 1024
SEQ = 8192
TOK_CORE = 4096
RANGE = 2048
NT = RANGE // 128
NG = RANGE // 512
DFF = 2816
NF = DFF // 128
NE = 8
FGROUPS = [list(range(0, 8)), list(range(8, 15)), list(range(15, 22))]
HALO = 128
SPARSE = True
COMBINED = True
ARENA = 65536

ENGS = ("pe", "act", "dve", "pool", "sp")


def _snap(fn):
    if fn is None or fn.__closure__ is None:
        return fn
    cells = []
    for c in fn.__closure__:
        try:
            cells.append(types.CellType(c.cell_contents))
        except ValueError:
            cells.append(c)
    return types.FunctionType(fn.__code__, fn.__globals__, fn.__name__, fn.__defaults__, tuple(cells))


class Chan:
    def __init__(self, h):
        self.h = h
        self.count = 0


class Prog:
    def __init__(self, nc, stack):
        self.nc = nc
        self.stack = stack
        self.streams = {e: [] for e in ENGS}
        self.chan = {e: Chan(stack.enter_context(nc.semaphore("c_" + e))) for e in ENGS}
        self.allchans = list(self.chan.values())
        self.engchans = set(self.chan.values())
        self.seen = {e: {} for e in ENGS}
        self.lastw = {}
        self.readers = {}
        self.nsem = 0
        self.named = {}

    def new_chan(self, name):
        if name in self.named:
            return self.named[name]
        c = Chan(self.stack.enter_context(self.nc.semaphore("d_" + name)))
        self.allchans.append(c)
        self.named[name] = c
        return c

    def op(self, eng, fn, r=(), w=(), chan=None):
        need = {}

        def add(ch, val):
            if eng == "pe" and ch is self.chan["pe"]:
                return
            if ch not in self.engchans:
                val = ch.count
            if self.seen[eng].get(ch, 0) >= val:
                return
            if need.get(ch, 0) < val:
                need[ch] = val

        for k in r:
            t = self.lastw.get(k)
            if t:
                add(*t)
        for k in w:
            t = self.lastw.get(k)
            if t:
                add(*t)
            for ch, val in self.readers.get(k, {}).items():
                add(ch, val)
        for ch, val in need.items():
            self.seen[eng][ch] = val
        if chan is None:
            ch = self.chan[eng]
            ch.count += 1
            inc = 1
        else:
            ch = chan
            ch.count += 16
            inc = 16
        tok = (ch, ch.count)
        self.streams[eng].append((list(need.items()), _snap(fn), ch.h, inc))
        for k in r:
            d = self.readers.setdefault(k, {})
            d[ch] = ch.count
        for k in w:
            self.lastw[k] = tok
            self.readers[k] = {}
        return tok

    def barrier(self):
        for e in ENGS:
            waits = []
            for ch in self.allchans:
                if ch.count > 0 and self.seen[e].get(ch, 0) < ch.count:
                    if e == "pe" and ch is self.chan["pe"]:
                        continue
                    waits.append((ch, ch.count))
                    self.seen[e][ch] = ch.count
            if waits:
                self.streams[e].append((waits, None, None, 0))
        self.lastw = {}
        self.readers = {}

    def replay(self, eng, e):
        for waits, fn, semh, inc in self.streams[eng]:
            for ch, val in waits:
                e.wait_ge(ch.h, val)
            if fn is not None:
                fn(e).then_inc(semh, inc)


class Ring:
    def __init__(self, P, name, views):
        self.views = views
        self.n = len(views)
        self.chans = [P.new_chan(f"{name}{i}") for i in range(self.n)]
        self.chans2 = [P.new_chan(f"{name}b{i}") for i in range(self.n)]
        self.i = 0
        self.name = name

    def next(self):
        s = self.i % self.n
        self.i += 1
        self.last2 = self.chans2[s]
        return s, self.views[s], self.chans[s], (self.name, s)


def build_program(stop=None, nranges=2, only=None):
    nc = bass.Bass("TRN2", target_bir_lowering=False)

    def din(name, shape):
        return nc.dram_tensor(name, list(shape), F32, kind="ExternalInput").ap()

    x_d = din("x", [TOK_CORE, D])
    xh_d = din("xh", [2, 128, D])
    mem_d = din("mem", [256, D])
    gcols_d = din("gcols", [128, 64])
    gfin_d = din("gfin", [128, D])
    ident_d = din("ident", [128, 128])
    tri_d = din("tri", [128, 128])
    cvin_d = din("cvin", [20, 128, 1024])
    cvsm_d = din("cvsm", [128, 148])
    cvout_d = din("cvout", [D, D])
    xq_d = [din(f"xq{i}", [8, 128, 1024]) for i in range(2)]
    xk_d = [din(f"xk{i}", [8, 128, 1024]) for i in range(2)]
    xv_d = [din(f"xv{i}", [D, D]) for i in range(2)]
    xo_d = [din(f"xo{i}", [D, D]) for i in range(2)]
    fg_d = din("fg", [NF, 128, 1024])
    fu_d = din("fu", [NF, 128, 1024])
    fd_d = din("fd", [DFF, D])
    sgu_d = din("sgu", [8, 128, 1024])
    sgv_d = din("sgv", [D, D])
    sglng_d = din("sglng", [128, D])
    sglnb_d = din("sglnb", [128, D])
    sgws_d = din("sgws", [128, 1024])
    sgbs_d = din("sgbs", [128, 1024])
    sgout_d = din("sgout", [D, D])
    mr_d = din("mr", [128, 64])
    mg_d = din("mg", [NE * NF, 128, 1024])
    mu_d = din("mu", [NE * NF, 128, 1024])
    md_d = din("md", [NE * DFF, D])
    y_d = nc.dram_tensor("y", [TOK_CORE, D], F32, kind="ExternalOutput").ap()
    NSLOT = 12288
    kt_d = [nc.dram_tensor(f"kt_scr{i}", [128, 2048], BF16, kind="Internal").ap() for i in range(2)]
    v_d = [nc.dram_tensor(f"v_scr{i}", [128, 2048], BF16, kind="Internal").ap() for i in range(2)]
    hn_d = nc.dram_tensor("hn_scr", [TOK_CORE, D], BF16, kind="Internal").ap()
    hpark_d = nc.dram_tensor("hpark", [TOK_CORE, D], F32, kind="Internal").ap()
    slot_tab_d = nc.dram_tensor("slot_tab", [NSLOT, 16], I32, kind="Internal").ap()
    yslot_d = nc.dram_tensor("yslot", [NSLOT, D], F32, kind="Internal").ap()
    mg_flat = mg_d.rearrange("c p n -> (c p) n")
    mu_flat = mu_d.rearrange("c p n -> (c p) n")

    stack = contextlib.ExitStack()
    with stack:
        def sb(name, shape, dt):
            return stack.enter_context(nc.sbuf_tensor(name, list(shape), dt))

        h_t = sb("h", [128, NT, D], F32)
        arena = sb("arena", [128, ARENA], BF16)
        identb = sb("identb", [128, 128], BF16)
        identf = sb("identf", [128, 128], F32)
        onesb = sb("onesb", [128, 128], BF16)
        onesm = sb("onesm", [128, 128], BF16)
        trib = sb("trib", [128, 128], BF16)
        gcols = sb("gcols_s", [128, 64], F32)
        gfin = sb("gfin_s", [128, D], F32)
        eps6 = sb("eps6", [128, 1], F32)
        eps5 = sb("eps5", [128, 1], F32)
        stat = sb("stat", [128, 64], F32)
        gates_g = sb("gates_g", [128, 2 * NT, 8], F32)[:, :, :]
        sel_g = sb("sel_g", [128, 2 * NT, 8], F32)[:, :, :]
        slotA_ig = sb("slotA_ig", [128, 2 * NT], I32)[:, :]
        slotB_ig = sb("slotB_ig", [128, 2 * NT], I32)[:, :]
        widx_g = sb("widx_g", [128, 24, NF], I32)[:, :, :]
        psT_t = stack.enter_context(nc.psum_tensor("psT", [128, 1024], F32))
        psM_t = [stack.enter_context(nc.psum_tensor(f"psM{i}", [128, 512], F32)) for i in range(4)]
        psO_t = [stack.enter_context(nc.psum_tensor(f"psO{i}", [128, 512], F32)) for i in range(2)]

        P = Prog(nc, stack)
        c_const = P.new_chan("const")
        c_constsw = P.new_chan("constsw")
        c_xs = [P.new_chan(f"x{q}") for q in range(4)]
        c_outs = [P.new_chan(f"out{q}") for q in range(2)]
        c_out = c_outs[0]
        c_ph = P.new_chan("ph")
        c_phsw = P.new_chan("phsw")

        psTb = psT_t[:, :].bitcast(BF16).rearrange("p (a b) -> p a b", a=8)
        psTf = psT_t[:, :].rearrange("p (a b) -> p a b", a=8)

        state = {"off": 0, "pm": 0, "po": 0, "pp": 0, "nrm": 0, "po4": 0}

        def reset_arena(off=0):
            state["off"] = off

        def alloc(shape, dt):
            n = int(np.prod(shape))
            wide = dt in (F32, I32)
            ne = n * (2 if wide else 1)
            ne = (ne + 31) // 32 * 32
            off = state["off"]
            assert off + ne <= ARENA, f"arena overflow {off}+{ne}"
            state["off"] = off + ne
            v = arena[:, off:off + ne]
            if wide:
                v = v.bitcast(dt)
            v = v[:, 0:n]
            if len(shape) == 1:
                return v
            if len(shape) == 2:
                return v.rearrange("p (a b) -> p a b", a=shape[0])
            if len(shape) == 3:
                return v.rearrange("p (a b c) -> p a b c", a=shape[0], b=shape[1])
            raise ValueError

        def pm():
            i = state["pm"] % 4
            state["pm"] += 1
            return psM_t[i], ("psM", i)

        def pm_pair():
            i = 2 * (state["pp"] % 2)
            state["pp"] += 1
            return (psM_t[i], ("psM", i)), (psM_t[i + 1], ("psM", i + 1))

        def po():
            i = state["po"] % 2
            state["po"] += 1
            return psO_t[i], ("psO", i)

        def po4():
            i = state["po4"] % 4
            state["po4"] += 1
            if i < 2:
                return psO_t[i], ("psO", i)
            return psM_t[i], ("psM", i)

        P.op("pool", lambda e: e.dma_start(out=identb[:], in_=ident_d), w=["identb"], chan=c_constsw)
        P.op("sp", lambda e: e.dma_start(out=identf[:], in_=ident_d), w=["identf"], chan=c_const)
        P.op("pool", lambda e: e.dma_start(out=trib[:], in_=tri_d), w=["trib"], chan=c_constsw)
        P.op("sp", lambda e: e.dma_start(out=gcols[:], in_=gcols_d), w=["gcols"], chan=c_const)
        P.op("sp", lambda e: e.dma_start(out=gfin[:], in_=gfin_d), w=["gfin"], chan=c_const)
        P.op("dve", lambda e: e.memset(onesb[:], 1.0), w=["onesb"])
        P.op("dve", lambda e: e.memset(onesm[:], 1.0 / 512.0), w=["onesm"])
        P.op("dve", lambda e: e.memset(eps6[:], 1e-6), w=["eps6"])
        P.op("dve", lambda e: e.memset(eps5[:], 1e-5), w=["eps5"])

        def hkey(s):
            return ("h", s)

        def rstd_of(src_ap, src_key, junk, col, eps_t, eps_key):
            P.op("act", lambda e: e.activation(out=junk, in_=src_ap, func=AF.Square,
                                               accum_out=stat[:, col:col + 1]),
                 r=[src_key], w=["junk", ("stat", col)])
            P.op("act", lambda e: e.activation(out=stat[:, col + 1:col + 2], in_=stat[:, col:col + 1],
                                               func=AF.Sqrt, scale=1.0 / D, bias=eps_t[:, 0:1]),
                 r=[("stat", col), eps_key], w=[("stat", col + 1)])
            P.op("dve", lambda e: e.reciprocal(out=stat[:, col + 2:col + 3], in_=stat[:, col + 1:col + 2]),
                 r=[("stat", col + 1)], w=[("stat", col + 2)])

        def norm_tiles(srcs, gidx, hnT, hnT_keyf, hn_bufs, junk, col0):
            n = len(srcs)
            base = 32 * (state["nrm"] % 2)
            state["nrm"] += 1
            sk = ("statg", base)
            for j in range(n):
                src, skey = srcs[j]
                P.op("act", lambda e: e.activation(out=junk, in_=src, func=AF.Square, accum_out=stat[:, base + j:base + j + 1]),
                     r=[skey], w=["junk", (sk, "ss", j)])
            P.op("act", lambda e: e.activation(out=stat[:, base + 8:base + 8 + n], in_=stat[:, base:base + n],
                                               func=AF.Sqrt, scale=1.0 / D, bias=eps6[:, 0:1]),
                 r=[(sk, "ss", j) for j in range(n)] + ["eps6"], w=[(sk, "sd")])
            P.op("dve", lambda e: e.reciprocal(out=stat[:, base + 16:base + 16 + n], in_=stat[:, base + 8:base + 8 + n]),
                 r=[(sk, "sd")], w=[(sk, "rs")])
            for j0 in range(0, n, 2):
                pair = list(range(j0, min(j0 + 2, n)))
                for j in pair:
                    src, skey = srcs[j]
                    hb, hbk = hn_bufs[j % 2]
                    P.op("act", lambda e: e.activation(out=hb, in_=src, func=AF.Identity, scale=stat[:, base + 16 + j:base + 17 + j]),
                         r=[skey, (sk, "rs")], w=[hbk])
                    for kc in range(8):
                        jj = j - j0
                        P.op("pe", lambda e: e.transpose(out=psTb[:, kc, jj * 128:(jj + 1) * 128], in_=hb[:, kc * 128:(kc + 1) * 128],
                                                         identity=identb[:]), r=[hbk, "identb"], w=[("psT", jj)])
                w = len(pair) * 128
                c0 = j0 * 128
                P.op("dve", lambda e: e.tensor_tensor(
                    out=hnT[:, :, c0:c0 + w], in0=psTb[:, :, 0:w],
                    in1=gcols[:, gidx * 8:(gidx + 1) * 8].unsqueeze(2).to_broadcast([128, 8, w]), op=ALU.mult),
                    r=[("psT", jj) for jj in range(len(pair))] + ["gcols"],
                    w=[hnT_keyf(j) for j in pair])

        def load_resident(dst, src_ap, key):
            P.op("pool", lambda e: e.dma_start(out=dst, in_=src_ap), w=[key], chan=c_phsw)

        def nat_view(w_d, kc=8):
            return w_d.rearrange("(kc p) n -> p kc n", p=128)

        def load_x(r):
            src = x_d[r * RANGE:(r + 1) * RANGE, :].rearrange("(s p) d -> p s d", p=128)
            for q in range(4):
                P.op("sp", lambda e, q=q: e.dma_start(out=h_t[:, q * 4:(q + 1) * 4, :], in_=src[:, q * 4:(q + 1) * 4, :]),
                     w=[hkey(s) for s in range(q * 4, q * 4 + 4)], chan=c_xs[q])

        def conv_mixer(r):
            reset_arena()
            wout = alloc([8, 1024], BF16)
            diagA = alloc([4, 31, 128], BF16)
            diagB = alloc([4, 3, 128], BF16)
            wc = Ring(P, "wc", [alloc([128 * 8], BF16) for _ in range(4)])
            hn_bufs = [(alloc([1024], BF16), ("hn", i)) for i in range(2)]
            junk = alloc([1024], BF16)
            hnT = alloc([8, 512], BF16)
            aT = alloc([4, HALO + 512], BF16)
            pT = alloc([4, HALO + 512], BF16)
            gbT = alloc([4, 512], BF16)
            sig = [alloc([512], F32) for _ in range(2)]
            gct = [alloc([512], F32) for _ in range(2)]
            cA = alloc([4, 512], BF16)
            sq = alloc([4, 512], BF16)
            mean_sb = alloc([512], F32)
            var_sb = alloc([512], F32)
            rstd_sb = alloc([512], F32)
            t1 = [alloc([512], F32) for _ in range(2)]
            mixT = alloc([8, 512], BF16)
            xht = alloc([1024], F32)
            cvsm = alloc([148], F32)
            OFF_AW, OFF_AB, OFF_LG, OFF_LB, OFF_BW = 0, 124, 128, 132, 136

            P.op("sp", lambda e: e.dma_start(out=cvsm, in_=cvsm_d), w=["cvsm"], chan=c_ph)
            P.op("sp", lambda e: e.dma_start(out=xht, in_=xh_d[r]), w=["xht"], chan=c_ph)
            load_resident(wout, nat_view(cvout_d), "wout")
            def build_diags():
                for c in range(4):
                    for k in range(31):
                        P.op("dve", lambda e, c=c, k=k: e.tensor_scalar(
                            out=diagA[:, c, k, :], in0=identf[:], scalar1=cvsm[:, OFF_AW + c * 31 + k:OFF_AW + c * 31 + k + 1],
                            scalar2=None, op0=ALU.mult), r=["identf", "cvsm"], w=[("diagA", c)])
                    for k in range(3):
                        P.op("dve", lambda e, c=c, k=k: e.tensor_scalar(
                            out=diagB[:, c, k, :], in0=identf[:], scalar1=cvsm[:, OFF_BW + c * 3 + k:OFF_BW + c * 3 + k + 1],
                            scalar2=None, op0=ALU.mult), r=["identf", "cvsm"], w=[("diagB", c)])

            def in_chunk(ci, ntok):
                s, view, ch, key = wc.next()
                P.op("pool", lambda e: e.dma_start(out=view, in_=cvin_d[ci]), w=[key], chan=ch)
                ps, pk = pm()
                rk = [("hnTt", j) for j in range(max(1, ntok // 128))]
                for kc in range(8):
                    P.op("pe", lambda e, kc=kc: e.matmul(ps[:, 0:ntok], lhsT=view[:, kc * 128:(kc + 1) * 128],
                                                         rhs=hnT[:, kc, 0:ntok], start=(kc == 0), stop=(kc == 7)),
                         r=[key] + rk, w=[pk])
                return ps, pk

            def in_proj(ntok, col0, with_gb):
                for c in range(4):
                    pv, pvk = in_chunk(c, ntok)
                    pg, pgk = in_chunk(4 + c, ntok)
                    sg_, sgk = sig[c % 2], ("sig", c % 2)
                    P.op("act", lambda e, pg=pg, sg_=sg_: e.activation(out=sg_[:, 0:ntok], in_=pg[:, 0:ntok], func=AF.Sigmoid),
                         r=[pgk], w=[sgk])
                    P.op("dve", lambda e, pv=pv, sg_=sg_, c=c: e.tensor_tensor(
                        out=aT[:, c, col0:col0 + ntok], in0=pv[:, 0:ntok], in1=sg_[:, 0:ntok], op=ALU.mult),
                        r=[pvk, sgk], w=[("aT", c)])
                for c in range(4):
                    pgc, pgck = in_chunk(12 + c, ntok)
                    phb, phbk = in_chunk(16 + c, ntok)
                    g_, gk = gct[c % 2], ("gct", c % 2)
                    P.op("act", lambda e, pgc=pgc, g_=g_: e.activation(out=g_[:, 0:ntok], in_=pgc[:, 0:ntok], func=AF.Identity),
                         r=[pgck], w=[gk])
                    P.op("dve", lambda e, phb=phb, g_=g_, c=c: e.tensor_tensor(
                        out=pT[:, c, col0:col0 + ntok], in0=phb[:, 0:ntok], in1=g_[:, 0:ntok], op=ALU.mult),
                        r=[phbk, gk], w=[("pT", c)])
                if with_gb:
                    for c in range(4):
                        pgb, pgbk = in_chunk(8 + c, ntok)
                        P.op("act", lambda e, pgb=pgb, c=c: e.activation(out=gbT[:, c, 0:ntok], in_=pgb[:, 0:ntok], func=AF.Identity),
                             r=[pgbk], w=[("gbT", c)])

            def norm_into(srcs):
                norm_tiles(srcs, 0, hnT, lambda j: ("hnTt", j), hn_bufs, junk, 0)

            norm_into([(xht, "xht")])
            in_proj(128, 0, False)

            def normin(g):
                srcs = [(h_t[:, g * 4 + j, :], hkey(g * 4 + j)) for j in range(4)]
                norm_into(srcs)
                in_proj(512, HALO, True)

            normin(0)
            build_diags()
            for g in range(NG):
                for c in range(4):
                    ps, pk = pm()
                    for k in range(31):
                        o = HALO - 30 + k
                        P.op("pe", lambda e, c=c, k=k, o=o, ps=ps: e.matmul(
                            ps[:, :], lhsT=diagA[:, c, k, :], rhs=aT[:, c, o:o + 512], start=(k == 0), stop=(k == 30)),
                            r=[("diagA", c), ("aT", c)], w=[pk])
                    P.op("act", lambda e, c=c, ps=ps: e.activation(out=cA[:, c, :], in_=ps[:, :], func=AF.Identity,
                                                                   bias=cvsm[:, OFF_AB + c:OFF_AB + c + 1]),
                         r=[pk, "cvsm"], w=[("cA", c)])
                    P.op("act", lambda e, c=c, ps=ps: e.activation(out=sq[:, c, :], in_=ps[:, :], func=AF.Square,
                                                                   bias=cvsm[:, OFF_AB + c:OFF_AB + c + 1]),
                         r=[pk, "cvsm"], w=[("sq", c)])
                pmean, pmk = pm()
                for c in range(4):
                    P.op("pe", lambda e, c=c: e.matmul(pmean[:, :], lhsT=onesm[:], rhs=cA[:, c, :], start=(c == 0), stop=(c == 3)),
                         r=["onesm", ("cA", c)], w=[pmk])
                pex, pexk = pm()
                for c in range(4):
                    P.op("pe", lambda e, c=c: e.matmul(pex[:, :], lhsT=onesm[:], rhs=sq[:, c, :], start=(c == 0), stop=(c == 3)),
                         r=["onesm", ("sq", c)], w=[pexk])
                P.op("act", lambda e: e.activation(out=mean_sb, in_=pmean[:, :], func=AF.Identity), r=[pmk], w=["mean_sb"])
                P.op("dve", lambda e: e.tensor_tensor(out=var_sb, in0=mean_sb, in1=mean_sb, op=ALU.mult), r=["mean_sb"], w=["var_sb"])
                P.op("dve", lambda e: e.tensor_tensor(out=var_sb, in0=pex[:, :], in1=var_sb, op=ALU.subtract), r=[pexk, "var_sb"], w=["var_sb"])
                P.op("act", lambda e: e.activation(out=var_sb, in_=var_sb, func=AF.Sqrt, bias=eps5[:, 0:1]), r=["var_sb", "eps5"], w=["var_sb"])
                P.op("dve", lambda e: e.reciprocal(out=rstd_sb, in_=var_sb), r=["var_sb"], w=["rstd_sb"])
                for c in range(4):
                    tt, tk = t1[c % 2], ("t1", c % 2)
                    P.op("dve", lambda e, c=c, tt=tt: e.tensor_tensor(out=tt, in0=cA[:, c, :], in1=mean_sb, op=ALU.subtract),
                         r=[("cA", c), "mean_sb"], w=[tk])
                    P.op("dve", lambda e, tt=tt: e.tensor_tensor(out=tt, in0=tt, in1=rstd_sb, op=ALU.mult), r=[tk, "rstd_sb"], w=[tk])
                    P.op("act", lambda e, c=c, tt=tt: e.activation(out=mixT[:, c, :], in_=tt, func=AF.Silu,
                                                                   scale=cvsm[:, OFF_LG + c:OFF_LG + c + 1],
                                                                   bias=cvsm[:, OFF_LB + c:OFF_LB + c + 1]),
                         r=[tk, "cvsm"], w=[("mixT", c)])
                for c in range(4):
                    ps, pk = pm()
                    for k in range(3):
                        o = HALO - 2 + k
                        P.op("pe", lambda e, c=c, k=k, o=o, ps=ps: e.matmul(
                            ps[:, :], lhsT=diagB[:, c, k, :], rhs=pT[:, c, o:o + 512], start=(k == 0), stop=(k == 2)),
                            r=[("diagB", c), ("pT", c)], w=[pk])
                    P.op("dve", lambda e, c=c, ps=ps: e.tensor_tensor(out=mixT[:, 4 + c, :], in0=ps[:, :], in1=gbT[:, c, :], op=ALU.mult),
                         r=[pk, ("gbT", c)], w=[("mixT", 4 + c)])
                for c in range(4):
                    P.op("dve", lambda e, c=c: e.tensor_copy(out=aT[:, c, 0:HALO], in_=aT[:, c, 512:512 + HALO]), r=[("aT", c)], w=[("aT", c)])
                    P.op("dve", lambda e, c=c: e.tensor_copy(out=pT[:, c, 0:HALO], in_=pT[:, c, 512:512 + HALO]), r=[("pT", c)], w=[("pT", c)])
                if g + 1 < NG:
                    normin(g + 1)
                for s in range(4):
                    for half in range(2):
                        pso, pok = po4()
                        for kc in range(8):
                            P.op("pe", lambda e, kc=kc, s=s, half=half, pso=pso: e.matmul(
                                pso[:, :], lhsT=mixT[:, kc, s * 128:(s + 1) * 128], rhs=wout[:, kc, half * 512:(half + 1) * 512],
                                start=(kc == 0), stop=(kc == 7)), r=[("mixT", kc), "wout"], w=[pok])
                        ti = g * 4 + s
                        P.op("dve", lambda e, ti=ti, half=half, pso=pso: e.tensor_tensor(
                            out=h_t[:, ti, half * 512:(half + 1) * 512], in0=pso[:, :], in1=h_t[:, ti, half * 512:(half + 1) * 512], op=ALU.add),
                            r=[pok, hkey(ti)], w=[hkey(ti)])
            P.barrier()

        def xattn(r, li):
            reset_arena()
            KT = alloc([8, 256], BF16)
            V = alloc([2, 1024], BF16)
            base = state["off"]
            gi_x, gi_m = (1, 2) if li == 0 else (5, 6)
            if r == 0:
                wk = alloc([8, 1024], BF16)
                wv = alloc([8, 1024], BF16)
                memx = alloc([2, 1024], F32)
                hn_bufs = [(alloc([1024], BF16), ("hn", i)) for i in range(2)]
                junk = alloc([1024], BF16)
                memT = alloc([8, 256], BF16)
                P.op("sp", lambda e: e.dma_start(out=memx, in_=mem_d.rearrange("(s p) d -> p s d", p=128)), w=["memx"], chan=c_ph)
                load_resident(wk, xk_d[li].rearrange("c p n -> p c n"), "wk")
                load_resident(wv, nat_view(xv_d[li]), "wv")
                norm_tiles([(memx[:, j, :], "memx") for j in range(2)], gi_m, memT, lambda j: "memT", hn_bufs, junk, 0)
                for oc in range(8):
                    ps, pk = pm()
                    for kc in range(8):
                        P.op("pe", lambda e, oc=oc, kc=kc, ps=ps: e.matmul(ps[:, 0:256], lhsT=wk[:, oc, kc * 128:(kc + 1) * 128],
                                                                          rhs=memT[:, kc, :], start=(kc == 0), stop=(kc == 7)),
                             r=["wk", "memT"], w=[pk])
                    P.op("act", lambda e, oc=oc, ps=ps: e.activation(out=KT[:, oc, :], in_=ps[:, 0:256], func=AF.Identity), r=[pk], w=["KT"])
                for mt in range(2):
                    for half in range(2):
                        ps, pk = pm()
                        for kc in range(8):
                            P.op("pe", lambda e, mt=mt, half=half, kc=kc, ps=ps: e.matmul(
                                ps[:, :], lhsT=memT[:, kc, mt * 128:(mt + 1) * 128], rhs=wv[:, kc, half * 512:(half + 1) * 512],
                                start=(kc == 0), stop=(kc == 7)), r=["wv", "memT"], w=[pk])
                        P.op("dve", lambda e, mt=mt, half=half, ps=ps: e.tensor_copy(out=V[:, mt, half * 512:(half + 1) * 512], in_=ps[:, :]),
                             r=[pk], w=["V"])
                P.op("sp", lambda e: e.dma_start(out=kt_d[li].rearrange("p (a b) -> p a b", a=8), in_=KT), r=["KT"], w=[("kt_d", li)], chan=c_ph)
                P.op("sp", lambda e: e.dma_start(out=v_d[li].rearrange("p (a b) -> p a b", a=2), in_=V), r=["V"], w=[("v_d", li)], chan=c_ph)
                P.barrier()
            else:
                P.op("sp", lambda e: e.dma_start(out=KT, in_=kt_d[li].rearrange("p (a b) -> p a b", a=8)), w=["KT"], chan=c_ph)
                P.op("sp", lambda e: e.dma_start(out=V, in_=v_d[li].rearrange("p (a b) -> p a b", a=2)), w=["V"], chan=c_ph)
            reset_arena(base)
            wq = alloc([8, 1024], BF16)
            wo = alloc([8, 1024], BF16)
            hn_bufs = [(alloc([1024], BF16), ("hn", i)) for i in range(2)]
            junk = alloc([1024], BF16)
            hnT2 = [alloc([8, 512], BF16) for _ in range(2)]
            qT2 = [alloc([8, 512], BF16) for _ in range(2)]
            ET = alloc([4, 2, 512], BF16)
            rden = [alloc([512], F32) for _ in range(4)]
            oT = alloc([8, 512], BF16)
            load_resident(wq, xq_d[li].rearrange("c p n -> p c n"), "wq")
            load_resident(wo, nat_view(xo_d[li]), "wo")

            def stage_a(g):
                hnT, qT, pb = hnT2[g % 2], qT2[g % 2], g % 2
                srcs = [(h_t[:, g * 4 + j, :], hkey(g * 4 + j)) for j in range(4)]
                norm_tiles(srcs, gi_x, hnT, lambda j: ("hnT", pb), hn_bufs, junk, 0)
                for oc in range(8):
                    ps, pk = pm()
                    for kc in range(8):
                        P.op("pe", lambda e: e.matmul(ps[:, :], lhsT=wq[:, oc, kc * 128:(kc + 1) * 128],
                                                      rhs=hnT[:, kc, :], start=(kc == 0), stop=(kc == 7)),
                             r=["wq", ("hnT", pb)], w=[pk])
                    P.op("act", lambda e: e.activation(out=qT[:, oc, :], in_=ps[:, :], func=AF.Identity, scale=0.0625),
                         r=[pk], w=[("qT", pb, oc)])

            def stage_b(g):
                qT, pb = qT2[g % 2], g % 2
                for hh in range(4):
                    for mc in range(2):
                        ps, pk = pm()
                        for dc in range(2):
                            P.op("pe", lambda e: e.matmul(
                                ps[:, :], lhsT=KT[:, 2 * hh + dc, mc * 128:(mc + 1) * 128], rhs=qT[:, 2 * hh + dc, :],
                                start=(dc == 0), stop=(dc == 1)), r=["KT", ("qT", pb, 2 * hh + dc)], w=[pk])
                        P.op("act", lambda e: e.activation(out=ET[:, hh, mc, :], in_=ps[:, :], func=AF.Exp),
                             r=[pk], w=[("ET", hh, mc)])
                for hh in range(4):
                    ps, pk = pm()
                    for mc in range(2):
                        P.op("pe", lambda e: e.matmul(ps[:, :], lhsT=onesb[:], rhs=ET[:, hh, mc, :],
                                                      start=(mc == 0), stop=(mc == 1)),
                             r=["onesb", ("ET", hh, mc)], w=[pk])
                    rd, rdk = rden[hh], ("rden", hh)
                    P.op("dve", lambda e: e.reciprocal(out=rd, in_=ps[:, :]), r=[pk], w=[rdk])
                for hh in range(4):
                    rd, rdk = rden[hh], ("rden", hh)
                    for dc in range(2):
                        ps2, pk2 = pm()
                        oc = 2 * hh + dc
                        for mc in range(2):
                            P.op("pe", lambda e: e.matmul(
                                ps2[:, :], lhsT=V[:, mc, oc * 128:(oc + 1) * 128], rhs=ET[:, hh, mc, :],
                                start=(mc == 0), stop=(mc == 1)), r=["V", ("ET", hh, mc)], w=[pk2])
                        P.op("dve", lambda e: e.tensor_tensor(out=oT[:, oc, :], in0=ps2[:, :], in1=rd, op=ALU.mult),
                             r=[pk2, rdk], w=[("oT", oc)])
                for s in range(4):
                    for half in range(2):
                        pso, pok = po4()
                        for kc in range(8):
                            P.op("pe", lambda e: e.matmul(
                                pso[:, :], lhsT=oT[:, kc, s * 128:(s + 1) * 128], rhs=wo[:, kc, half * 512:(half + 1) * 512],
                                start=(kc == 0), stop=(kc == 7)), r=[("oT", kc), "wo"], w=[pok])
                        ti = g * 4 + s
                        hv = h_t[:, ti, half * 512:(half + 1) * 512]
                        P.op("dve", lambda e: e.tensor_tensor(out=hv, in0=pso[:, :], in1=hv, op=ALU.add),
                             r=[pok, hkey(ti)], w=[hkey(ti)])

            stage_a(0)
            for g in range(NG):
                if g + 1 < NG:
                    stage_a(g + 1)
                stage_b(g)
            P.barrier()

        def ffn_core(hnT, h1T, gu, dr, silu_t, wg_of, wu_of, wd_of, gate_of):
            for grp in FGROUPS:
                dslots = {}
                for fi, f in enumerate(grp):
                    s, gv, gch, gkey = gu.next()
                    P.op("pool", lambda e, gv=gv, f=f: e.dma_start(out=gv[:, 0, :], in_=wg_of(f)), w=[(gkey, 0)], chan=gch)
                    P.op("pool", lambda e, gv=gv, f=f: e.dma_start(out=gv[:, 1, :], in_=wu_of(f)), w=[(gkey, 1)], chan=gu.last2)
                    s2, dv, dch, dkey = dr.next()
                    P.op("pool", lambda e, dv=dv, f=f: e.dma_start(out=dv, in_=wd_of(f)), w=[dkey], chan=dch)
                    dslots[fi] = (dv, dkey)
                    for sg in range(NG):
                        (pg, pgk), (pu, puk) = pm_pair()
                        for kc in range(8):
                            P.op("pe", lambda e, kc=kc, sg=sg, pg=pg, gv=gv: e.matmul(
                                pg[:, :], lhsT=gv[:, 0, kc * 128:(kc + 1) * 128], rhs=hnT[:, kc, sg * 512:(sg + 1) * 512],
                                start=(kc == 0), stop=(kc == 7)), r=[(gkey, 0), ("hnT", sg)], w=[pgk])
                        for kc in range(8):
                            P.op("pe", lambda e, kc=kc, sg=sg, pu=pu, gv=gv: e.matmul(
                                pu[:, :], lhsT=gv[:, 1, kc * 128:(kc + 1) * 128], rhs=hnT[:, kc, sg * 512:(sg + 1) * 512],
                                start=(kc == 0), stop=(kc == 7)), r=[(gkey, 1), ("hnT", sg)], w=[puk])
                        st_, stk = silu_t[sg % 2], ("silu", sg % 2)
                        P.op("act", lambda e, pg=pg, st_=st_: e.activation(out=st_, in_=pg[:, :], func=AF.Silu), r=[pgk], w=[stk])
                        P.op("dve", lambda e, pu=pu, st_=st_, fi=fi, sg=sg: e.tensor_tensor(
                            out=h1T[:, fi, sg * 512:(sg + 1) * 512], in0=pu[:, :], in1=st_, op=ALU.mult),
                            r=[puk, stk], w=[("h1T", fi, sg)])
                nfi = len(grp)
                for s in range(NT):
                    for half in range(2):
                        pso, pok = po()
                        for fi in range(nfi):
                            dv, dkey = dslots[fi]
                            P.op("pe", lambda e, fi=fi, s=s, half=half, pso=pso, dv=dv: e.matmul(
                                pso[:, :], lhsT=h1T[:, fi, s * 128:(s + 1) * 128], rhs=dv[:, half * 512:(half + 1) * 512],
                                start=(fi == 0), stop=(fi == nfi - 1)), r=[("h1T", fi, s // 4), dkey], w=[pok])
                        hv = h_t[:, s, half * 512:(half + 1) * 512]
                        if gate_of is None:
                            P.op("dve", lambda e, pso=pso, hv=hv: e.tensor_tensor(out=hv, in0=pso[:, :], in1=hv, op=ALU.add),
                                 r=[pok, hkey(s)], w=[hkey(s)])
                        else:
                            gap, gk = gate_of(s)
                            P.op("dve", lambda e, pso=pso, hv=hv, gap=gap: e.scalar_tensor_tensor(
                                out=hv, in0=pso[:, :], scalar=gap, in1=hv, op0=ALU.mult, op1=ALU.add),
                                r=[pok, hkey(s), gk], w=[hkey(s)])

        def ffn_alloc(r, tag):
            hnT = alloc([8, RANGE], BF16)
            h1T = alloc([8, RANGE], BF16)
            gu = Ring(P, "gu", [alloc([2, 1024], BF16) for _ in range(4)])
            dr = Ring(P, "dr", [alloc([1024], BF16) for _ in range(11)])
            silu_t = [alloc([512], BF16) for _ in range(2)]
            hn_bufs = [(alloc([1024], BF16), ("hn", i)) for i in range(2)]
            junk = alloc([1024], BF16)
            return hnT, h1T, gu, dr, silu_t, hn_bufs, junk

        def ffn_dense(r):
            reset_arena()
            hnT, h1T, gu, dr, silu_t, hn_bufs, junk = ffn_alloc(r, "f")
            for g in range(NG):
                srcs = [(h_t[:, g * 4 + j, :], hkey(g * 4 + j)) for j in range(4)]
                norm_tiles(srcs, 3, hnT[:, :, g * 512:(g + 1) * 512], lambda j, g=g: ("hnT", g), hn_bufs, junk, 0)
            ffn_core(hnT, h1T, gu, dr, silu_t,
                     lambda f: fg_d[f], lambda f: fu_d[f], lambda f: fd_d[f * 128:(f + 1) * 128, :], None)
            P.barrier()

        def gmlp(r):
            reset_arena()
            wu = alloc([8, 1024], BF16)
            wv = alloc([8, 1024], BF16)
            wout = alloc([8, 1024], BF16)
            wsT = alloc([8, 128], BF16)
            bsb = alloc([8, 128], F32)
            lng = alloc([1024], F32)
            lnb = alloc([1024], F32)
            hn_bufs = [(alloc([1024], BF16), ("hn", i)) for i in range(2)]
            junk = alloc([1024], BF16)
            hnT = alloc([8, 512], BF16)
            uT = alloc([8, 512], BF16)
            vg = [alloc([1024], F32) for _ in range(2)]
            vN = alloc([4, 1024], BF16)
            tmp = [alloc([512], F32) for _ in range(2)]
            mT = alloc([8, 512], BF16)
            bst = alloc([4, 8], F32)
            load_resident(wu, sgu_d.rearrange("c p n -> p c n"), "wu")
            load_resident(wv, nat_view(sgv_d), "wv")
            load_resident(wout, nat_view(sgout_d), "wout")
            load_resident(wsT, sgws_d.rearrange("p (g i) -> p g i", g=8), "wsT")
            P.op("sp", lambda e: e.dma_start(out=bsb, in_=sgbs_d.rearrange("p (g i) -> p g i", g=8)), w=["bsb"], chan=c_ph)
            P.op("sp", lambda e: e.dma_start(out=lng, in_=sglng_d), w=["lng"], chan=c_ph)
            P.op("sp", lambda e: e.dma_start(out=lnb, in_=sglnb_d), w=["lnb"], chan=c_ph)
            P.op("dve", lambda e: e.memset(wsT[64:128, :, 0:64], 0.0), r=[], w=["wsT"])
            bstf = bst.rearrange("p a b -> p (a b)")
            for g in range(NG):
                srcs = [(h_t[:, g * 4 + j, :], hkey(g * 4 + j)) for j in range(4)]
                norm_tiles(srcs, 4, hnT, lambda j: "hnT", hn_bufs, junk, 0)
                for s in range(4):
                    vgt, vgk = vg[s % 2], ("vg", s % 2)
                    so = (s % 2) * 16
                    for half in range(2):
                        ps, pk = pm()
                        for kc in range(8):
                            P.op("pe", lambda e, s=s, half=half, kc=kc, ps=ps: e.matmul(
                                ps[:, :], lhsT=hnT[:, kc, s * 128:(s + 1) * 128], rhs=wv[:, kc, half * 512:(half + 1) * 512],
                                start=(kc == 0), stop=(kc == 7)), r=["wv", "hnT"], w=[pk])
                        P.op("act", lambda e, half=half, ps=ps, vgt=vgt: e.activation(
                            out=vgt[:, half * 512:(half + 1) * 512], in_=ps[:, :], func=AF.Gelu_apprx_tanh), r=[pk], w=[vgk])
                        P.op("dve", lambda e, half=half, vgt=vgt, so=so: e.bn_stats(
                            out=bstf[:, so + half * 6:so + half * 6 + 6], in_=vgt[:, half * 512:(half + 1) * 512]),
                            r=[vgk], w=[("bst", s % 2)])
                    P.op("dve", lambda e, so=so: e.bn_aggr(out=bstf[:, so + 12:so + 14], in_=bstf[:, so:so + 12]),
                         r=[("bst", s % 2)], w=[("bst", s % 2)])
                    P.op("act", lambda e, so=so: e.activation(out=bstf[:, so + 14:so + 15], in_=bstf[:, so + 13:so + 14],
                                                              func=AF.Sqrt, bias=eps5[:, 0:1]),
                         r=[("bst", s % 2), "eps5"], w=[("bst", s % 2)])
                    P.op("dve", lambda e, so=so: e.reciprocal(out=bstf[:, so + 15:so + 16], in_=bstf[:, so + 14:so + 15]),
                         r=[("bst", s % 2)], w=[("bst", s % 2)])
                    P.op("dve", lambda e, so=so, vgt=vgt: e.tensor_scalar(
                        out=vgt, in0=vgt, scalar1=bstf[:, so + 12:so + 13], scalar2=bstf[:, so + 15:so + 16],
                        op0=ALU.subtract, op1=ALU.mult), r=[vgk, ("bst", s % 2)], w=[vgk])
                    P.op("pool", lambda e, vgt=vgt: e.tensor_tensor(out=vgt, in0=vgt, in1=lng, op=ALU.mult), r=[vgk, "lng"], w=[vgk])
                    P.op("pool", lambda e, vgt=vgt, s=s: e.tensor_tensor(out=vN[:, s, :], in0=vgt, in1=lnb, op=ALU.add),
                         r=[vgk, "lnb"], w=[("vN", s)])
                for oc in range(8):
                    ps, pk = pm()
                    for kc in range(8):
                        P.op("pe", lambda e, oc=oc, kc=kc, ps=ps: e.matmul(ps[:, :], lhsT=wu[:, oc, kc * 128:(kc + 1) * 128],
                                                                          rhs=hnT[:, kc, :], start=(kc == 0), stop=(kc == 7)),
                             r=["wu", "hnT"], w=[pk])
                    P.op("act", lambda e, oc=oc, ps=ps: e.activation(out=uT[:, oc, :], in_=ps[:, :], func=AF.Gelu_apprx_tanh),
                         r=[pk], w=[("uT", oc)])
                for gg in range(8):
                    ps, pk = pm()
                    for s in range(4):
                        P.op("pe", lambda e, gg=gg, s=s, ps=ps: e.matmul(
                            ps[:, s * 128:(s + 1) * 128], lhsT=vN[:, s, gg * 128:(gg + 1) * 128], rhs=wsT[:, gg, :],
                            start=True, stop=True), r=[("vN", s), "wsT"], w=[pk])
                    tt, tk = tmp[gg % 2], ("tmp", gg % 2)
                    for s in range(4):
                        P.op("dve", lambda e, gg=gg, s=s, ps=ps, tt=tt: e.tensor_tensor(
                            out=tt[:, s * 128:(s + 1) * 128], in0=ps[:, s * 128:(s + 1) * 128], in1=bsb[:, gg, :], op=ALU.add),
                            r=[pk, "bsb"], w=[tk])
                    P.op("dve", lambda e, gg=gg, tt=tt: e.tensor_tensor(out=mT[:, gg, :], in0=tt, in1=uT[:, gg, :], op=ALU.mult),
                         r=[tk, ("uT", gg)], w=[("mT", gg)])
                for s in range(4):
                    for half in range(2):
                        pso, pok = po4()
                        for kc in range(8):
                            P.op("pe", lambda e, kc=kc, s=s, half=half, pso=pso: e.matmul(
                                pso[:, :], lhsT=mT[:, kc, s * 128:(s + 1) * 128], rhs=wout[:, kc, half * 512:(half + 1) * 512],
                                start=(kc == 0), stop=(kc == 7)), r=[("mT", kc), "wout"], w=[pok])
                        ti = g * 4 + s
                        P.op("dve", lambda e, ti=ti, half=half, pso=pso: e.tensor_tensor(
                            out=h_t[:, ti, half * 512:(half + 1) * 512], in0=pso[:, :], in1=h_t[:, ti, half * 512:(half + 1) * 512], op=ALU.add),
                            r=[pok, hkey(ti)], w=[hkey(ti)])
            P.barrier()

        def moe(r):
            reset_arena()
            hnT, h1T, gu, dr, silu_t, hn_bufs, junk = ffn_alloc(r, "m")
            hn32 = [alloc([1024], F32) for _ in range(2)]
            hnT32 = [alloc([8, 128], F32) for _ in range(2)]
            wr = alloc([8, 8], F32)
            gates = alloc([NT, 8], F32)
            rt = alloc([NT, 64], F32)
            P.op("sp", lambda e: e.dma_start(out=wr, in_=mr_d.rearrange("p (k e) -> p k e", k=8)), w=["wr"], chan=c_ph)
            for s in range(NT):
                src, skey = h_t[:, s, :], hkey(s)
                col = (s % 4) * 4
                rstd_of(src, skey, junk, col, eps6, "eps6")
                hb, hbk = hn32[s % 2], ("hn32", s % 2)
                P.op("act", lambda e, src=src, hb=hb, col=col: e.activation(out=hb, in_=src, func=AF.Identity,
                                                                             scale=stat[:, col + 2:col + 3]),
                     r=[skey, ("stat", col + 2)], w=[hbk])
                for kc in range(8):
                    P.op("pe", lambda e, hb=hb, kc=kc: e.transpose(out=psTf[:, kc, :], in_=hb[:, kc * 128:(kc + 1) * 128],
                                                                  identity=identf[:]), r=[hbk, "identf"], w=["psTf"])
                ht, htk = hnT32[s % 2], ("hnT32", s % 2)
                P.op("dve", lambda e, ht=ht: e.tensor_tensor(
                    out=ht, in0=psTf, in1=gcols[:, 56:64].unsqueeze(2).to_broadcast([128, 8, 128]), op=ALU.mult),
                    r=["psTf", "gcols"], w=[htk])
                P.op("act", lambda e, ht=ht, s=s: e.activation(out=hnT[:, :, s * 128:(s + 1) * 128], in_=ht, func=AF.Identity),
                     r=[htk], w=[("hnT", s // 4)])
                ps, pk = pm()
                for kc in range(8):
                    P.op("pe", lambda e, ht=ht, kc=kc, ps=ps: e.matmul(ps[:, 0:8], lhsT=ht[:, kc, :], rhs=wr[:, kc, :],
                                                                      start=(kc == 0), stop=(kc == 7)), r=[htk, "wr"], w=[pk])
                rk = ("rt", s)
                lg = rt[:, s, 0:8]
                srt = rt[:, s, 8:16]
                nm1 = rt[:, s, 16:17]
                ex = rt[:, s, 24:32]
                sel = rt[:, s, 32:40]
                gs = rt[:, s, 40:48]
                den = rt[:, s, 48:49]
                rdn = rt[:, s, 49:50]
                P.op("dve", lambda e, lg=lg, ps=ps: e.tensor_copy(out=lg, in_=ps[:, 0:8]), r=[pk], w=[rk])
                P.op("dve", lambda e, lg=lg, srt=srt: e.max(out=srt, in_=lg), r=[rk], w=[rk])
                P.op("dve", lambda e, srt=srt, nm1=nm1: e.tensor_scalar(out=nm1, in0=srt[:, 0:1], scalar1=-1.0, scalar2=None, op0=ALU.mult),
                     r=[rk], w=[rk])
                P.op("act", lambda e, lg=lg, ex=ex, nm1=nm1: e.activation(out=ex, in_=lg, func=AF.Exp, bias=nm1), r=[rk], w=[rk])
                P.op("dve", lambda e, lg=lg, sel=sel, srt=srt: e.tensor_scalar(out=sel, in0=lg, scalar1=srt[:, 1:2], scalar2=None, op0=ALU.is_ge),
                     r=[rk], w=[rk])
                P.op("dve", lambda e, ex=ex, sel=sel, gs=gs, den=den: e.scalar_tensor_tensor(
                    out=gs, in0=ex, scalar=1.0, in1=sel, op0=ALU.mult, op1=ALU.mult, accum_out=den), r=[rk], w=[rk])
                P.op("dve", lambda e, den=den, rdn=rdn: e.reciprocal(out=rdn, in_=den), r=[rk], w=[rk])
                P.op("dve", lambda e, gs=gs, rdn=rdn, s=s: e.tensor_scalar(out=gates[:, s, :], in0=gs, scalar1=rdn, scalar2=None, op0=ALU.mult),
                     r=[rk], w=[("gates", s)])
            for ex_i in range(NE):
                ffn_core(hnT, h1T, gu, dr, silu_t,
                         lambda f, ex_i=ex_i: mg_d[ex_i * NF + f], lambda f, ex_i=ex_i: mu_d[ex_i * NF + f],
                         lambda f, ex_i=ex_i: md_d[ex_i * DFF + f * 128:ex_i * DFF + (f + 1) * 128, :],
                         lambda s, ex_i=ex_i: (gates[:, s, ex_i:ex_i + 1], ("gates", s)))
            P.barrier()


        def moe_sparse(r):
            BIG = 65536.0
            reset_arena()
            gates = alloc([NT, 8], F32)
            sel = alloc([NT, 8], F32)
            slotA_i = alloc([NT], I32)
            slotB_i = alloc([NT], I32)
            widx = alloc([16, NF], I32)
            base0 = state["off"]
            junk = alloc([1024], BF16)
            hn32 = [alloc([1024], F32) for _ in range(2)]
            hb16 = [alloc([1024], BF16) for _ in range(2)]
            hnT32 = [alloc([8, 128], F32) for _ in range(2)]
            wr = alloc([8, 8], F32)
            rt = alloc([NT, 64], F32)
            c_hn = [P.new_chan(f"hnst{i}") for i in range(2)]
            P.op("sp", lambda e: e.dma_start(out=wr, in_=mr_d.rearrange("p (k e) -> p k e", k=8)), w=["wr"], chan=c_ph)
            for s in range(NT):
                src, skey = h_t[:, s, :], hkey(s)
                col = (s % 4) * 4
                rstd_of(src, skey, junk, col, eps6, "eps6")
                hb, hbk = hn32[s % 2], ("hn32", s % 2)
                P.op("act", lambda e: e.activation(out=hb, in_=src, func=AF.Identity, scale=stat[:, col + 2:col + 3]),
                     r=[skey, ("stat", col + 2)], w=[hbk])
                h16, h16k = hb16[s % 2], ("hb16", s % 2)
                P.op("act", lambda e: e.activation(out=h16, in_=src, func=AF.Identity, scale=stat[:, col + 2:col + 3]),
                     r=[skey, ("stat", col + 2)], w=[h16k])
                P.op("sp", lambda e: e.dma_start(out=hn_d[s * 128:(s + 1) * 128, :], in_=h16), r=[h16k], w=[("hn_d", s)],
                     chan=c_hn[s % 2])
                for kc in range(8):
                    P.op("pe", lambda e: e.transpose(out=psTf[:, kc, :], in_=hb[:, kc * 128:(kc + 1) * 128], identity=identf[:]),
                         r=[hbk, "identf"], w=["psTf"])
                ht, htk = hnT32[s % 2], ("hnT32", s % 2)
                P.op("dve", lambda e: e.tensor_tensor(
                    out=ht, in0=psTf, in1=gcols[:, 56:64].unsqueeze(2).to_broadcast([128, 8, 128]), op=ALU.mult),
                    r=["psTf", "gcols"], w=[htk])
                ps, pk = pm()
                for kc in range(8):
                    P.op("pe", lambda e: e.matmul(ps[:, 0:8], lhsT=ht[:, kc, :], rhs=wr[:, kc, :], start=(kc == 0), stop=(kc == 7)),
                         r=[htk, "wr"], w=[pk])
                rk = ("rt", s)
                lg = rt[:, s, 0:8]
                srt = rt[:, s, 8:16]
                nm1 = rt[:, s, 16:17]
                ex = rt[:, s, 24:32]
                gs = rt[:, s, 40:48]
                den = rt[:, s, 48:49]
                rdn = rt[:, s, 49:50]
                selv = sel[:, s, :]
                P.op("dve", lambda e: e.tensor_copy(out=lg, in_=ps[:, 0:8]), r=[pk], w=[rk])
                P.op("dve", lambda e: e.max(out=srt, in_=lg), r=[rk], w=[rk])
                P.op("dve", lambda e: e.tensor_scalar(out=nm1, in0=srt[:, 0:1], scalar1=-1.0, scalar2=None, op0=ALU.mult), r=[rk], w=[rk])
                P.op("act", lambda e: e.activation(out=ex, in_=lg, func=AF.Exp, bias=nm1), r=[rk], w=[rk])
                P.op("dve", lambda e: e.tensor_scalar(out=selv, in0=lg, scalar1=srt[:, 1:2], scalar2=None, op0=ALU.is_ge), r=[rk], w=[rk, "sel"])
                P.op("dve", lambda e: e.scalar_tensor_tensor(out=gs, in0=ex, scalar=1.0, in1=selv, op0=ALU.mult, op1=ALU.mult, accum_out=den),
                     r=[rk, "sel"], w=[rk])
                P.op("dve", lambda e: e.reciprocal(out=rdn, in_=den), r=[rk], w=[rk])
                P.op("dve", lambda e: e.tensor_scalar(out=gates[:, s, :], in0=gs, scalar1=rdn, scalar2=None, op0=ALU.mult), r=[rk], w=["gates"])
            selb = alloc([128], BF16)
            cs = alloc([NT, 8], F32)
            pre = alloc([NT, 8], F32)
            cnt = alloc([8], F32)
            ntl = alloc([8], F32)
            endt = alloc([8], F32)
            baset = alloc([8], F32)
            slot = alloc([NT, 8], F32)
            m1 = alloc([NT, 8], F32)
            m2 = alloc([NT, 8], F32)
            isA = alloc([NT, 8], F32)
            gtmp = alloc([NT, 8], F32)
            slotA = alloc([NT], F32)
            slotB = alloc([NT], F32)
            gateA = alloc([NT], F32)
            gateB = alloc([NT], F32)
            tokf = alloc([NT], F32)
            rowsA = alloc([NT, 16], F32)
            rowsB = alloc([NT, 16], F32)
            zt = alloc([1024], I32)
            thr = alloc([16], F32)
            te = alloc([16], F32)
            pcol = alloc([1], F32)
            wbase = alloc([16], F32)
            f128 = alloc([NF], F32)
            widxf = alloc([16, NF], F32)
            self_flat = sel.rearrange("p a b -> p (a b)")
            P.op("dve", lambda e: e.tensor_copy(out=selb, in_=self_flat), r=["sel"], w=["selb"])
            prank, prk = pm()
            P.op("pe", lambda e: e.matmul(prank[:, 0:128], lhsT=trib[:], rhs=selb, start=True, stop=True), r=["trib", "selb"], w=[prk])
            pcs, pck = pm()
            P.op("pe", lambda e: e.matmul(pcs[:, 0:128], lhsT=onesb[:], rhs=selb, start=True, stop=True), r=["onesb", "selb"], w=[pck])
            P.op("dve", lambda e: e.tensor_copy(out=cs.rearrange("p a b -> p (a b)"), in_=pcs[:, 0:128]), r=[pck], w=["cs"])
            P.op("dve", lambda e: e.memset(pre[:, 0, :], 0.0), w=["pre"])
            for s in range(1, NT):
                P.op("dve", lambda e: e.tensor_tensor(out=pre[:, s, :], in0=pre[:, s - 1, :], in1=cs[:, s - 1, :], op=ALU.add),
                     r=["pre", "cs"], w=["pre"])
            P.op("dve", lambda e: e.tensor_tensor(out=cnt, in0=pre[:, NT - 1, :], in1=cs[:, NT - 1, :], op=ALU.add), r=["pre", "cs"], w=["cnt"])
            P.op("dve", lambda e: e.tensor_scalar(out=ntl, in0=cnt, scalar1=0.5, scalar2=None, op0=ALU.is_gt), r=["cnt"], w=["ntl"])
            for th in (512.5, 1024.5, 1536.5):
                P.op("dve", lambda e: e.scalar_tensor_tensor(out=ntl, in0=cnt, scalar=th, in1=ntl, op0=ALU.is_gt, op1=ALU.add),
                     r=["cnt", "ntl"], w=["ntl"])
            P.op("dve", lambda e: e.tensor_scalar(out=ntl, in0=ntl, scalar1=512.0, scalar2=None, op0=ALU.mult), r=["ntl"], w=["ntl"])
            P.op("dve", lambda e: e.tensor_copy(out=endt[:, 0:1], in_=ntl[:, 0:1]), r=["ntl"], w=["endt"])
            for ei in range(1, 8):
                P.op("dve", lambda e: e.tensor_tensor(out=endt[:, ei:ei + 1], in0=endt[:, ei - 1:ei], in1=ntl[:, ei:ei + 1], op=ALU.add),
                     r=["endt", "ntl"], w=["endt"])
            P.op("dve", lambda e: e.tensor_tensor(out=baset, in0=endt, in1=ntl, op=ALU.subtract), r=["endt", "ntl"], w=["baset"])
            P.op("dve", lambda e: e.tensor_tensor(out=slot, in0=prank[:, 0:128].rearrange("p (a b) -> p a b", a=NT), in1=pre, op=ALU.add),
                 r=[prk, "pre"], w=["slot"])
            P.op("dve", lambda e: e.tensor_tensor(out=slot, in0=slot, in1=baset.unsqueeze(1).to_broadcast([128, NT, 8]), op=ALU.add),
                 r=["slot", "baset"], w=["slot"])
            P.op("dve", lambda e: e.tensor_tensor(out=m1, in0=slot, in1=sel, op=ALU.mult), r=["slot", "sel"], w=["m1"])
            P.op("dve", lambda e: e.tensor_reduce(out=slotB, in_=m1, axis=AX.X, op=ALU.max), r=["m1"], w=["slotB"])
            P.op("dve", lambda e: e.tensor_scalar(out=m2, in0=sel, scalar1=-BIG, scalar2=BIG, op0=ALU.mult, op1=ALU.add), r=["sel"], w=["m2"])
            P.op("dve", lambda e: e.tensor_tensor(out=m2, in0=m2, in1=m1, op=ALU.add), r=["m2", "m1"], w=["m2"])
            P.op("dve", lambda e: e.tensor_reduce(out=slotA, in_=m2, axis=AX.X, op=ALU.min), r=["m2"], w=["slotA"])
            P.op("dve", lambda e: e.tensor_tensor(out=isA, in0=m2, in1=slotA.unsqueeze(2).to_broadcast([128, NT, 8]), op=ALU.is_equal),
                 r=["m2", "slotA"], w=["isA"])
            P.op("dve", lambda e: e.tensor_tensor(out=gtmp, in0=gates, in1=isA, op=ALU.mult), r=["gates", "isA"], w=["gtmp"])
            P.op("dve", lambda e: e.tensor_reduce(out=gateA, in_=gtmp, axis=AX.X, op=ALU.add), r=["gtmp"], w=["gateA"])
            P.op("dve", lambda e: e.tensor_tensor(out=isA, in0=sel, in1=isA, op=ALU.subtract), r=["sel", "isA"], w=["isA"])
            P.op("dve", lambda e: e.tensor_tensor(out=gtmp, in0=gates, in1=isA, op=ALU.mult), r=["gates", "isA"], w=["gtmp"])
            P.op("dve", lambda e: e.tensor_reduce(out=gateB, in_=gtmp, axis=AX.X, op=ALU.add), r=["gtmp"], w=["gateB"])
            P.op("dve", lambda e: e.tensor_copy(out=slotA_i, in_=slotA), r=["slotA"], w=["slotA_i"])
            P.op("dve", lambda e: e.tensor_copy(out=slotB_i, in_=slotB), r=["slotB"], w=["slotB_i"])
            P.op("pool", lambda e: e.iota(tokf, pattern=[[128, NT]], base=0, channel_multiplier=1, allow_small_or_imprecise_dtypes=True), w=["tokf"])
            P.op("pool", lambda e: e.iota(thr, pattern=[[512, 16]], base=0, channel_multiplier=0, allow_small_or_imprecise_dtypes=True), w=["thr"])
            P.op("pool", lambda e: e.iota(pcol, pattern=[[0, 1]], base=0, channel_multiplier=1, allow_small_or_imprecise_dtypes=True), w=["pcol"])
            P.op("pool", lambda e: e.iota(f128, pattern=[[128, NF]], base=0, channel_multiplier=0, allow_small_or_imprecise_dtypes=True), w=["f128"])
            P.op("pool", lambda e: e.memset(zt, 0), w=["zt"])
            for rows, gt_, nm in ((rowsA, gateA, "A"), (rowsB, gateB, "B")):
                rows_i = rows.bitcast(I32)
                P.op("dve", lambda e: e.memset(rows, 0.0), w=["rows" + nm])
                P.op("dve", lambda e: e.tensor_copy(out=rows_i[:, :, 0], in_=tokf), r=["tokf"], w=["rows" + nm])
                P.op("dve", lambda e: e.tensor_copy(out=rows[:, :, 1], in_=gt_), r=["gate" + nm], w=["rows" + nm])
            c_sc = P.new_chan("scat")
            c_z = P.new_chan("tabzero")
            P.op("pool", lambda e: e.dma_start(out=slot_tab_d.rearrange("(p a) c -> p (a c)", p=128), in_=zt), r=["zt"], w=["tabz"], chan=c_z)
            for s in range(NT):
                for rows, si, nm in ((rowsA, slotA_i, "A"), (rowsB, slotB_i, "B")):
                    rows_i = rows.bitcast(I32)
                    P.op("pool", lambda e: e.indirect_dma_start(
                        out=slot_tab_d, out_offset=bass.IndirectOffsetOnAxis(ap=si[:, s:s + 1], axis=0),
                        in_=rows_i[:, s, :], in_offset=None),
                        r=["rows" + nm, "slot" + nm + "_i", "tabz"], w=[("tab", nm, s)], chan=c_sc)
            tabkeys = [("tab", nm, s) for nm in "AB" for s in range(NT)]
            P.op("dve", lambda e: e.memset(te, 0.0), w=["te"])
            for ei in range(8):
                P.op("dve", lambda e: e.scalar_tensor_tensor(out=te, in0=thr, scalar=endt[:, ei:ei + 1], in1=te, op0=ALU.is_ge, op1=ALU.add),
                     r=["thr", "endt", "te"], w=["te"])
            P.op("dve", lambda e: e.tensor_scalar(out=te, in0=te, scalar1=7.0, scalar2=None, op0=ALU.min), r=["te"], w=["te"])
            P.op("dve", lambda e: e.tensor_scalar(out=wbase, in0=te, scalar1=float(DFF), scalar2=pcol[:, 0:1], op0=ALU.mult, op1=ALU.add),
                 r=["te", "pcol"], w=["wbase"])
            for i in range(16):
                P.op("dve", lambda e: e.tensor_scalar(out=widxf[:, i, :], in0=f128, scalar1=wbase[:, i:i + 1], scalar2=None, op0=ALU.add),
                     r=["f128", "wbase"], w=["widxf"])
            P.op("dve", lambda e: e.tensor_copy(out=widx, in_=widxf), r=["widxf"], w=["widx"])
            P.barrier()
            reset_arena(base0)
            gu = Ring(P, "gu", [alloc([2, 1024], BF16) for _ in range(6)])
            dr = Ring(P, "dr", [alloc([1024], BF16) for _ in range(10)])
            silu_t = [alloc([512], BF16) for _ in range(2)]
            NH1 = 8
            h1T = alloc([NH1, 512], BF16)
            hrows2 = [alloc([4, 1024], BF16) for _ in range(2)]
            hnTt = [alloc([8, 512], BF16) for _ in range(2)]
            acc = [alloc([4, 1024], F32) for _ in range(2)]
            tabt = [alloc([4, 16], F32) for _ in range(2)]
            c_tab = [P.new_chan(f"tabld{i}") for i in range(2)]
            c_hg = [P.new_chan(f"hgath{i}") for i in range(2)]
            c_ys = [P.new_chan(f"yst{i}") for i in range(2)]
            hnkeys = [("hn_d", s) for s in range(NT)]

            def prefetch(i):
                b = i % 2
                tb_i = tabt[b].bitcast(I32)
                P.op("sp", lambda e: e.dma_start(out=tb_i, in_=slot_tab_d[i * 512:(i + 1) * 512, :].rearrange("(j p) c -> p j c", p=128)),
                     r=tabkeys, w=[("tabt", b)], chan=c_tab[b])
                for j in range(4):
                    P.op("pool", lambda e: e.indirect_dma_start(
                        out=hrows2[b][:, j, :], out_offset=None, in_=hn_d,
                        in_offset=bass.IndirectOffsetOnAxis(ap=tb_i[:, j, 0:1], axis=0)),
                        r=[("tabt", b)] + hnkeys, w=[("hrows", b, j)], chan=c_hg[b])

            def transposes(i):
                b = i % 2
                hT = hnTt[b]
                for j0 in (0, 2):
                    for jj in range(2):
                        j = j0 + jj
                        for kc in range(8):
                            P.op("pe", lambda e: e.transpose(out=psTb[:, kc, jj * 128:(jj + 1) * 128],
                                                             in_=hrows2[b][:, j, kc * 128:(kc + 1) * 128], identity=identb[:]),
                                 r=[("hrows", b, j), "identb"], w=[("psT", jj)])
                    c0 = j0 * 128
                    P.op("dve", lambda e: e.tensor_tensor(
                        out=hT[:, :, c0:c0 + 256], in0=psTb[:, :, 0:256],
                        in1=gcols[:, 56:64].unsqueeze(2).to_broadcast([128, 8, 256]), op=ALU.mult),
                        r=[("psT", 0), ("psT", 1), "gcols"], w=[("hnTt", b)])

            prefetch(0)
            transposes(0)
            for i in range(16):
                b = i % 2
                tb = tabt[b]
                tbk = ("tabt", b)
                hT = hnTt[b]
                hTk = ("hnTt", b)
                ac = acc[b]
                ack = ("acc", b)
                if i + 1 < 16:
                    prefetch(i + 1)
                cstate = {"c": 0}
                info = {}

                def emit_gu(f):
                    cidx = cstate["c"]
                    cstate["c"] += 1
                    hs = cidx % NH1
                    _, gv, gch, gkey = gu.next()
                    gch2 = gu.last2
                    P.op("pool", lambda e: e.indirect_dma_start(
                        out=gv[:, 0, :], out_offset=None, in_=mg_flat,
                        in_offset=bass.IndirectOffsetOnAxis(ap=widx[:, i, f:f + 1], axis=0)), r=["widx"], w=[(gkey, 0)], chan=gch)
                    P.op("pool", lambda e: e.indirect_dma_start(
                        out=gv[:, 1, :], out_offset=None, in_=mu_flat,
                        in_offset=bass.IndirectOffsetOnAxis(ap=widx[:, i, f:f + 1], axis=0)), r=["widx"], w=[(gkey, 1)], chan=gch2)
                    _, dv, dch, dkey = dr.next()
                    P.op("pool", lambda e: e.indirect_dma_start(
                        out=dv, out_offset=None, in_=md_d,
                        in_offset=bass.IndirectOffsetOnAxis(ap=widx[:, i, f:f + 1], axis=0)), r=["widx"], w=[dkey], chan=dch)
                    info[f] = (hs, dv, dkey)
                    (pg, pgk), (pu, puk) = pm_pair()
                    for kc in range(8):
                        P.op("pe", lambda e: e.matmul(pg[:, :], lhsT=gv[:, 0, kc * 128:(kc + 1) * 128], rhs=hT[:, kc, :],
                                                      start=(kc == 0), stop=(kc == 7)), r=[(gkey, 0), hTk], w=[pgk])
                    for kc in range(8):
                        P.op("pe", lambda e: e.matmul(pu[:, :], lhsT=gv[:, 1, kc * 128:(kc + 1) * 128], rhs=hT[:, kc, :],
                                                      start=(kc == 0), stop=(kc == 7)), r=[(gkey, 1), hTk], w=[puk])
                    st_, stk = silu_t[cidx % 2], ("silu", cidx % 2)
                    P.op("act", lambda e: e.activation(out=st_, in_=pg[:, :], func=AF.Silu), r=[pgk], w=[stk])
                    P.op("dve", lambda e: e.tensor_tensor(out=h1T[:, hs, :], in0=pu[:, :], in1=st_, op=ALU.mult),
                         r=[puk, stk], w=[("h1T", hs)])

                def emit_down(gi, grp):
                    nfi = len(grp)
                    for j in range(4):
                        for half in range(2):
                            pso, pok = po()
                            for fi, f in enumerate(grp):
                                hs, dv, dkey = info[f]
                                P.op("pe", lambda e: e.matmul(pso[:, :], lhsT=h1T[:, hs, j * 128:(j + 1) * 128],
                                                              rhs=dv[:, half * 512:(half + 1) * 512],
                                                              start=(fi == 0), stop=(fi == nfi - 1)), r=[("h1T", hs), dkey], w=[pok])
                            av = ac[:, j, half * 512:(half + 1) * 512]
                            gap = tb[:, j, 1:2]
                            if gi == 0:
                                P.op("dve", lambda e: e.tensor_scalar(out=av, in0=pso[:, :], scalar1=gap, scalar2=None, op0=ALU.mult),
                                     r=[pok, tbk], w=[ack])
                            else:
                                P.op("dve", lambda e: e.scalar_tensor_tensor(out=av, in0=pso[:, :], scalar=gap, in1=av,
                                                                             op0=ALU.mult, op1=ALU.add), r=[pok, tbk, ack], w=[ack])

                for gi, grp in enumerate(FGROUPS):
                    for k, f in enumerate(grp):
                        if k == 0 and gi > 0:
                            continue
                        emit_gu(f)
                    if gi + 1 < len(FGROUPS):
                        emit_gu(FGROUPS[gi + 1][0])
                    if gi == 1 and i + 1 < 16:
                        transposes(i + 1)
                    emit_down(gi, grp)
                P.op("sp", lambda e: e.dma_start(out=yslot_d[i * 512:(i + 1) * 512, :].rearrange("(j p) d -> p j d", p=128), in_=ac),
                     r=[ack], w=[("yslot", i)], chan=c_ys[b])
            P.barrier()
            reset_arena(base0)
            ya = [alloc([1024], F32) for _ in range(2)]
            yb = [alloc([1024], F32) for _ in range(2)]
            c_ya = [P.new_chan(f"ya{i}") for i in range(2)]
            c_yb = [P.new_chan(f"yb{i}") for i in range(2)]
            for s in range(NT):
                b = s % 2
                for yy, si, cc, nm in ((ya[b], slotA_i, c_ya[b], "ya"), (yb[b], slotB_i, c_yb[b], "yb")):
                    P.op("pool", lambda e: e.indirect_dma_start(
                        out=yy, out_offset=None, in_=yslot_d, in_offset=bass.IndirectOffsetOnAxis(ap=si[:, s:s + 1], axis=0)),
                        r=["slotA_i", "slotB_i"], w=[(nm, b)], chan=cc)
                    P.op("dve", lambda e: e.tensor_tensor(out=h_t[:, s, :], in0=h_t[:, s, :], in1=yy, op=ALU.add),
                         r=[(nm, b), hkey(s)], w=[hkey(s)])
            P.barrier()

        def moe2_stage1(r):
            gates, sel = gates_g, sel_g
            reset_arena()
            junk = alloc([1024], BF16)
            hn32 = [alloc([1024], F32) for _ in range(2)]
            hb16 = [alloc([1024], BF16) for _ in range(2)]
            hnT32 = [alloc([8, 128], F32) for _ in range(2)]
            wr = alloc([8, 8], F32)
            rt = alloc([NT, 64], F32)
            c_hn = [P.new_chan(f"hnst{i}") for i in range(2)]
            P.op("sp", lambda e: e.dma_start(out=wr, in_=mr_d.rearrange("p (k e) -> p k e", k=8)), w=["wr"], chan=c_ph)
            for s in range(NT):
                src, skey = h_t[:, s, :], hkey(s)
                P.op("act", lambda e: e.activation(out=junk, in_=src, func=AF.Square, accum_out=stat[:, s:s + 1]),
                     r=[skey], w=["junk", ("mss", s)])
            P.op("act", lambda e: e.activation(out=stat[:, 16:32], in_=stat[:, 0:16], func=AF.Sqrt, scale=1.0 / D, bias=eps6[:, 0:1]),
                 r=[("mss", s) for s in range(NT)] + ["eps6"], w=["msd"])
            P.op("dve", lambda e: e.reciprocal(out=stat[:, 32:48], in_=stat[:, 16:32]), r=["msd"], w=["mrs"])
            for s in range(NT):
                src, skey = h_t[:, s, :], hkey(s)
                col = 30 + s
                hb, hbk = hn32[s % 2], ("hn32", s % 2)
                P.op("act", lambda e: e.activation(out=hb, in_=src, func=AF.Identity, scale=stat[:, col + 2:col + 3]),
                     r=[skey, "mrs"], w=[hbk])
                h16, h16k = hb16[s % 2], ("hb16", s % 2)
                P.op("act", lambda e: e.activation(out=h16, in_=src, func=AF.Identity, scale=stat[:, col + 2:col + 3]),
                     r=[skey, "mrs"], w=[h16k])
                P.op("sp", lambda e: e.dma_start(out=hn_d[(r * NT + s) * 128:(r * NT + s + 1) * 128, :], in_=h16), r=[h16k], w=[("hn_d", r * NT + s)],
                     chan=c_hn[s % 2])
                for kc in range(8):
                    P.op("pe", lambda e: e.transpose(out=psTf[:, kc, :], in_=hb[:, kc * 128:(kc + 1) * 128], identity=identf[:]),
                         r=[hbk, "identf"], w=["psTf"])
                ht, htk = hnT32[s % 2], ("hnT32", s % 2)
                P.op("dve", lambda e: e.tensor_tensor(
                    out=ht, in0=psTf, in1=gcols[:, 56:64].unsqueeze(2).to_broadcast([128, 8, 128]), op=ALU.mult),
                    r=["psTf", "gcols"], w=[htk])
                ps, pk = pm()
                for kc in range(8):
                    P.op("pe", lambda e: e.matmul(ps[:, 0:8], lhsT=ht[:, kc, :], rhs=wr[:, kc, :], start=(kc == 0), stop=(kc == 7)),
                         r=[htk, "wr"], w=[pk])
                rk = ("rt", s)
                lg = rt[:, s, 0:8]
                srt = rt[:, s, 8:16]
                nm1 = rt[:, s, 16:17]
                ex = rt[:, s, 24:32]
                gs = rt[:, s, 40:48]
                den = rt[:, s, 48:49]
                rdn = rt[:, s, 49:50]
                selv = sel[:, r * NT + s, :]
                P.op("dve", lambda e: e.tensor_copy(out=lg, in_=ps[:, 0:8]), r=[pk], w=[rk])
                P.op("dve", lambda e: e.max(out=srt, in_=lg), r=[rk], w=[rk])
                P.op("dve", lambda e: e.tensor_scalar(out=nm1, in0=srt[:, 0:1], scalar1=-1.0, scalar2=None, op0=ALU.mult), r=[rk], w=[rk])
                P.op("act", lambda e: e.activation(out=ex, in_=lg, func=AF.Exp, bias=nm1), r=[rk], w=[rk])
                P.op("dve", lambda e: e.tensor_scalar(out=selv, in0=lg, scalar1=srt[:, 1:2], scalar2=None, op0=ALU.is_ge), r=[rk], w=[rk, "sel"])
                P.op("dve", lambda e: e.scalar_tensor_tensor(out=gs, in0=ex, scalar=1.0, in1=selv, op0=ALU.mult, op1=ALU.mult, accum_out=den),
                     r=[rk, "sel"], w=[rk])
                P.op("dve", lambda e: e.reciprocal(out=rdn, in_=den), r=[rk], w=[rk])
                P.op("dve", lambda e: e.tensor_scalar(out=gates[:, r * NT + s, :], in0=gs, scalar1=rdn, scalar2=None, op0=ALU.mult), r=[rk], w=["gates"])
            if r < nranges - 1:
                dst = hpark_d[r * RANGE:(r + 1) * RANGE, :].rearrange("(s p) d -> p s d", p=128)
                for q in range(4):
                    P.op("sp", lambda e: e.dma_start(out=dst[:, q * 4:(q + 1) * 4, :], in_=h_t[:, q * 4:(q + 1) * 4, :]),
                         r=[hkey(s) for s in range(q * 4, q * 4 + 4)], w=[("hpark", r, q)], chan=c_xs[q])
            P.barrier()

        def moe2_rest(nr):
            BIG = 65536.0
            NTT = NT * nr
            NTL = 8 * nr + 8
            gates, sel, slotA_i, slotB_i, widx = gates_g, sel_g, slotA_ig, slotB_ig, widx_g
            reset_arena()
            base0 = state["off"]
            selb = alloc([NTT * 8], BF16)
            cs = alloc([NTT, 8], F32)
            pre = alloc([NTT, 8], F32)
            cnt = alloc([8], F32)
            ntl = alloc([8], F32)
            endt = alloc([8], F32)
            baset = alloc([8], F32)
            slot = alloc([NTT, 8], F32)
            m1 = alloc([NTT, 8], F32)
            m2 = alloc([NTT, 8], F32)
            isA = alloc([NTT, 8], F32)
            gtmp = alloc([NTT, 8], F32)
            slotA = alloc([NTT], F32)
            slotB = alloc([NTT], F32)
            gateA = alloc([NTT], F32)
            gateB = alloc([NTT], F32)
            tokf = alloc([NTT], F32)
            rowsA = alloc([NTT, 16], F32)
            rowsB = alloc([NTT, 16], F32)
            zt = alloc([NTL * 64], I32)
            thr = alloc([NTL], F32)
            te = alloc([NTL], F32)
            pcol = alloc([1], F32)
            wbase = alloc([NTL], F32)
            f128 = alloc([NF], F32)
            widxf = alloc([NTL, NF], F32)
            self_flat = sel.rearrange("p a b -> p (a b)")
            P.op("dve", lambda e: e.tensor_copy(out=selb, in_=self_flat), r=["sel"], w=["selb"])
            prank, prk = pm()
            P.op("pe", lambda e: e.matmul(prank[:, 0:NTT * 8], lhsT=trib[:], rhs=selb, start=True, stop=True), r=["trib", "selb"], w=[prk])
            pcs, pck = pm()
            P.op("pe", lambda e: e.matmul(pcs[:, 0:NTT * 8], lhsT=onesb[:], rhs=selb, start=True, stop=True), r=["onesb", "selb"], w=[pck])
            P.op("dve", lambda e: e.tensor_copy(out=cs.rearrange("p a b -> p (a b)"), in_=pcs[:, 0:NTT * 8]), r=[pck], w=["cs"])
            P.op("dve", lambda e: e.memset(pre[:, 0, :], 0.0), w=["pre"])
            for s in range(1, NTT):
                P.op("dve", lambda e: e.tensor_tensor(out=pre[:, s, :], in0=pre[:, s - 1, :], in1=cs[:, s - 1, :], op=ALU.add),
                     r=["pre", "cs"], w=["pre"])
            P.op("dve", lambda e: e.tensor_tensor(out=cnt, in0=pre[:, NTT - 1, :], in1=cs[:, NTT - 1, :], op=ALU.add), r=["pre", "cs"], w=["cnt"])
            P.op("dve", lambda e: e.tensor_scalar(out=ntl, in0=cnt, scalar1=0.5, scalar2=None, op0=ALU.is_gt), r=["cnt"], w=["ntl"])
            for th in [512.0 * k + 0.5 for k in range(1, NTT // 4)]:
                P.op("dve", lambda e: e.scalar_tensor_tensor(out=ntl, in0=cnt, scalar=th, in1=ntl, op0=ALU.is_gt, op1=ALU.add),
                     r=["cnt", "ntl"], w=["ntl"])
            P.op("dve", lambda e: e.tensor_scalar(out=ntl, in0=ntl, scalar1=512.0, scalar2=None, op0=ALU.mult), r=["ntl"], w=["ntl"])
            P.op("dve", lambda e: e.tensor_copy(out=endt[:, 0:1], in_=ntl[:, 0:1]), r=["ntl"], w=["endt"])
            for ei in range(1, 8):
                P.op("dve", lambda e: e.tensor_tensor(out=endt[:, ei:ei + 1], in0=endt[:, ei - 1:ei], in1=ntl[:, ei:ei + 1], op=ALU.add),
                     r=["endt", "ntl"], w=["endt"])
            P.op("dve", lambda e: e.tensor_tensor(out=baset, in0=endt, in1=ntl, op=ALU.subtract), r=["endt", "ntl"], w=["baset"])
            P.op("dve", lambda e: e.tensor_tensor(out=slot, in0=prank[:, 0:NTT * 8].rearrange("p (a b) -> p a b", a=NTT), in1=pre, op=ALU.add),
                 r=[prk, "pre"], w=["slot"])
            P.op("dve", lambda e: e.tensor_tensor(out=slot, in0=slot, in1=baset.unsqueeze(1).to_broadcast([128, NTT, 8]), op=ALU.add),
                 r=["slot", "baset"], w=["slot"])
            P.op("dve", lambda e: e.tensor_tensor(out=m1, in0=slot, in1=sel, op=ALU.mult), r=["slot", "sel"], w=["m1"])
            P.op("dve", lambda e: e.tensor_reduce(out=slotB, in_=m1, axis=AX.X, op=ALU.max), r=["m1"], w=["slotB"])
            P.op("dve", lambda e: e.tensor_scalar(out=m2, in0=sel, scalar1=-BIG, scalar2=BIG, op0=ALU.mult, op1=ALU.add), r=["sel"], w=["m2"])
            P.op("dve", lambda e: e.tensor_tensor(out=m2, in0=m2, in1=m1, op=ALU.add), r=["m2", "m1"], w=["m2"])
            P.op("dve", lambda e: e.tensor_reduce(out=slotA, in_=m2, axis=AX.X, op=ALU.min), r=["m2"], w=["slotA"])
            P.op("dve", lambda e: e.tensor_tensor(out=isA, in0=m2, in1=slotA.unsqueeze(2).to_broadcast([128, NTT, 8]), op=ALU.is_equal),
                 r=["m2", "slotA"], w=["isA"])
            P.op("dve", lambda e: e.tensor_tensor(out=gtmp, in0=gates, in1=isA, op=ALU.mult), r=["gates", "isA"], w=["gtmp"])
            P.op("dve", lambda e: e.tensor_reduce(out=gateA, in_=gtmp, axis=AX.X, op=ALU.add), r=["gtmp"], w=["gateA"])
            P.op("dve", lambda e: e.tensor_tensor(out=isA, in0=sel, in1=isA, op=ALU.subtract), r=["sel", "isA"], w=["isA"])
            P.op("dve", lambda e: e.tensor_tensor(out=gtmp, in0=gates, in1=isA, op=ALU.mult), r=["gates", "isA"], w=["gtmp"])
            P.op("dve", lambda e: e.tensor_reduce(out=gateB, in_=gtmp, axis=AX.X, op=ALU.add), r=["gtmp"], w=["gateB"])
            for sl_, nm_ in ((slotA, "slotA"), (slotB, "slotB")):
                P.op("dve", lambda e: e.tensor_scalar(out=sl_, in0=sl_, scalar1=0.0, scalar2=float(NTL * 512 - 1),
                                                      op0=ALU.max, op1=ALU.min), r=[nm_], w=[nm_])
            P.op("dve", lambda e: e.tensor_copy(out=slotA_i, in_=slotA), r=["slotA"], w=["slotA_i"])
            P.op("dve", lambda e: e.tensor_copy(out=slotB_i, in_=slotB), r=["slotB"], w=["slotB_i"])
            P.op("pool", lambda e: e.iota(tokf, pattern=[[128, NTT]], base=0, channel_multiplier=1, allow_small_or_imprecise_dtypes=True), w=["tokf"])
            P.op("pool", lambda e: e.iota(thr, pattern=[[512, NTL]], base=0, channel_multiplier=0, allow_small_or_imprecise_dtypes=True), w=["thr"])
            P.op("pool", lambda e: e.iota(pcol, pattern=[[0, 1]], base=0, channel_multiplier=1, allow_small_or_imprecise_dtypes=True), w=["pcol"])
            P.op("pool", lambda e: e.iota(f128, pattern=[[128, NF]], base=0, channel_multiplier=0, allow_small_or_imprecise_dtypes=True), w=["f128"])
            P.op("pool", lambda e: e.memset(zt, 0), w=["zt"])
            for rows, gt_, nm in ((rowsA, gateA, "A"), (rowsB, gateB, "B")):
                rows_i = rows.bitcast(I32)
                P.op("dve", lambda e: e.memset(rows, 0.0), w=["rows" + nm])
                P.op("dve", lambda e: e.tensor_copy(out=rows_i[:, :, 0], in_=tokf), r=["tokf"], w=["rows" + nm])
                P.op("dve", lambda e: e.tensor_copy(out=rows[:, :, 1], in_=gt_), r=["gate" + nm], w=["rows" + nm])
            c_scs = [P.new_chan(f"scat{i}") for i in range(4)]
            c_z = P.new_chan("tabzero")
            P.op("pool", lambda e: e.dma_start(out=slot_tab_d.rearrange("(p a) c -> p (a c)", p=128), in_=zt), r=["zt"], w=["tabz"], chan=c_z)
            for s in range(NTT):
                for rows, si, nm in ((rowsA, slotA_i, "A"), (rowsB, slotB_i, "B")):
                    rows_i = rows.bitcast(I32)
                    P.op("pool", lambda e: e.indirect_dma_start(
                        out=slot_tab_d, out_offset=bass.IndirectOffsetOnAxis(ap=si[:, s:s + 1], axis=0),
                        in_=rows_i[:, s, :], in_offset=None),
                        r=["rows" + nm, "slot" + nm + "_i", "tabz", ("scq", (2 * s + (nm == "B")) % 4)],
                        w=[("tab", nm, s), ("scq", (2 * s + (nm == "B")) % 4)], chan=c_scs[(2 * s + (nm == "B")) % 4])
            tabkeys = [("tab", nm, s) for nm in "AB" for s in range(NTT)]
            P.op("dve", lambda e: e.memset(te, 0.0), w=["te"])
            for ei in range(8):
                P.op("dve", lambda e: e.scalar_tensor_tensor(out=te, in0=thr, scalar=endt[:, ei:ei + 1], in1=te, op0=ALU.is_ge, op1=ALU.add),
                     r=["thr", "endt", "te"], w=["te"])
            P.op("dve", lambda e: e.tensor_scalar(out=te, in0=te, scalar1=7.0, scalar2=0.0, op0=ALU.min, op1=ALU.max), r=["te"], w=["te"])
            P.op("dve", lambda e: e.tensor_scalar(out=wbase, in0=te, scalar1=float(DFF), scalar2=pcol[:, 0:1], op0=ALU.mult, op1=ALU.add),
                 r=["te", "pcol"], w=["wbase"])
            for i in range(NTL):
                P.op("dve", lambda e: e.tensor_scalar(out=widxf[:, i, :], in0=f128, scalar1=wbase[:, i:i + 1], scalar2=None, op0=ALU.add),
                     r=["f128", "wbase"], w=["widxf"])
            P.op("dve", lambda e: e.tensor_copy(out=widx, in_=widxf), r=["widxf"], w=["widx"])
            P.barrier()
            reset_arena(base0)
            gu = Ring(P, "gu", [alloc([2, 1024], BF16) for _ in range(6)])
            dr = Ring(P, "dr", [alloc([1024], BF16) for _ in range(11)])
            silu_t = [alloc([512], BF16) for _ in range(2)]
            NH1 = 10
            h1T = alloc([NH1, 512], BF16)
            hrows2 = [alloc([4, 1024], BF16) for _ in range(2)]
            hnTt = [alloc([8, 512], BF16) for _ in range(2)]
            acc = [alloc([4, 1024], F32) for _ in range(2)]
            tabt = [alloc([4, 16], F32) for _ in range(2)]
            c_tab = [P.new_chan(f"tabld{i}") for i in range(2)]
            c_hg = [P.new_chan(f"hgath{i}") for i in range(2)]
            c_ys = [P.new_chan(f"yst{i}") for i in range(2)]
            hnkeys = [("hn_d", s) for s in range(NTT)]

            def prefetch(i):
                b = i % 2
                tb_i = tabt[b].bitcast(I32)
                P.op("sp", lambda e: e.dma_start(out=tb_i, in_=slot_tab_d[i * 512:(i + 1) * 512, :].rearrange("(j p) c -> p j c", p=128)),
                     r=tabkeys, w=[("tabt", b)], chan=c_tab[b])
                for j in range(4):
                    P.op("pool", lambda e: e.indirect_dma_start(
                        out=hrows2[b][:, j, :], out_offset=None, in_=hn_d,
                        in_offset=bass.IndirectOffsetOnAxis(ap=tb_i[:, j, 0:1], axis=0)),
                        r=[("tabt", b)] + hnkeys, w=[("hrows", b, j)], chan=c_hg[b])

            def transposes(i):
                b = i % 2
                hT = hnTt[b]
                for j0 in (0, 2):
                    for jj in range(2):
                        j = j0 + jj
                        for kc in range(8):
                            P.op("pe", lambda e: e.transpose(out=psTb[:, kc, jj * 128:(jj + 1) * 128],
                                                             in_=hrows2[b][:, j, kc * 128:(kc + 1) * 128], identity=identb[:]),
                                 r=[("hrows", b, j), "identb"], w=[("psT", jj)])
                    c0 = j0 * 128
                    P.op("dve", lambda e: e.tensor_tensor(
                        out=hT[:, :, c0:c0 + 256], in0=psTb[:, :, 0:256],
                        in1=gcols[:, 56:64].unsqueeze(2).to_broadcast([128, 8, 256]), op=ALU.mult),
                        r=[("psT", 0), ("psT", 1), "gcols"], w=[("hnTt", b)])

            prefetch(0)
            transposes(0)
            for i in range(NTL):
                b = i % 2
                tb = tabt[b]
                tbk = ("tabt", b)
                hT = hnTt[b]
                hTk = ("hnTt", b)
                ac = acc[b]
                ack = ("acc", b)
                if i + 1 < NTL:
                    prefetch(i + 1)
                cstate = {"c": 0}
                info = {}

                def emit_gu(f):
                    cidx = cstate["c"]
                    cstate["c"] += 1
                    hs = cidx % NH1
                    _, gv, gch, gkey = gu.next()
                    gch2 = gu.last2
                    P.op("pool", lambda e: e.indirect_dma_start(
                        out=gv[:, 0, :], out_offset=None, in_=mg_flat,
                        in_offset=bass.IndirectOffsetOnAxis(ap=widx[:, i, f:f + 1], axis=0)), r=["widx"], w=[(gkey, 0)], chan=gch)
                    P.op("pool", lambda e: e.indirect_dma_start(
                        out=gv[:, 1, :], out_offset=None, in_=mu_flat,
                        in_offset=bass.IndirectOffsetOnAxis(ap=widx[:, i, f:f + 1], axis=0)), r=["widx"], w=[(gkey, 1)], chan=gch2)
                    _, dv, dch, dkey = dr.next()
                    P.op("pool", lambda e: e.indirect_dma_start(
                        out=dv, out_offset=None, in_=md_d,
                        in_offset=bass.IndirectOffsetOnAxis(ap=widx[:, i, f:f + 1], axis=0)), r=["widx"], w=[dkey], chan=dch)
                    info[f] = (hs, dv, dkey)
                    (pg, pgk), (pu, puk) = pm_pair()
                    for kc in range(8):
                        P.op("pe", lambda e: e.matmul(pg[:, :], lhsT=gv[:, 0, kc * 128:(kc + 1) * 128], rhs=hT[:, kc, :],
                                                      start=(kc == 0), stop=(kc == 7)), r=[(gkey, 0), hTk], w=[pgk])
                    for kc in range(8):
                        P.op("pe", lambda e: e.matmul(pu[:, :], lhsT=gv[:, 1, kc * 128:(kc + 1) * 128], rhs=hT[:, kc, :],
                                                      start=(kc == 0), stop=(kc == 7)), r=[(gkey, 1), hTk], w=[puk])
                    st_, stk = silu_t[cidx % 2], ("silu", cidx % 2)
                    P.op("act", lambda e: e.activation(out=st_, in_=pg[:, :], func=AF.Silu), r=[pgk], w=[stk])
                    P.op("dve", lambda e: e.tensor_tensor(out=h1T[:, hs, :], in0=pu[:, :], in1=st_, op=ALU.mult),
                         r=[puk, stk], w=[("h1T", hs)])

                def emit_down(gi, grp):
                    nfi = len(grp)
                    for j in range(4):
                        for half in range(2):
                            pso, pok = po()
                            for fi, f in enumerate(grp):
                                hs, dv, dkey = info[f]
                                P.op("pe", lambda e: e.matmul(pso[:, :], lhsT=h1T[:, hs, j * 128:(j + 1) * 128],
                                                              rhs=dv[:, half * 512:(half + 1) * 512],
                                                              start=(fi == 0), stop=(fi == nfi - 1)), r=[("h1T", hs), dkey], w=[pok])
                            av = ac[:, j, half * 512:(half + 1) * 512]
                            gap = tb[:, j, 1:2]
                            if gi == 0:
                                P.op("dve", lambda e: e.tensor_scalar(out=av, in0=pso[:, :], scalar1=gap, scalar2=None, op0=ALU.mult),
                                     r=[pok, tbk], w=[ack])
                            else:
                                P.op("dve", lambda e: e.scalar_tensor_tensor(out=av, in0=pso[:, :], scalar=gap, in1=av,
                                                                             op0=ALU.mult, op1=ALU.add), r=[pok, tbk, ack], w=[ack])

                for gi, grp in enumerate(FGROUPS):
                    for k, f in enumerate(grp):
                        if k == 0 and gi > 0:
                            continue
                        emit_gu(f)
                    if gi + 1 < len(FGROUPS):
                        emit_gu(FGROUPS[gi + 1][0])
                    if gi == 1 and i + 1 < NTL:
                        transposes(i + 1)
                    emit_down(gi, grp)
                P.op("sp", lambda e: e.dma_start(out=yslot_d[i * 512:(i + 1) * 512, :].rearrange("(j p) d -> p j d", p=128), in_=ac),
                     r=[ack], w=[("yslot", i)], chan=c_ys[b])
            P.barrier()
            for r in [nr - 1] + list(range(nr - 1)):
                reset_arena(base0)
                ya = [alloc([1024], F32) for _ in range(2)]
                yb = [alloc([1024], F32) for _ in range(2)]
                c_ya = [P.new_chan(f"ya{i}") for i in range(2)]
                c_yb = [P.new_chan(f"yb{i}") for i in range(2)]
                srcp = hpark_d[r * RANGE:(r + 1) * RANGE, :].rearrange("(s p) d -> p s d", p=128)
                for q in range(4):
                    if r == nr - 1:
                        break
                    P.op("sp", lambda e: e.dma_start(out=h_t[:, q * 4:(q + 1) * 4, :], in_=srcp[:, q * 4:(q + 1) * 4, :]),
                         w=[hkey(s) for s in range(q * 4, q * 4 + 4)], chan=c_xs[q])
                for s in range(NT):
                    b = s % 2
                    S = r * NT + s
                    for yy, si, cc, nm in ((ya[b], slotA_i, c_ya[b], "ya"), (yb[b], slotB_i, c_yb[b], "yb")):
                        P.op("pool", lambda e: e.indirect_dma_start(
                            out=yy, out_offset=None, in_=yslot_d, in_offset=bass.IndirectOffsetOnAxis(ap=si[:, S:S + 1], axis=0)),
                            r=["slotA_i", "slotB_i"], w=[(nm, b)], chan=cc)
                        P.op("dve", lambda e: e.tensor_tensor(out=h_t[:, s, :], in0=h_t[:, s, :], in1=yy, op=ALU.add),
                             r=[(nm, b), hkey(s)], w=[hkey(s)])
                P.barrier()
                final_norm(r)

        def final_norm(r):
            reset_arena()
            junk = alloc([1024], BF16)
            ot = [alloc([1024], F32) for _ in range(2)]
            for s in range(NT):
                src, skey = h_t[:, s, :], hkey(s)
                col = (s % 4) * 4
                rstd_of(src, skey, junk, col, eps6, "eps6")
                o, ok = ot[s % 2], ("ot", s % 2)
                P.op("act", lambda e, src=src, o=o, col=col: e.activation(out=o, in_=src, func=AF.Identity, scale=stat[:, col + 2:col + 3]),
                     r=[skey, ("stat", col + 2)], w=[ok])
                P.op("dve", lambda e, o=o: e.tensor_tensor(out=o, in0=o, in1=gfin[:], op=ALU.mult), r=[ok, "gfin"], w=[ok])
                row = r * RANGE + s * 128
                P.op("sp", lambda e, o=o, row=row: e.dma_start(out=y_d[row:row + 128, :], in_=o), r=[ok], chan=c_outs[s % 2])
            P.barrier()

        def dump_h(r):
            dst = y_d[r * RANGE:(r + 1) * RANGE, :].rearrange("(s p) d -> p s d", p=128)
            for q in range(4):
                P.op("sp", lambda e, q=q: e.dma_start(out=dst[:, q * 4:(q + 1) * 4, :], in_=h_t[:, q * 4:(q + 1) * 4, :]),
                     r=[hkey(s) for s in range(q * 4, q * 4 + 4)], chan=c_out)
            P.barrier()

        phases = [("mix0", lambda r: conv_mixer(r)), ("xa0", lambda r: xattn(r, 0)), ("ffn0", lambda r: ffn_dense(r)),
                  ("mix1", lambda r: gmlp(r)), ("xa1", lambda r: xattn(r, 1)), ("ffn1", lambda r: (moe_sparse(r) if SPARSE else moe(r)))]
        P.barrier()
        combined = SPARSE and COMBINED and stop is None and only is None
        for r in range(nranges):
            load_x(r)
            stopped = False
            for name, fn in phases:
                if only is not None and name not in only:
                    continue
                if combined and name == "ffn1":
                    moe2_stage1(r)
                    continue
                fn(r)
                if stop == name:
                    stopped = True
                    break
            if combined:
                continue
            if stopped:
                dump_h(r)
            else:
                final_norm(r)
        if combined:
            moe2_rest(nranges)

        with nc.Block() as block:
            @block.tensor
            def _(e):
                P.replay("pe", e)

            @block.scalar
            def _(e):
                P.replay("act", e)

            @block.vector
            def _(e):
                P.replay("dve", e)

            @block.gpsimd
            def _(e):
                P.replay("pool", e)

            @block.sync
            def _(e):
                P.replay("sp", e)
    return nc


def _chunks(W):
    K, N = W.shape
    kc, ncn = K // 128, N // 128
    return np.ascontiguousarray(W.reshape(kc, 128, ncn, 128).transpose(2, 1, 0, 3)).reshape(ncn, 128, kc * 128)


def _cols(v):
    return np.ascontiguousarray(v.reshape(-1, 128).T)


def _rep(v):
    v = np.asarray(v).reshape(-1)
    return np.ascontiguousarray(np.broadcast_to(v[None, :], (128, v.shape[0])))


def prepare_inputs(inp):
    f = lambda a: np.ascontiguousarray(np.asarray(a, dtype=np.float32))
    x = f(inp["x"])
    mem = f(inp["mem"])
    shared = {}
    g = [inp["norm_mix_g"][0], inp["norm_xattn_g"][0], inp["norm_mem_g"][0], inp["norm_ffn_g"][0],
         inp["norm_mix_g"][1], inp["norm_xattn_g"][1], inp["norm_mem_g"][1], inp["norm_ffn_g"][1]]
    shared["gcols"] = np.ascontiguousarray(np.concatenate([_cols(f(v)) for v in g], axis=1))
    shared["gfin"] = _rep(f(inp["final_norm_g"]))
    shared["ident"] = np.eye(128, dtype=np.float32)
    shared["tri"] = np.triu(np.ones((128, 128), np.float32), 1)
    shared["cvin"] = _chunks(f(inp["cv_w_in"][0]))
    aw = f(inp["cv_a_conv_w"][0])
    bw = f(inp["cv_b_conv_w"][0])
    cvsm = np.zeros((128, 148), np.float32)
    cvsm[:, 0:124] = aw.reshape(31, 4, 128).transpose(2, 1, 0).reshape(128, 124)
    cvsm[:, 124:128] = _cols(f(inp["cv_a_conv_b"][0]))
    cvsm[:, 128:132] = _cols(f(inp["cv_a_ln_g"][0]))
    cvsm[:, 132:136] = _cols(f(inp["cv_a_ln_b"][0]))
    cvsm[:, 136:148] = bw.reshape(3, 4, 128).transpose(2, 1, 0).reshape(128, 12)
    shared["cvsm"] = cvsm
    shared["cvout"] = f(inp["cv_w_out"][0])
    for i in range(2):
        shared[f"xq{i}"] = _chunks(f(inp["xa_w_q"][i]))
        shared[f"xk{i}"] = _chunks(f(inp["xa_w_k"][i]))
        shared[f"xv{i}"] = f(inp["xa_w_v"][i])
        shared[f"xo{i}"] = f(inp["xa_w_o"][i])
    shared["fg"] = _chunks(f(inp["ffn_w_gate"][0]))
    shared["fu"] = _chunks(f(inp["ffn_w_up"][0]))
    shared["fd"] = f(inp["ffn_w_down"][0])
    sgin = f(inp["sg_w_in"][0])
    shared["sgu"] = _chunks(sgin[:, 0:1024])
    shared["sgv"] = np.ascontiguousarray(sgin[:, 1024:2048])
    shared["sglng"] = _rep(f(inp["sg_ln_g"][0]))
    shared["sglnb"] = _rep(f(inp["sg_ln_b"][0]))
    ws = f(inp["sg_w_s"][0])
    shared["sgws"] = np.ascontiguousarray(ws.transpose(2, 0, 1)).reshape(128, 1024)
    shared["sgbs"] = _rep(f(inp["sg_b_s"][0]))
    shared["sgout"] = f(inp["sg_w_out"][0])
    shared["mr"] = np.ascontiguousarray(f(inp["moe_w_router"][0]).reshape(8, 128, 8).transpose(1, 0, 2)).reshape(128, 64)
    mg = f(inp["moe_w_gate"][0])
    mu = f(inp["moe_w_up"][0])
    shared["mg"] = np.concatenate([_chunks(mg[e]) for e in range(NE)], axis=0)
    shared["mu"] = np.concatenate([_chunks(mu[e]) for e in range(NE)], axis=0)
    shared["md"] = f(inp["moe_w_down"][0]).reshape(NE * DFF, D)
    in_maps = []
    for c in range(NCORES):
        b, hf = c // 2, c % 2
        t0 = hf * TOK_CORE
        m = dict(shared)
        m["x"] = np.ascontiguousarray(x[b, t0:t0 + TOK_CORE])
        xh = np.zeros((2, 128, D), np.float32)
        for r in range(2):
            s = t0 + r * RANGE
            if s >= 128:
                xh[r] = x[b, s - 128:s]
        m["xh"] = xh
        m["mem"] = mem[b]
        in_maps.append(m)
    return in_maps


_NC_CACHE = {}


def kernel(**inputs):
    in_maps = prepare_inputs(inputs)
    if "nc" not in _NC_CACHE:
        _NC_CACHE["nc"] = build_program()
    nc = _NC_CACHE["nc"]
    res = run_bass_kernel_spmd(nc, in_maps, core_ids=list(range(NCORES)))
    out = np.empty((4, SEQ, D), np.float32)
    for c in range(NCORES):
        b, hf = c // 2, c % 2
        out[b, hf * TOK_CORE:(hf + 1) * TOK_CORE] = res.results[c]["y"]
    return out
```
